# Optimizing a Trainium2 kernel written in Bass

```python
import math
import functools
import jax
import jax.numpy as jnp
from jax import lax
import numpy as np

D_MODEL = 1024
BATCH = 1
SEQ = 16384
DEPTH = 1
DEC_BATCH = 128
DEC_SEQ = 1
PAST_LEN = 8192
PAGE_SIZE = 128

HEAD_DIM = 64
HEADS_PER_GROUP = 8
DIL_GROUPS = ((128, 1), (512, 4), (2048, 16))
N_DIL_GROUPS = len(DIL_GROUPS)
N_ATTN_HEADS = N_DIL_GROUPS * HEADS_PER_GROUP
QKV_WIDTH = N_ATTN_HEADS * HEAD_DIM
ATTN_OUT_WIDTH = HEADS_PER_GROUP * HEAD_DIM
SCALE = HEAD_DIM ** -0.5
POOL_WINDOWS = (2, 4, 8, 16)
N_POOL_GROUPS = len(POOL_WINDOWS)
POOL_WIDTH = D_MODEL // 2
POOL_GROUP_WIDTH = POOL_WIDTH // N_POOL_GROUPS
POOL_BUF = max(POOL_WINDOWS) - 1
SPLITS = (QKV_WIDTH, 2 * QKV_WIDTH, 3 * QKV_WIDTH, 3 * QKV_WIDTH + POOL_WIDTH,
          3 * QKV_WIDTH + POOL_WIDTH + D_MODEL)
IN_WIDTH = 3 * QKV_WIDTH + POOL_WIDTH + 2 * D_MODEL
NUM_BUCKETS = 32
MAX_DISTANCE = 2048
N_EXPERTS = 32
TOP_K = 4
D_FF = D_MODEL
SWIGLU_LIMIT = 7.0
SWIGLU_ALPHA = 1.702
MOE_BLOCK = 128
N_ADA = 6
EPS = 1e-6
NEG_INF = -1e30

kernel_name = "hybrid_dilated_attn_pool_moe_step"


def rms_norm(x, g):
    xf = x.astype(jnp.float32)
    y = xf * lax.rsqrt(jnp.mean(xf * xf, axis=-1, keepdims=True) + EPS)
    return (y * g.astype(jnp.float32)).astype(x.dtype)


def t5_bucket(dist):
    max_exact = NUM_BUCKETS // 2
    d = dist.astype(jnp.int32)
    ratio = jnp.log(jnp.maximum(d, 1).astype(jnp.float32) / max_exact) / math.log(MAX_DISTANCE / max_exact)
    large = jnp.minimum(max_exact + (ratio * (NUM_BUCKETS - max_exact)).astype(jnp.int32), NUM_BUCKETS - 1)
    return jnp.where(d < max_exact, d, large)


def softmax_lse(logits):
    m = jnp.max(logits, axis=-1, keepdims=True)
    p = jnp.exp(logits - m)
    l = jnp.sum(p, axis=-1, keepdims=True)
    return p / l, (m + jnp.log(l))[..., 0]


def dilated_group_prompt(q, k, v, window, dil, bias_tab):
    n_b, s, h, e = q.shape
    blk = window // dil
    sub_len = -(-s // (dil * blk)) * blk
    n_blk = sub_len // blk
    pad = sub_len * dil - s

    def strided(t):
        t = jnp.pad(t, ((0, 0), (0, pad), (0, 0), (0, 0)))
        return t.reshape(n_b, n_blk, blk, dil, h, e).transpose(0, 3, 1, 2, 4, 5)

    def with_prev(t):
        prev = jnp.pad(t, ((0, 0), (0, 0), (1, 0), (0, 0), (0, 0), (0, 0)))[:, :, :-1]
        return jnp.concatenate([prev, t], axis=3)

    qs = strided(q)
    ks = with_prev(strided(k))
    vs = with_prev(strided(v))
    steps = jnp.arange(blk)[:, None] + blk - jnp.arange(2 * blk)[None, :]
    band = (steps >= 0) & (steps <= blk)
    has_prev = (jnp.arange(2 * blk) >= blk)[None, None, :] | (jnp.arange(n_blk) > 0)[:, None, None]
    mask = band[None] & has_prev
    bias = bias_tab[t5_bucket(jnp.maximum(steps, 0) * dil)].astype(jnp.float32).transpose(2, 0, 1)
    logits = jnp.einsum('brnqhe,brnkhe->brnhqk', qs, ks).astype(jnp.float32) * SCALE + bias
    logits = jnp.where(mask[None, None, :, None], logits, NEG_INF)
    p, lse = softmax_lse(logits)
    o = jnp.einsum('brnhqk,brnkhe->brnqhe', p.astype(v.dtype), vs)
    o = o.transpose(0, 2, 3, 1, 4, 5).reshape(n_b, sub_len * dil, h, e)[:, :s]
    lse = lse.transpose(0, 2, 4, 1, 3).reshape(n_b, sub_len * dil, h)[:, :s]
    return o, lse


def dilated_group_sample(q, k, v, kv_buf, window, dil, bias_tab):
    t_new = q.shape[1]
    w_buf = kv_buf.shape[1]
    n_keys = window // dil + 1
    kv_all = jnp.concatenate([kv_buf.astype(k.dtype), jnp.stack([k, v], axis=2)], axis=1)
    j = jnp.arange(n_keys)
    idx = w_buf + jnp.arange(t_new)[:, None] - dil * j[None, :]
    kv_sel = jnp.take(kv_all, jnp.maximum(idx, 0), axis=1)
    bias = bias_tab[t5_bucket(dil * j)].astype(jnp.float32).T
    logits = jnp.einsum('nthe,ntjhe->nthj', q, kv_sel[:, :, :, 0]).astype(jnp.float32) * SCALE + bias
    logits = jnp.where((idx >= 0)[None, :, None, :], logits, NEG_INF)
    p, lse = softmax_lse(logits)
    o = jnp.einsum('nthj,ntjhe->nthe', p.astype(v.dtype), kv_sel[:, :, :, 1])
    return o, lse


def combine_groups(outs, lses):
    w = jax.nn.softmax(jnp.stack(lses, 0), axis=0)
    o = jnp.einsum('gnlh,gnlhe->nlhe', w.astype(outs[0].dtype), jnp.stack(outs, 0))
    return o.reshape(o.shape[0], o.shape[1], ATTN_OUT_WIDTH)


def attn_prompt(q, k, v, rel_bias):
    outs, lses, states = [], [], []
    for g, (window, dil) in enumerate(DIL_GROUPS):
        tab = rel_bias[:, g * HEADS_PER_GROUP:(g + 1) * HEADS_PER_GROUP]
        o, lse = dilated_group_prompt(q[:, :, g], k[:, :, g], v[:, :, g], window, dil, tab)
        keep = min(window, q.shape[1])
        outs.append(o)
        lses.append(lse)
        states.append(jnp.stack([k[:, -keep:, g], v[:, -keep:, g]], axis=2))
    return combine_groups(outs, lses), states


def attn_sample(q, k, v, kv_bufs, rel_bias):
    outs, lses, states = [], [], []
    for g, (window, dil) in enumerate(DIL_GROUPS):
        tab = rel_bias[:, g * HEADS_PER_GROUP:(g + 1) * HEADS_PER_GROUP]
        o, lse = dilated_group_sample(q[:, :, g], k[:, :, g], v[:, :, g], kv_bufs[g], window, dil, tab)
        outs.append(o)
        lses.append(lse)
        states.append(jnp.stack([k[:, :, g], v[:, :, g]], axis=2))
    return combine_groups(outs, lses), states


def multi_scale_pool(u, first_pos):
    n_b, length, _ = u.shape
    ug = u.reshape(n_b, length, N_POOL_GROUPS, POOL_GROUP_WIDTH).astype(jnp.float32)
    cs = jnp.pad(jnp.cumsum(ug, axis=1), ((0, 0), (1, 0), (0, 0), (0, 0)))
    hi = jnp.arange(length) + 1
    outs = []
    for g, w in enumerate(POOL_WINDOWS):
        lo = jnp.maximum(hi - w, 0)
        cnt = jnp.minimum(hi + first_pos, w).astype(jnp.float32)
        s = cs[:, hi, g] - cs[:, lo, g]
        outs.append(s / cnt[None, :, None] - ug[:, :, g])
    return jnp.stack(outs, axis=2)


def pool_prompt(u):
    return multi_scale_pool(u, 0), u[:, -POOL_BUF:]


def pool_sample(u, buf):
    cat = jnp.concatenate([buf.astype(u.dtype), u], axis=1)
    pooled = multi_scale_pool(cat, PAST_LEN - POOL_BUF)[:, POOL_BUF:]
    return pooled, cat[:, -POOL_BUF:]


def moe_ffn(x, w_router, b_router, w_gate_up, b_gate_up, w_down, b_down):
    n_tok = x.shape[0]
    n_asg = n_tok * TOP_K
    logits = (x @ w_router).astype(jnp.float32) + b_router.astype(jnp.float32)
    top_val, top_idx = lax.top_k(logits, TOP_K)
    gates = jax.nn.softmax(top_val, axis=-1)
    flat_e = top_idx.reshape(n_asg)
    flat_tok = jnp.repeat(jnp.arange(n_tok, dtype=jnp.int32), TOP_K)
    order = jnp.argsort(flat_e)
    se = flat_e[order]
    counts = jnp.bincount(flat_e, length=N_EXPERTS)
    start = jnp.cumsum(counts) - counts
    padded = (counts + MOE_BLOCK - 1) // MOE_BLOCK * MOE_BLOCK
    pad_end = jnp.cumsum(padded)
    pad_start = pad_end - padded
    slot = pad_start[se] + jnp.arange(n_asg) - start[se]
    n_blocks = -(-n_asg // MOE_BLOCK) + N_EXPERTS
    cap = n_blocks * MOE_BLOCK
    tok_buf = jnp.zeros((cap,), jnp.int32).at[slot].set(flat_tok[order])
    gate_buf = jnp.zeros((cap,), jnp.float32).at[slot].set(gates.reshape(n_asg)[order])
    block_expert = jnp.minimum(
        jnp.searchsorted(pad_end, jnp.arange(n_blocks) * MOE_BLOCK, side='right'), N_EXPERTS - 1)
    xb = x[tok_buf].reshape(n_blocks, MOE_BLOCK, D_MODEL)

    def expert_block(args):
        xi, e = args
        hgu = xi @ w_gate_up[e] + b_gate_up[e]
        hg, hu = jnp.split(hgu, 2, axis=-1)
        hg = jnp.minimum(hg, SWIGLU_LIMIT)
        hu = jnp.clip(hu, -SWIGLU_LIMIT, SWIGLU_LIMIT)
        act = hg * jax.nn.sigmoid(SWIGLU_ALPHA * hg) * (hu + 1)
        return act @ w_down[e] + b_down[e]

    yb = lax.map(expert_block, (xb, block_expert)).reshape(cap, D_MODEL)
    return jnp.zeros_like(x).at[tok_buf].add(yb * gate_buf[:, None].astype(yb.dtype))


def decoder_layer(x, c, attn_fn, pool_fn, w_ada, b_ada, norm_mix_g, norm_ffn_g, w_in, q_norm_g,
                  k_norm_g, w_pool_mix, pool_scale, w_up_attn, w_up_pool, w_out, w_router, b_router,
                  w_gate_up, b_gate_up, w_down, b_down):
    n_b, length, _ = x.shape
    mod = (jax.nn.silu(c) @ w_ada + b_ada)[:, None, :]
    sh1, sc1, gt1, sh2, sc2, gt2 = jnp.split(mod, N_ADA, axis=-1)
    h = rms_norm(x, norm_mix_g) * (1 + sc1) + sh1
    q, k, v, u, g_a, g_p = jnp.split(h @ w_in, list(SPLITS), axis=-1)
    shp = (n_b, length, N_DIL_GROUPS, HEADS_PER_GROUP, HEAD_DIM)
    q = rms_norm(q.reshape(shp), q_norm_g)
    k = rms_norm(k.reshape(shp), k_norm_g)
    v = v.reshape(shp)
    attn_o, attn_state = attn_fn(q, k, v)
    pooled, pool_state = pool_fn(u)
    pool_o = jnp.einsum('nlgc,gcd->nlgd', pooled.astype(x.dtype), w_pool_mix).reshape(
        n_b, length, POOL_WIDTH) * pool_scale
    merged = jax.nn.sigmoid(g_a) * (attn_o @ w_up_attn) + jax.nn.sigmoid(g_p) * (pool_o @ w_up_pool)
    x = x + gt1 * (merged @ w_out)
    h2 = rms_norm(x, norm_ffn_g) * (1 + sc2) + sh2
    ffn = moe_ffn(h2.reshape(n_b * length, D_MODEL), w_router, b_router, w_gate_up, b_gate_up,
                  w_down, b_down).reshape(n_b, length, D_MODEL)
    x = x + gt2 * ffn
    return x, attn_state, pool_state


def setup_inputs(seed: int = 0) -> dict:
    key = jax.random.key(seed)
    ks = jax.random.split(key, 27)

    def nrm(k, shape, scale):
        return jax.random.normal(k, shape, jnp.float32) * scale

    def kv_shape(window):
        return (DEPTH, DEC_BATCH, min(window, PAST_LEN), 2, HEADS_PER_GROUP, HEAD_DIM)

    L = DEPTH
    return {
        'x_prompt': nrm(ks[0], (BATCH, SEQ, D_MODEL), 1.0),
        'x_sample': nrm(ks[1], (DEC_BATCH, DEC_SEQ, D_MODEL), 1.0),
        'cache_kv_w128': nrm(ks[2], kv_shape(DIL_GROUPS[0][0]), 1.0),
        'cache_kv_w512': nrm(ks[3], kv_shape(DIL_GROUPS[1][0]), 1.0),
        'cache_kv_w2048': nrm(ks[4], kv_shape(DIL_GROUPS[2][0]), 1.0),
        'state_pool': nrm(ks[5], (L, DEC_BATCH, POOL_BUF, POOL_WIDTH), 1.0),
        'c_prompt': nrm(ks[6], (BATCH, D_MODEL), 1.0),
        'c_sample': nrm(ks[7], (DEC_BATCH, D_MODEL), 1.0),
        'w_ada': nrm(ks[8], (L, D_MODEL, N_ADA * D_MODEL), 0.25 * D_MODEL ** -0.5),
        'b_ada': nrm(ks[9], (L, N_ADA * D_MODEL), 0.01),
        'norm_mix_g': 1.0 + nrm(ks[10], (L, D_MODEL), 0.02),
        'norm_ffn_g': 1.0 + nrm(ks[11], (L, D_MODEL), 0.02),
        'w_in': nrm(ks[12], (L, D_MODEL, IN_WIDTH), D_MODEL ** -0.5),
        'q_norm_g': 1.0 + nrm(ks[13], (L, HEAD_DIM), 0.02),
        'k_norm_g': 1.0 + nrm(ks[14], (L, HEAD_DIM), 0.02),
        'rel_bias': nrm(ks[15], (NUM_BUCKETS, N_ATTN_HEADS), 0.2),
        'w_pool_mix': nrm(ks[16], (L, N_POOL_GROUPS, POOL_GROUP_WIDTH, POOL_GROUP_WIDTH),
                          POOL_GROUP_WIDTH ** -0.5),
        'pool_scale': 1.0 + nrm(ks[17], (L, POOL_WIDTH), 0.02),
        'w_up_attn': nrm(ks[18], (L, ATTN_OUT_WIDTH, D_MODEL), ATTN_OUT_WIDTH ** -0.5),
        'w_up_pool': nrm(ks[19], (L, POOL_WIDTH, D_MODEL), POOL_WIDTH ** -0.5),
        'w_out': nrm(ks[20], (L, D_MODEL, D_MODEL), D_MODEL ** -0.5),
        'w_router': nrm(ks[21], (L, D_MODEL, N_EXPERTS), D_MODEL ** -0.5),
        'b_router': nrm(ks[22], (L, N_EXPERTS), 0.01),
        'w_gate_up': nrm(ks[23], (L, N_EXPERTS, D_MODEL, 2 * D_FF), D_MODEL ** -0.5),
        'b_gate_up': nrm(ks[24], (L, N_EXPERTS, 2 * D_FF), 0.01),
        'w_down': nrm(ks[25], (L, N_EXPERTS, D_FF, D_MODEL), D_FF ** -0.5),
        'b_down': nrm(ks[26], (L, N_EXPERTS, D_MODEL), 0.01),
    }


def reference(x_prompt, x_sample, cache_kv_w128, cache_kv_w512, cache_kv_w2048, state_pool, c_prompt,
              c_sample, w_ada, b_ada, norm_mix_g, norm_ffn_g, w_in, q_norm_g, k_norm_g, rel_bias,
              w_pool_mix, pool_scale, w_up_attn, w_up_pool, w_out, w_router, b_router, w_gate_up,
              b_gate_up, w_down, b_down):
    y_prompt, y_sample = x_prompt, x_sample
    kv_p = [[] for _ in DIL_GROUPS]
    kv_s = [[] for _ in DIL_GROUPS]
    pool_p, pool_s = [], []
    for layer in range(DEPTH):
        lw = (w_ada[layer], b_ada[layer], norm_mix_g[layer], norm_ffn_g[layer], w_in[layer],
              q_norm_g[layer], k_norm_g[layer], w_pool_mix[layer], pool_scale[layer],
              w_up_attn[layer], w_up_pool[layer], w_out[layer], w_router[layer], b_router[layer],
              w_gate_up[layer], b_gate_up[layer], w_down[layer], b_down[layer])
        y_prompt, st_a, st_p = decoder_layer(
            y_prompt, c_prompt, functools.partial(attn_prompt, rel_bias=rel_bias), pool_prompt, *lw)
        bufs = (cache_kv_w128[layer], cache_kv_w512[layer], cache_kv_w2048[layer])
        y_sample, sa, sp = decoder_layer(
            y_sample, c_sample, functools.partial(attn_sample, kv_bufs=bufs, rel_bias=rel_bias),
            functools.partial(pool_sample, buf=state_pool[layer]), *lw)
        for g in range(N_DIL_GROUPS):
            kv_p[g].append(st_a[g])
            kv_s[g].append(sa[g])
        pool_p.append(st_p)
        pool_s.append(sp)
    return (y_prompt, y_sample, jnp.stack(kv_p[0]), jnp.stack(kv_p[1]), jnp.stack(kv_p[2]),
            jnp.stack(pool_p), jnp.stack(kv_s[0]), jnp.stack(kv_s[1]), jnp.stack(kv_s[2]),
            jnp.stack(pool_s))
```

```python
import os
from contextlib import ExitStack
import numpy as np
import concourse.bass as bass
import concourse.mybir as mybir
from concourse.bass_utils import run_bass_kernel_spmd

F32 = mybir.dt.float32
BF16 = mybir.dt.bfloat16
I32 = mybir.dt.int32
AF = mybir.ActivationFunctionType
ALU = mybir.AluOpType
AX = mybir.AxisListType

NCORES = 8
D = 1024
TOK = 2048
HALO = 2048
NS = 16
OWN0 = HALO
SMP0 = HALO + TOK
TCOL = HALO + TOK + NS
NTOK = TOK + NS
CAP = 512
CG = 320
SCH = ((0, 128), (128, 128), (256, 64))
NEXP = 32
NROWS = NEXP * CAP
EPS = 1e-6
DILS = (1, 4, 16)
STAGE = int(os.environ.get("MK_STAGE", "9"))
CL = int(os.environ.get("MK_CL", "99"))
CONE = int(os.environ.get("MK_ONE", "0"))
VL = int(os.environ.get("MK_VL", "9"))


class Prog:
    ENG = ("pe", "act", "dve", "pool", "sp")

    def __init__(self):
        self.q = {e: [] for e in self.ENG}
        self.lastw = {}
        self.readers = {}
        self.dmakeys = {}
        self.keymap = {}
        self.psn = 0

    def _collect(self, reads, writes):
        deps = []
        for r in reads:
            if r in self.lastw:
                deps.append(self.lastw[r])
        for w in writes:
            if w in self.lastw:
                deps.append(self.lastw[w])
            deps.extend(self.readers.get(w, ()))
        return deps

    def _commit(self, rec, reads, writes):
        for r in reads:
            self.readers.setdefault(r, []).append(rec)
        for w in writes:
            self.lastw[w] = rec
            self.readers[w] = []

    def op(self, eng, fn, reads=(), writes=(), extra=()):
        writes = list(writes) + [r for r in reads if r.startswith("ps") and r[2:3] in "01234567OL"]
        reads = [r for r in reads if not (r.startswith("ps") and r[2:3] in "01234567OL")]
        rec = {"eng": eng, "fn": fn, "deps": self._collect(reads, writes) + list(extra), "dma": None, "needed": False}
        self._commit(rec, reads, writes)
        self.q[eng].append(rec)
        return rec

    def dma(self, eng, fn, key, reads=(), writes=(), extra=()):
        if (eng, key) not in self.keymap:
            self.keymap[(eng, key)] = (eng, sum(1 for k in self.keymap if k[0] == eng))
        key = self.keymap[(eng, key)]
        cnt = self.dmakeys.get(key, 0) + 16
        self.dmakeys[key] = cnt
        rec = {"eng": eng, "fn": fn, "deps": self._collect(reads, writes) + list(extra), "dma": (key, cnt), "needed": False}
        self._commit(rec, reads, writes)
        self.q[eng].append(rec)
        return rec

    def barrier(self):
        last = []
        for e in self.ENG:
            for rec in reversed(self.q[e]):
                if rec["dma"] is None and rec["fn"] is not None:
                    last.append(rec)
                    break
        seen = {}
        for e in self.ENG:
            for rec in self.q[e]:
                if rec["dma"] is not None:
                    seen[rec["dma"][0]] = rec
        deps = last + list(seen.values())
        for e in self.ENG:
            rec = {"eng": e, "fn": None, "deps": list(deps), "dma": None, "needed": False}
            self.q[e].append(rec)
        self.lastw = {}
        self.readers = {}
        self.keymap = {}

    def emit(self, nc, final_deps):
        for e in self.ENG:
            for rec in self.q[e]:
                for d in rec["deps"]:
                    if d["dma"] is None and not (d["eng"] == "pe" and rec["eng"] == "pe"):
                        d["needed"] = True
        for e in self.ENG:
            c = 0
            for rec in self.q[e]:
                if rec["dma"] is None and rec["needed"]:
                    c += 1
                    rec["cnt"] = c
        keys = sorted(self.dmakeys.keys(), key=str)
        with ExitStack() as st:
            esem = {e: st.enter_context(nc.semaphore("es_" + e)) for e in self.ENG}
            dsem = {k: st.enter_context(nc.semaphore("ds%d" % i)) for i, k in enumerate(keys)}
            block = st.enter_context(nc.Block())

            def run(e, E):
                waited = {}
                recs = list(self.q[e])
                if e == "sp":
                    recs.append({"eng": "sp", "fn": None, "deps": list(final_deps), "dma": None, "needed": False})
                for rec in recs:
                    for d in rec["deps"]:
                        if d["dma"] is not None:
                            k = ("d", d["dma"][0])
                            sem, val = dsem[d["dma"][0]], d["dma"][1]
                        else:
                            if d["eng"] == "pe" and e == "pe":
                                continue
                            k = ("c", d["eng"])
                            sem, val = esem[d["eng"]], d["cnt"]
                        if waited.get(k, 0) >= val:
                            continue
                        E.wait_ge(sem, val)
                        waited[k] = val
                    if rec["fn"] is None:
                        continue
                    ins = rec["fn"](E)
                    if rec["dma"] is not None:
                        ins.then_inc(dsem[rec["dma"][0]], 16)
                    elif rec["needed"]:
                        ins.then_inc(esem[e], 1)

            @block.tensor
            def _(E):
                run("pe", E)

            @block.scalar
            def _(E):
                run("act", E)

            @block.vector
            def _(E):
                run("dve", E)

            @block.gpsimd
            def _(E):
                run("pool", E)

            @block.sync
            def _(E):
                run("sp", E)


def MM(out, lhsT, rhs, start=True, stop=True):
    return lambda E: E.matmul(out, lhsT, rhs, start=start, stop=stop)


def TR(out, in_, ident):
    return lambda E: E.transpose(out, in_, ident)


def ACTV(out, in_, func, **kw):
    return lambda E: E.activation(out=out, in_=in_, func=func, **kw)


def TT(out, in0, in1, op):
    return lambda E: E.tensor_tensor(out=out, in0=in0, in1=in1, op=op)


def TS(out, in0, s1, s2, op0, op1=None, **kw):
    if op1 is None:
        return lambda E: E.tensor_scalar(out=out, in0=in0, scalar1=s1, scalar2=None, op0=op0, **kw)
    return lambda E: E.tensor_scalar(out=out, in0=in0, scalar1=s1, scalar2=s2, op0=op0, op1=op1, **kw)


def STT(out, in0, scalar, in1, op0, op1):
    return lambda E: E.scalar_tensor_tensor(out=out, in0=in0, scalar=scalar, in1=in1, op0=op0, op1=op1)


def CP(out, in_):
    return lambda E: E.tensor_copy(out=out, in_=in_)


def RCP(out, in_):
    return lambda E: E.reciprocal(out=out, in_=in_)


def RCPF(out, in_):
    return lambda E: E.reciprocal_approx_fast(out=out, in_=in_)


def MSET(ap, v):
    return lambda E: E.memset(ap, v)


def DMA(out, in_, **kw):
    return lambda E: E.dma_start(out=out, in_=in_, **kw)


def t5_bucket_np(d):
    d = np.asarray(d, np.int64)
    ratio = np.log(np.maximum(d, 1).astype(np.float32) / np.float32(16)) / np.float32(np.log(2048 / 16))
    large = np.minimum(16 + (ratio.astype(np.float32) * np.float32(16)).astype(np.int32), 31)
    return np.where(d < 16, d, large)


def static_consts(core):
    c = {}
    c["ident"] = np.eye(128, dtype=np.float32)
    c["jflip"] = np.eye(128, dtype=np.float32)[::-1].copy()
    bo = np.zeros((128, 128), np.float32)
    bo[:64, :64] = 1.0 / 64
    bo[64:, 64:] = 1.0 / 64
    c["blockones"] = bo
    us = np.triu(np.ones((128, 128), np.float32), 1)
    c["ustrict"] = us
    ohc = np.zeros((3, 32, 256), np.float32)
    ohp = np.zeros((3, 32, 256), np.float32)
    val = np.zeros((2, 8, 256), np.float32)
    for g, dil in enumerate(DILS):
        for m in range(255):
            u = m - 127
            if u >= 0:
                ohc[g, t5_bucket_np(u * dil), m] = 1.0
            if u <= 0:
                ohp[g, t5_bucket_np((u + 128) * dil), m] = 1.0
    for m in range(255):
        u = m - 127
        val[1, :, m] = 1.0 if u >= 0 else 0.0
        val[0, :, m] = 1.0 if u <= 0 else 0.0
    c["ohc"] = ohc
    c["ohp"] = ohp
    c["valid"] = val
    ohs = np.zeros((3, 32, 128), np.float32)
    for g, dil in enumerate(DILS):
        for i in range(128):
            ohs[g, t5_bucket_np(dil * (128 - i)), i] = 1.0
    c["ohs"] = ohs
    selbc = np.zeros((16, 16, 128), np.float32)
    for n in range(16):
        selbc[n, n, :] = 1.0
    c["selbc"] = selbc.reshape(16, 16 * 128)
    selcol = np.zeros((128, 16, 16), np.float32)
    for n in range(16):
        selcol[:, n, n] = 1.0
    c["selcol"] = selcol.reshape(128, 256)
    c["hp"] = np.full((128, 1), 0.0 if core == 0 else 1.0, np.float32)
    corr = np.ones((128, 4, 16), np.float32)
    for gi, w in enumerate((2, 4, 8, 16)):
        for t in range(16):
            cnt = min(t + 1, w) if core == 0 else w
            corr[:, gi, t] = 1.0 / cnt
    c["poolcorr"] = corr.reshape(128, 64)
    c["iota32"] = np.tile(np.arange(32, dtype=np.float32)[None, :] * CAP, (128, 1))
    c["iotac"] = np.tile(np.arange(CG, dtype=np.float32)[None, :], (128, 1))
    c["iotaps"] = (np.arange(128, dtype=np.float32)[:, None] + 128.0 * np.arange(3, dtype=np.float32)[None, :])
    return c


CONST_SHAPES = {
    "ident": (128, 128), "jflip": (128, 128), "blockones": (128, 128), "ustrict": (128, 128),
    "ohc": (3, 32, 256), "ohp": (3, 32, 256), "valid": (2, 8, 256), "ohs": (3, 32, 128),
    "selbc": (16, 2048), "selcol": (128, 256), "hp": (128, 1), "poolcorr": (128, 64), "iota32": (128, 32), "iotac": (128, CG), "iotaps": (128, 3),
}

IN_SHAPES = {
    "xh": (HALO + TOK, D), "xs": (NS, D), "cp": (1, D), "cs": (NS, D),
    "kv1": (NS, 128, 1024), "kv2": (NS, 128, 1024), "kv3": (NS, 128, 1024), "spool": (NS, 15, 512),
    "w_ada": (D, 6 * D), "b_ada": (1, 6 * D), "g1": (1, D), "g2": (1, D), "w_in": (D, 7168),
    "qg": (1, 64), "kg": (1, 64), "rel_bias": (32, 24), "w_pool_mix": (4, 128, 128), "pool_scale": (1, 512),
    "w_up_attn": (512, D), "w_up_pool": (512, D), "w_out": (D, D), "w_router": (D, 32), "b_router": (1, 32),
    "w_gate_up": (NEXP, D, 2 * D), "b_gate_up": (NEXP, 2 * D), "w_down": (NEXP, D, D), "b_down": (NEXP, D),
}
if STAGE < 5:
    for _k in ("w_gate_up", "b_gate_up", "w_down", "b_down"):
        IN_SHAPES.pop(_k)
OUT_SHAPES = {
    "y": (NTOK, D), "kvp1": (128, 1024), "kvp2": (512, 1024), "kvp3": (2048, 1024), "poolp": (16, 512),
    "kvs": (NS, 3, 1024), "pools": (NS, 15, 512),
}


def build():
    nc = bass.Bass("TRN2", target_bir_lowering=False)
    P = Prog()
    I = {k: nc.dram_tensor(k, list(s), F32, kind="ExternalInput").ap() for k, s in IN_SHAPES.items()}
    C = {k: nc.dram_tensor("c_" + k, list(s), F32, kind="ExternalInput").ap() for k, s in CONST_SHAPES.items()}
    O = {k: nc.dram_tensor(k, list(s), F32, kind="ExternalOutput").ap() for k, s in OUT_SHAPES.items()}

    def scr(name, shape, dt=F32):
        return nc.dram_tensor(name, list(shape), dt, kind="Internal").ap()

    modd = scr("modd", (17, 6, D))
    evd = scr("evd", (3, 2, 8, 256))
    x1d = scr("x1d", (NTOK, D))
    xsd = scr("xsd", (NROWS, D), BF16)
    ysd = scr("ysd", (NROWS, D))
    posd = scr("posd", (NTOK, 32))
    gd = scr("gd", (NTOK, 32))
    posTd = scr("posTd", (32, NTOK))
    h2d = scr("h2d", (NTOK, D), BF16)
    finals = []
    DBGO = {}

    def dbg(name, ap_sb, shape, dt, reads):
        if not os.environ.get("MK_DBG"):
            return
        t = nc.dram_tensor("dbg_" + name, list(shape), dt, kind="ExternalOutput").ap()
        DBGO[name] = t
        finals.append(P.dma("sp", DMA(t, ap_sb), "dbg_" + name, reads=reads, writes=["dbg_" + name]))

    with ExitStack() as top:
        def sbt(st, name, shape, dt=F32):
            return st.enter_context(nc.sbuf_tensor(name, list(shape), dt))

        PS = [top.enter_context(nc.psum_tensor("ps%d" % i, [128, 512], F32)) for i in range(8)]

        P.psmod = 8

        def ps():
            i = P.psn % P.psmod
            P.psn += 1
            return PS[i], "ps%d" % i

        ident = sbt(top, "ident", (128, 128))
        jflip = sbt(top, "jflip", (128, 128))
        ones_f = sbt(top, "ones_f", (128, 128))
        zo_f = sbt(top, "zo_f", (1, 128))
        ones_b = sbt(top, "ones_b", (128, 128), BF16)
        bones_b = sbt(top, "bones_b", (128, 128), BF16)
        ustr_b = sbt(top, "ustr_b", (128, 128), BF16)
        epst = sbt(top, "epst", (128, 1))
        hpt = sbt(top, "hpt", (128, 1))
        gq = sbt(top, "gq", (128, 1))
        gk = sbt(top, "gk", (128, 1))
        ident_b = sbt(top, "ident_b", (128, 128), BF16)
        hlast = sbt(top, "hlast", (128, 8, 16), BF16)
        P.dma("pool", DMA(ident_b[:], C["ident"]), "ident_b", writes=["ident_b"])
        P.dma("sp", DMA(ident[:], C["ident"]), "ident", writes=["ident"])
        P.dma("sp", DMA(jflip[:], C["jflip"]), "jflip", writes=["jflip"])
        P.dma("sp", DMA(hpt[:], C["hp"]), "hpt", writes=["hpt"])
        P.dma("pool", DMA(bones_b[:], C["blockones"]), "bones_b", writes=["bones_b"])
        P.dma("pool", DMA(ustr_b[:], C["ustrict"]), "ustr_b", writes=["ustr_b"])
        P.op("dve", MSET(ones_f[:], 1.0), writes=["ones_f"])
        P.op("dve", MSET(zo_f[:, 0:64], 0.0), writes=["zo_f"])
        P.op("dve", MSET(zo_f[:, 64:128], 1.0), writes=["zo_f"])
        P.op("dve", MSET(ones_b[:], 1.0), writes=["ones_b"])
        P.op("dve", MSET(epst[:], EPS), writes=["epst"])
        for (t, src, nm) in ((gq, I["qg"], "gq"), (gk, I["kg"], "gk")):
            srcT = src.rearrange("o e -> e o")
            with nc.allow_non_contiguous_dma(reason="tiny gain vector"):
                P.dma("sp", DMA(t[0:64, :], srcT), nm, writes=[nm])
                P.dma("sp", DMA(t[64:128, :], srcT), nm, writes=[nm])
        P.op("dve", TS(gq[:], gq[:], 0.125, None, ALU.mult), reads=["gq"], writes=["gq"])

        with ExitStack() as ph:
            csb = sbt(ph, "csb", (16, D))
            cpb = sbt(ph, "cpb", (1, D))
            scT = sbt(ph, "scT", (128, 8, 17), BF16)
            modp = sbt(ph, "modp", (1, 6 * D))
            mods = sbt(ph, "mods", (16, 6 * D))
            badas = sbt(ph, "badas", (16, 6 * D))
            g1s = sbt(ph, "g1s", (16, 2, D))
            wad = [sbt(ph, "wad%d" % i, (128, 8, 1024), BF16) for i in range(2)]
            P.dma("sp", DMA(csb[:], I["cs"]), "csb", writes=["csb"])
            P.dma("sp", DMA(cpb[:], I["cp"]), "cpb", writes=["cpb"])
            P.dma("sp", DMA(badas[:], I["b_ada"].partition_broadcast(16)), "badas", writes=["badas"])
            P.dma("sp", DMA(g1s[:, 0, :], I["g1"].partition_broadcast(16)), "g1s", writes=["g1s"])
            P.dma("sp", DMA(g1s[:, 1, :], I["g2"].partition_broadcast(16)), "g1s", writes=["g1s"])
            P.op("act", ACTV(csb[:], csb[:], AF.Silu), reads=["csb"], writes=["csb"])
            P.op("act", ACTV(cpb[:], cpb[:], AF.Silu), reads=["cpb"], writes=["cpb"])
            pt, pn = ps()
            for kc in range(8):
                P.op("pe", TR(pt[:, kc * 17:kc * 17 + 1], cpb[0:1, kc * 128:(kc + 1) * 128], ident[0:1, 0:1]),
                     reads=["cpb", "ident"], writes=[pn])
                P.op("pe", TR(pt[:, kc * 17 + 1:kc * 17 + 17], csb[0:16, kc * 128:(kc + 1) * 128], ident[0:16, 0:16]),
                     reads=["csb", "ident"], writes=[pn])
            P.op("dve", CP(scT[:].rearrange("p k c -> p (k c)"), pt[:, 0:136]), reads=[pn], writes=["scT"])
            for blk in range(6):
                w = wad[blk % 2]
                wn = "wad%d" % (blk % 2)
                P.dma("pool", DMA(w[:], I["w_ada"][:, blk * 1024:(blk + 1) * 1024].rearrange("(k p) c -> p k c", p=128)),
                      wn, writes=[wn])
                for half in range(2):
                    c0 = blk * 1024 + half * 512
                    pa, pan = ps()
                    pb, pbn = ps()
                    for kc in range(8):
                        P.op("pe", MM(pa[0:1, :], scT[:, kc, 0:1], w[:, kc, half * 512:(half + 1) * 512], kc == 0, kc == 7),
                             reads=["scT", wn], writes=[pan])
                    for kc in range(8):
                        P.op("pe", MM(pb[0:16, :], scT[:, kc, 1:17], w[:, kc, half * 512:(half + 1) * 512], kc == 0, kc == 7),
                             reads=["scT", wn], writes=[pbn])
                    P.op("dve", TT(modp[:, c0:c0 + 512], pa[0:1, :], badas[0:1, c0:c0 + 512], ALU.add),
                         reads=[pan, "badas"], writes=["modp"])
                    P.op("dve", TT(mods[:, c0:c0 + 512], pb[0:16, :], badas[:, c0:c0 + 512], ALU.add),
                         reads=[pbn, "badas"], writes=["mods"])
            for (m, npart, eng) in ((modp, 1, "dve"), (mods, 16, "dve")):
                mn = "modp" if npart == 1 else "mods"
                for (sc, gi) in ((1, 0), (4, 1)):
                    P.op(eng, STT(m[0:npart, sc * D:(sc + 1) * D], m[0:npart, sc * D:(sc + 1) * D], 1.0, g1s[0:npart, gi, :],
                                  ALU.add, ALU.mult), reads=[mn, "g1s"], writes=[mn])
            P.dma("sp", DMA(modd[0:1].rearrange("o j d -> o (j d)"), modp[:]), "modd", reads=["modp"], writes=["modd"])
            P.dma("sp", DMA(modd[1:17].rearrange("o j d -> o (j d)"), mods[:]), "modd", reads=["mods"], writes=["modd"])
        P.barrier()
        MSH1, MA1, MGT1, MSH2, MA2, MGT2 = range(6)

        def load_mod(eng, dst, dname, j, sample, key):
            if sample:
                return P.dma(eng, DMA(dst[0:16, :], modd[1:17, j, :]), key, reads=["modd"], writes=[dname])
            return P.dma(eng, DMA(dst[:, :], modd[0, j:j + 1, :].partition_broadcast(128)), key, reads=["modd"], writes=[dname])

        sc_o = top.enter_context(ExitStack())
        aoT = sbt(sc_o, "aoT", (128, 4, NTOK), BF16)
        hTo = sbt(sc_o, "hTo", (128, 8, NTOK), BF16)
        sc_h = sc_o.enter_context(ExitStack())
        hTh = sbt(sc_h, "hTh", (128, 8, HALO), BF16)

        def hcols(c0, n, step=1):
            if c0 < OWN0:
                assert c0 + (n - 1) * step < OWN0
                return hTh, "hTh", c0
            return hTo, "hTo", c0 - OWN0

        def norm_tile(xt, xn, npart, a_t, a_n, b_t, b_n, out_t, out_n, tmp, tmpn, ssq, rs, tag):
            P.op("act", ACTV(tmp[0:npart, :], xt[0:npart, :], AF.Square, accum_out=ssq[0:npart, 0:1]),
                 reads=[xn], writes=[tmpn, tag + "ssq"])
            P.op("dve", TS(rs[0:npart, 0:1], ssq[0:npart, 0:1], 1.0 / D, EPS, ALU.mult, ALU.add),
                 reads=[tag + "ssq"], writes=[tag + "rs"])
            P.op("act", ACTV(rs[0:npart, 0:1], rs[0:npart, 0:1], AF.Sqrt), reads=[tag + "rs"], writes=[tag + "rs"])
            P.op("dve", RCP(rs[0:npart, 0:1], rs[0:npart, 0:1]), reads=[tag + "rs"], writes=[tag + "rs"])
            P.op("dve", STT(out_t[0:npart, :], xt[0:npart, :], rs[0:npart, 0:1], a_t[0:npart, :], ALU.mult, ALU.mult),
                 reads=[xn, tag + "rs", a_n], writes=[out_n])
            P.op("dve", TT(out_t[0:npart, :], out_t[0:npart, :], b_t[0:npart, :], ALU.add), reads=[out_n, b_n], writes=[out_n])

        with ExitStack() as ph:
            a1b = sbt(ph, "a1b", (128, D))
            b1b = sbt(ph, "b1b", (128, D))
            a1s = sbt(ph, "a1s", (16, D))
            b1s = sbt(ph, "b1s", (16, D))
            xts = [sbt(ph, "xt%d" % i, (128, D)) for i in range(3)]
            hts = [sbt(ph, "ht%d" % i, (128, D)) for i in range(2)]
            junk = sbt(ph, "junk", (128, D), BF16)
            ssq = sbt(ph, "ssq", (128, 1))
            rs = sbt(ph, "rs", (128, 1))
            load_mod("sp", a1b, "a1b", MA1, False, "a1b")
            load_mod("sp", b1b, "b1b", MSH1, False, "b1b")
            load_mod("sp", a1s, "a1s", MA1, True, "a1s")
            load_mod("sp", b1s, "b1s", MSH1, True, "b1s")
            def b_stage_a(ti):
                smp = ti == 32
                npart = 16 if smp else 128
                xt = xts[ti % 3]
                xn = "xt%d" % (ti % 3)
                ht = hts[ti % 2]
                hn = "ht%d" % (ti % 2)
                src = I["xs"] if smp else I["xh"][ti * 128:(ti + 1) * 128, :]
                P.dma("sp", DMA(xt[0:npart, :], src), xn, writes=[xn])
                norm_tile(xt, xn, npart, a1s if smp else a1b, "a1s" if smp else "a1b", b1s if smp else b1b,
                          "b1s" if smp else "b1b", ht, hn, junk, "junk", ssq, rs, "B")

            def b_stage_b(ti):
                smp = ti == 32
                npart = 16 if smp else 128
                xt = xts[ti % 3]
                xn = "xt%d" % (ti % 3)
                ht = hts[ti % 2]
                hn = "ht%d" % (ti % 2)
                p0, p0n = ps()
                p1, p1n = ps()
                for kc in range(8):
                    pp, ppn = (p0, p0n) if kc < 4 else (p1, p1n)
                    P.op("pe", TR(pp[:, (kc % 4) * 128:(kc % 4) * 128 + npart], ht[0:npart, kc * 128:(kc + 1) * 128],
                                  ident[0:npart, 0:npart]), reads=[hn, "ident"], writes=[ppn])
                c0 = SMP0 if smp else ti * 128
                hb, hbn, cc = hcols(c0, npart)
                for half, (pp, ppn) in enumerate(((p0, p0n), (p1, p1n))):
                    P.op("act", ACTV(hb[:, half * 4:(half + 1) * 4, cc:cc + npart],
                                     pp[:].rearrange("p (k t) -> p k t", t=128)[:, :, 0:npart], AF.Copy),
                         reads=[ppn], writes=[hbn])

            for tt_ in range(34):
                if tt_ < 33:
                    b_stage_a(tt_)
                if tt_ >= 1:
                    b_stage_b(tt_ - 1)
        P.op("pool", CP(hlast[:], hTh[:, :, HALO - 16:HALO]), reads=["hTh"], writes=["hlast"])
        P.barrier()

        if STAGE >= 2:
          with ExitStack() as ph:
            zs = sbt(ph, "zs", (16, 4608))
            sqs = sbt(ph, "sqs", (16, 3072))
            wsb = [sbt(ph, "wsb%d" % i, (128, 8, 512), BF16) for i in range(2)]
            kvt = [sbt(ph, "kvt%d" % i, (128, 1024)) for i in range(3)]
            prod = [sbt(ph, "prod%d" % i, (128, 512)) for i in range(2)]
            pvb = [sbt(ph, "pvb%d" % i, (128, 520), BF16) for i in range(2)]
            lgs = [sbt(ph, "lgs%d" % i, (128, 8)) for i in range(2)]
            ems = [sbt(ph, "ems%d" % i, (128, 8)) for i in range(2)]
            selbc = sbt(ph, "selbc", (16, 2048))
            selcol = sbt(ph, "selcol", (128, 256), BF16)
            ohs = sbt(ph, "ohs", (32, 3, 128))
            relb = sbt(ph, "relb", (32, 24))
            expbS = sbt(ph, "expbS", (128, 24))
            expb0 = sbt(ph, "expb0", (16, 24))
            g16 = sbt(ph, "g16", (16, 2, 64))
            mss = sbt(ph, "mss", (16, 48))
            l0 = sbt(ph, "l0", (16, 24))
            osum = sbt(ph, "osum", (16, 512))
            lsum = sbt(ph, "lsum", (16, 8))
            tmp5 = sbt(ph, "tmp5", (16, 512))
            P.dma("sp", DMA(selbc[:], C["selbc"]), "selbc", writes=["selbc"])
            P.dma("pool", DMA(selcol[:], C["selcol"]), "selcol", writes=["selcol"])
            P.dma("sp", DMA(ohs[:], C["ohs"].rearrange("g b i -> b g i")), "ohs", writes=["ohs"])
            P.dma("sp", DMA(relb[:], I["rel_bias"]), "relb", writes=["relb"])
            P.dma("sp", DMA(expb0[:], I["rel_bias"][0:1, :].partition_broadcast(16)), "expb0", writes=["expb0"])
            P.dma("sp", DMA(g16[:, 0, :], I["qg"].partition_broadcast(16)), "g16", writes=["g16"])
            P.dma("sp", DMA(g16[:, 1, :], I["kg"].partition_broadcast(16)), "g16", writes=["g16"])
            P.op("dve", TS(g16[:, 0, :], g16[:, 0, :], 0.125, None, ALU.mult), reads=["g16"], writes=["g16"])
            P.op("act", ACTV(expb0[:], expb0[:], AF.Exp), reads=["expb0"], writes=["expb0"])
            pt, pn = ps()
            for g in range(3):
                P.op("pe", MM(pt[:, g * 8:(g + 1) * 8], ohs[:, g, :], relb[:, g * 8:(g + 1) * 8]), reads=["ohs", "relb"], writes=[pn])
            P.op("act", ACTV(expbS[:], pt[:, 0:24], AF.Exp), reads=[pn], writes=["expbS"])
            for cb in range(9):
                w = wsb[cb % 2]
                wn = "wsb%d" % (cb % 2)
                P.dma("pool", DMA(w[:], I["w_in"][:, cb * 512:(cb + 1) * 512].rearrange("(k p) c -> p k c", p=128)), wn, writes=[wn])
                pt, pn = ps()
                for kc in range(8):
                    P.op("pe", MM(pt[0:16, :], hTo[:, kc, TOK:TOK + 16], w[:, kc, :], kc == 0, kc == 7), reads=["hTo", wn], writes=[pn])
                P.op("act", ACTV(zs[:, cb * 512:(cb + 1) * 512], pt[0:16, :], AF.Copy), reads=[pn], writes=["zs"])
            P.op("dve", TT(sqs[:], zs[:, 0:3072], zs[:, 0:3072], ALU.mult), reads=["zs"], writes=["sqs"])
            P.op("dve", lambda E: E.tensor_reduce(out=mss[:], in_=sqs[:].rearrange("p (h e) -> p h e", e=64), axis=AX.X, op=ALU.add),
                 reads=["sqs"], writes=["mss"])
            P.op("dve", TS(mss[:], mss[:], 1.0 / 64, EPS, ALU.mult, ALU.add), reads=["mss"], writes=["mss"])
            P.op("act", ACTV(mss[:], mss[:], AF.Sqrt), reads=["mss"], writes=["mss"])
            P.op("dve", RCP(mss[:], mss[:]), reads=["mss"], writes=["mss"])
            zqk = zs[:, 0:3072].rearrange("p (h e) -> p h e", e=64)
            P.op("dve", TT(zqk, zqk, mss[:].unsqueeze(2).to_broadcast([16, 48, 64]), ALU.mult), reads=["zs", "mss"], writes=["zs"])
            for qk in range(2):
                zz = zs[:, qk * 1536:(qk + 1) * 1536].rearrange("p (h e) -> p h e", e=64)
                P.op("dve", TT(zz, zz, g16[:, qk, :].unsqueeze(1).to_broadcast([16, 24, 64]), ALU.mult), reads=["zs", "g16"], writes=["zs"])
            finals.append(P.dma("sp", DMA(O["kvs"][:, :, 0:512], zs[:, 1536:3072].rearrange("p (g c) -> p g c", g=3)), "kvs_k", reads=["zs"], writes=["o_kvs_k"]))
            finals.append(P.dma("sp", DMA(O["kvs"][:, :, 512:1024], zs[:, 3072:4608].rearrange("p (g c) -> p g c", g=3)), "kvs_v", reads=["zs"], writes=["o_kvs_v"]))
            psO, psL = PS[6], PS[7]
            P.psmod = 6
            caches = (I["kv1"], I["kv2"], I["kv3"])
            sits = [(n, g) for n in range(16) for g in range(3)]
            SD = 2
            sst = {}
            for it2 in range(len(sits) + SD):
                if it2 < len(sits):
                    n, g = sits[it2]
                    kt = kvt[it2 % 3]
                    ktn = "kvt%d" % (it2 % 3)
                    P.dma("sp", DMA(kt[:], caches[g][n]), ktn, writes=[ktn])
                    pt, pn = ps()
                    P.op("pe", MM(pt[:, :], selbc[0:16, n * 128:(n + 1) * 128], zs[0:16, g * 512:(g + 1) * 512]), reads=["selbc", "zs"], writes=[pn])
                    sst[it2] = (pt, pn)
                it = it2 - SD
                if it < 0:
                    continue
                n, g = sits[it]
                pt, pn = sst.pop(it)
                kt = kvt[it % 3]
                ktn = "kvt%d" % (it % 3)
                pr = prod[it % 2]
                prn = "prod%d" % (it % 2)
                pv = pvb[it % 2]
                pvn = "pvb%d" % (it % 2)
                lg = lgs[it % 2]
                lgn = "lgs%d" % (it % 2)
                em = ems[it % 2]
                emn = "ems%d" % (it % 2)
                P.op("dve", TT(pr[:], kt[:, 0:512], pt[:, :], ALU.mult), reads=[ktn, pn], writes=[prn])
                P.op("dve", lambda E, lg=lg, pr=pr: E.tensor_reduce(out=lg[:], in_=pr[:].rearrange("p (h e) -> p h e", e=64), axis=AX.X, op=ALU.add),
                     reads=[prn], writes=[lgn])
                P.op("act", ACTV(lg[:], lg[:], AF.Exp), reads=[lgn], writes=[lgn])
                P.op("dve", TT(em[:], lg[:], expbS[:, g * 8:(g + 1) * 8], ALU.mult), reads=[lgn, "expbS"], writes=[emn])
                P.op("act", ACTV(pv[:, 512:520], em[:], AF.Copy), reads=[emn], writes=[pvn])
                P.op("dve", TT(pv[:, 0:512].rearrange("p (h e) -> p h e", e=64), kt[:, 512:1024].rearrange("p (h e) -> p h e", e=64),
                               em[:].unsqueeze(2).to_broadcast([128, 8, 64]), ALU.mult), reads=[ktn, emn], writes=[pvn])
                first, last = it == 0, it == 47
                P.op("pe", MM(psO[0:16, :], selcol[:, n * 16:(n + 1) * 16], pv[:, 0:512], first, last), reads=["selcol", pvn], writes=["psO"])
                P.op("pe", MM(psL[0:16, 0:8], selcol[:, n * 16:(n + 1) * 16], pv[:, 512:520], first, last), reads=["selcol", pvn], writes=["psL"])
            qv = zs[:, 0:1536].rearrange("p (h e) -> p h e", e=64)
            kv_ = zs[:, 1536:3072].rearrange("p (h e) -> p h e", e=64)
            sq3 = sqs[:, 0:1536].rearrange("p (h e) -> p h e", e=64)
            P.op("dve", TT(sq3, qv, kv_, ALU.mult), reads=["zs"], writes=["sqs"])
            P.op("dve", lambda E: E.tensor_reduce(out=l0[:], in_=sq3, axis=AX.X, op=ALU.add), reads=["sqs"], writes=["l0"])
            P.op("act", ACTV(l0[:], l0[:], AF.Exp), reads=["l0"], writes=["l0"])
            P.op("dve", TT(l0[:], l0[:], expb0[:], ALU.mult), reads=["l0", "expb0"], writes=["l0"])
            P.op("dve", CP(osum[:], psO[0:16, :]), reads=["psO"], writes=["osum"])
            P.op("dve", CP(lsum[:], psL[0:16, 0:8]), reads=["psL"], writes=["lsum"])
            for g in range(3):
                vg = zs[:, 3072 + g * 512:3072 + (g + 1) * 512].rearrange("p (h e) -> p h e", e=64)
                e0 = l0[:, g * 8:(g + 1) * 8]
                P.op("dve", TT(tmp5[:].rearrange("p (h e) -> p h e", e=64), vg, e0.unsqueeze(2).to_broadcast([16, 8, 64]), ALU.mult),
                     reads=["zs", "l0"], writes=["tmp5"])
                P.op("dve", TT(osum[:], osum[:], tmp5[:], ALU.add), reads=["tmp5", "osum"], writes=["osum"])
                P.op("dve", TT(lsum[:], lsum[:], e0, ALU.add), reads=["l0", "lsum"], writes=["lsum"])
            P.op("dve", RCP(lsum[:], lsum[:]), reads=["lsum"], writes=["lsum"])
            o3 = osum[:].rearrange("p (h e) -> p h e", e=64)
            P.op("dve", TT(o3, o3, lsum[:].unsqueeze(2).to_broadcast([16, 8, 64]), ALU.mult), reads=["osum", "lsum"], writes=["osum"])
            pt, pn = ps()
            for c in range(4):
                P.op("pe", TR(pt[:, c * 16:(c + 1) * 16], osum[0:16, c * 128:(c + 1) * 128], ident[0:16, 0:16]), reads=["osum", "ident"], writes=[pn])
            P.op("act", ACTV(aoT[:, :, TOK:TOK + 16], pt[:, 0:64].rearrange("p (c t) -> p c t", t=16), AF.Copy), reads=[pn], writes=["aoT_s"])
            P.psmod = 8
          P.barrier()

        if STAGE >= 3:
          with ExitStack() as ph0:
            relb = sbt(ph0, "relb2", (32, 24))
            ohcp = sbt(ph0, "ohcp", (32, 6, 256))
            valt = sbt(ph0, "valt", (8, 2, 256))
            evs = sbt(ph0, "evs", (8, 6, 256))
            P.dma("sp", DMA(relb[:], I["rel_bias"]), "relb2", writes=["relb2"])
            P.dma("sp", DMA(ohcp[:, 0:3, :], C["ohp"].rearrange("g b m -> b g m")), "ohcp", writes=["ohcp"])
            P.dma("sp", DMA(ohcp[:, 3:6, :], C["ohc"].rearrange("g b m -> b g m")), "ohcp", writes=["ohcp"])
            P.dma("sp", DMA(valt[:], C["valid"].rearrange("c h m -> h c m")), "valt", writes=["valt"])
            for g in range(3):
                for cp_ in range(2):
                    pt, pn = ps()
                    P.op("pe", MM(pt[0:8, 0:256], relb[:, g * 8:(g + 1) * 8], ohcp[:, cp_ * 3 + g, :]), reads=["relb2", "ohcp"], writes=[pn])
                    P.op("act", ACTV(evs[:, g * 2 + cp_, :], pt[0:8, 0:256], AF.Exp), reads=[pn], writes=["evs"])
                    P.op("dve", TT(evs[:, g * 2 + cp_, :], evs[:, g * 2 + cp_, :], valt[:, cp_, :], ALU.mult), reads=["evs", "valt"], writes=["evs"])
            P.dma("sp", DMA(evd.rearrange("g c h m -> h (g c) m"), evs[:]), "evd", reads=["evs"], writes=["evd"])
          P.barrier()
        if STAGE >= 3:
          with ExitStack() as ph:
            accE = sbt(ph, "accE", (65, 2, TOK))
            accO = sbt(ph, "accO", (128, 2, TOK))
            qT = sbt(ph, "qT", (128, 2, TOK), BF16)
            kT = sbt(ph, "kT", (128, 2, HALO + TOK), BF16)
            VbE = sbt(ph, "VbE", (128, 32, 2, 66), BF16)
            VbO = sbt(ph, "VbO", (128, 32, 2, 128), BF16)
            wq = sbt(ph, "wq", (128, 8, 256), BF16)
            wk = sbt(ph, "wk", (128, 8, 256), BF16)
            wv = sbt(ph, "wv", (128, 8, 256), BF16)
            mtab = sbt(ph, "mtab", (128, 4, 4, 128))
            Ef = [sbt(ph, "Ef%d" % i, (128, 256)) for i in range(2)]
            Eb = [sbt(ph, "Eb%d" % i, (128, 256), BF16) for i in range(2)]
            knf = [sbt(ph, "knf%d" % i, (128, 512)) for i in range(2)]
            sqb = [sbt(ph, "sqb%d" % i, (128, 512), BF16) for i in range(2)]
            rt = [sbt(ph, "rt%d" % i, (128, 512)) for i in range(2)]
            vst = [sbt(ph, "vst%d" % i, (128, 256)) for i in range(2)]
            kst = [sbt(ph, "kst%d" % i, (128, 128)) for i in range(2)]
            rl = [sbt(ph, "rl%d" % i, (128, 512)) for i in range(2)]
            P.op("pool", MSET(VbE[:, :, :, 64:65], 1.0), writes=["VbE"])
            P.op("pool", MSET(VbO[:, :, :, 0:64], 0.0), writes=["VbO"])
            P.op("pool", MSET(VbO[:, :, :, 0:1], 1.0), writes=["VbO"])
            kvouts = (O["kvp1"], O["kvp2"], O["kvp3"])
            cnt = {"p": 0, "e": 0, "s": 0, "hk": 0, "rl": 0, "pS": 0, "pO": 0, "pp": 0, "pv": 0}

            def psp():
                i_ = cnt["pp"] % 6
                cnt["pp"] += 1
                return PS[i_], "ps%d" % i_

            def psv():
                i_ = 6 + cnt["pv"] % 2
                cnt["pv"] += 1
                return PS[i_], "ps%d" % i_

            for hh in range(2 if CL >= 2 else 0):
                for g in range(3):
                    if CONE and (hh, g) != (0, CONE - 1):
                        continue
                    dil = DILS[g]
                    halo_g = 128 * dil
                    keep = halo_g
                    nb = 16 // dil
                    colq = g * 512 + hh * 256
                    for (wt, wnm, c0) in ((wq, "wq", colq), (wk, "wk", 1536 + colq), (wv, "wv", 3072 + colq)):
                        P.dma("pool", DMA(wt[:], I["w_in"][:, c0:c0 + 256].rearrange("(k p) c -> p k c", p=128)), wnm, writes=[wnm])
                    for hl in range(4):
                        for cp_ in range(2):
                            mi_ = hl * 2 + cp_
                            hkt = rl[mi_ // 4][:, (mi_ % 4) * 128:(mi_ % 4 + 1) * 128]
                            base = evd[g, cp_, hh * 4 + hl, 0:128]
                            hank = bass.AP(tensor=base.tensor, offset=base.offset, ap=[[1, 128], [1, 128]])
                            P.dma("sp", DMA(hkt, hank), "hk%d" % mi_, reads=["evd"], writes=["rl%d" % (mi_ // 4)])
                    if CL < 3:
                        continue
                    def emit_vblock(r, n):
                        bi = r * (nb + 1) + (n + 1)
                        c0 = OWN0 + n * 128 * dil + r
                        hb, hbn, cc = hcols(c0, 128, dil)
                        pv_, pvn_ = psv()
                        for kc in range(8):
                            P.op("pe", MM(pv_[:, 0:256], hb[:, kc, cc:cc + 127 * dil + 1:dil], wv[:, kc, :], kc == 0, kc == 7),
                                 reads=[hbn, "wv"], writes=[pvn_])
                        pv4 = pv_[:, 0:256].rearrange("p (h e) -> p h e", e=64)
                        if VL >= 2:
                            P.op("act", ACTV(VbE[:, bi, :, 0:64], pv4[:, 0:4:2, :], AF.Copy), reads=[pvn_], writes=["VbE"])
                        if VL >= 3:
                            P.op("act", ACTV(VbO[:, bi, :, 64:128], pv4[:, 1:4:2, :], AF.Copy), reads=[pvn_], writes=["VbO"])
                        trel = n * 128 * dil
                        if VL >= 4 and n >= 0 and trel >= TOK - keep:
                            vs_i = cnt["s"] % 2
                            cnt["s"] += 1
                            P.op("dve", CP(vst[vs_i][:], pv_[:, 0:256]), reads=[pvn_], writes=["vst%d" % vs_i])
                            r0 = trel - (TOK - keep) + r
                            dst = kvouts[g].rearrange("(j r) c -> r j c", r=dil)[r, r0 // dil:r0 // dil + 128, 512 + hh * 256:512 + (hh + 1) * 256]
                            finals.append(P.dma("sp", DMA(dst, vst[vs_i][:]), "vst%d" % vs_i, reads=["vst%d" % vs_i], writes=["o_v"]))
                    vlist = [(r, n) for r in range(dil) for n in range(-1, nb)]
                    pj = []
                    for isk in range(2):
                        c_lo = OWN0 - halo_g if isk else OWN0
                        blocks = [(cb, min(512, OWN0 + TOK - cb)) for cb in range(c_lo, OWN0 + TOK, 512)]
                        if isk and halo_g < 512:
                            blocks = [(c_lo, halo_g)] + [(cb, 512) for cb in range(OWN0, OWN0 + TOK, 512)]
                        for c in range(2):
                            for (cb, nn) in blocks:
                                pj.append((isk, c, cb, nn))
                    stp = {}
                    for i in range(len(pj) + 1):
                        if i < len(pj):
                            isk, c, cb, nn = pj[i]
                            wt, wnm = (wk, "wk") if isk else (wq, "wq")
                            hb, hbn, cc = hcols(cb, nn)
                            i2 = cnt["p"] % 2
                            cnt["p"] += 1
                            pz, pzn = psp()
                            for kc in range(8):
                                P.op("pe", MM(pz[:, 0:nn], wt[:, kc, c * 128:(c + 1) * 128], hb[:, kc, cc:cc + nn], kc == 0, kc == 7),
                                     reads=[wnm, hbn], writes=[pzn])
                            P.op("act", ACTV(sqb[i2][:, 0:nn], pz[:, 0:nn], AF.Square), reads=[pzn], writes=["sqb%d" % i2])
                            stp[i] = (pz, pzn, i2)
                            if vlist and CL >= 4:
                                emit_vblock(*vlist.pop(0))
                        j = i - 1
                        if j < 0:
                            continue
                        isk, c, cb, nn = pj[j]
                        pz, pzn, i2 = stp.pop(j)
                        gcol = gk if isk else gq
                        gn = "gk" if isk else "gq"
                        pm, pmn = psp()
                        P.op("pe", MM(pm[:, 0:nn], bones_b[:], sqb[i2][:, 0:nn]), reads=["bones_b", "sqb%d" % i2], writes=[pmn])
                        P.op("act", ACTV(rt[i2][:, 0:nn], pm[:, 0:nn], AF.Ln, bias=epst[:, 0:1]), reads=[pmn, "epst"], writes=["rt%d" % i2])
                        P.op("act", ACTV(rt[i2][:, 0:nn], rt[i2][:, 0:nn], AF.Exp, scale=-0.5), reads=["rt%d" % i2], writes=["rt%d" % i2])
                        if not isk:
                            P.op("dve", STT(qT[:, c, cb - OWN0:cb - OWN0 + nn], pz[:, 0:nn], gcol[:, 0:1], rt[i2][:, 0:nn], ALU.mult, ALU.mult),
                                 reads=[pzn, gn, "rt%d" % i2], writes=["qT"])
                        else:
                            P.op("dve", STT(knf[i2][:, 0:nn], pz[:, 0:nn], gcol[:, 0:1], rt[i2][:, 0:nn], ALU.mult, ALU.mult),
                                 reads=[pzn, gn, "rt%d" % i2], writes=["knf%d" % i2])
                            P.op("act", ACTV(kT[:, c, cb:cb + nn], knf[i2][:, 0:nn], AF.Copy), reads=["knf%d" % i2], writes=["kT"])
                            pk, pkn = None, None
                            for t0 in range(cb, cb + nn, 128):
                                trel = t0 - OWN0
                                if trel < TOK - keep or trel < 0:
                                    continue
                                ks_i = cnt["s"] % 2
                                cnt["s"] += 1
                                if pk is None:
                                    pk, pkn = psp()
                                qo = ((t0 - cb) // 128) * 128
                                P.op("pe", TR(pk[:, qo:qo + 128], knf[i2][:, t0 - cb:t0 - cb + 128], ident[:]), reads=["knf%d" % i2, "ident"], writes=[pkn])
                                P.op("act", ACTV(kst[ks_i][:], pk[:, qo:qo + 128], AF.Copy), reads=[pkn], writes=["kst%d" % ks_i])
                                r0 = trel - (TOK - keep)
                                finals.append(P.dma("sp", DMA(kvouts[g][r0:r0 + 128, hh * 256 + c * 128:hh * 256 + (c + 1) * 128], kst[ks_i][:]),
                                                    "kst%d" % ks_i, reads=["kst%d" % ks_i], writes=["o_k"]))
                    while vlist:
                        emit_vblock(*vlist.pop(0))
                    if CL < 5:
                        continue
                    for hl in range(4):
                        for cp_ in range(2):
                            mi_ = hl * 2 + cp_
                            hkt = rl[mi_ // 4][:, (mi_ % 4) * 128:(mi_ % 4 + 1) * 128]
                            pt, pn = ps()
                            P.op("pe", MM(pt[:, 0:128], jflip[:], hkt), reads=["jflip", "rl%d" % (mi_ // 4)], writes=[pn])
                            P.op("act", ACTV(mtab[:, hl, cp_, :], pt[:, 0:128], AF.Copy), reads=[pn], writes=["mtab"])
                            if cp_ == 0:
                                P.op("dve", TS(mtab[:, hl, 2, :], pt[:, 0:128], hpt[:, 0:1], None, ALU.mult), reads=[pn, "hpt"], writes=["mtab"])
                            else:
                                P.op("dve", CP(mtab[:, hl, 3, :], pt[:, 0:128]), reads=[pn], writes=["mtab"])
                    its = [(r, n, hl) for r in range(dil) for n in range(nb) for hl in range(4)]
                    DEPTH = 3
                    stg = {}
                    for i in range(len(its) + DEPTH):
                        if i < len(its):
                            r, n, hl = its[i]
                            q0 = n * 128 * dil + r
                            kc0 = OWN0 + q0
                            kp0 = kc0 - 128 * dil
                            c = hl // 2
                            pb = (hl % 2) * 64
                            pS, pSn = PS[cnt["pS"] % 5], "ps%d" % (cnt["pS"] % 5)
                            cnt["pS"] += 1
                            qsl = qT[pb:pb + 64, c, q0:q0 + 127 * dil + 1:dil]
                            P.op("pe", MM(pS[:, 0:128], kT[pb:pb + 64, c, kp0:kp0 + 127 * dil + 1:dil], qsl), reads=["kT", "qT"], writes=[pSn])
                            P.op("pe", MM(pS[:, 128:256], kT[pb:pb + 64, c, kc0:kc0 + 127 * dil + 1:dil], qsl), reads=["kT", "qT"], writes=[pSn])
                            stg[i] = (pS, pSn)
                        j = i - DEPTH
                        if j < 0:
                            continue
                        r, n, hl = its[j]
                        pS, pSn = stg.pop(j)
                        bprev = r * (nb + 1) + n
                        bcur = bprev + 1
                        q0 = n * 128 * dil + r
                        ei = cnt["e"] % 2
                        cnt["e"] += 1
                        P.op("act", ACTV(Ef[ei][:], pS[:, 0:256], AF.Exp), reads=[pSn], writes=["Ef%d" % ei])
                        mi = 2 if n == 0 else 0
                        P.op("dve", TT(Eb[ei][:].rearrange("p (a k) -> p a k", a=2), Ef[ei][:].rearrange("p (a k) -> p a k", a=2),
                                       mtab[:, hl, mi:mi + 2, :], ALU.mult), reads=["Ef%d" % ei, "mtab"], writes=["Eb%d" % ei])
                        pO, pOn = PS[5 + cnt["pO"] % 3], "ps%d" % (5 + cnt["pO"] % 3)
                        cnt["pO"] += 1
                        if hl % 2 == 0:
                            P.op("pe", MM(pO[0:65, 0:128], VbE[:, bprev, hl // 2, 0:65], Eb[ei][:, 0:128], True, False), reads=["VbE", "Eb%d" % ei], writes=[pOn])
                            P.op("pe", MM(pO[0:65, 0:128], VbE[:, bcur, hl // 2, 0:65], Eb[ei][:, 128:256], False, True), reads=["VbE", "Eb%d" % ei], writes=[pOn])
                            av = accE[0:65, hl // 2, q0:q0 + 127 * dil + 1:dil]
                            src = pO[0:65, 0:128]
                        else:
                            P.op("pe", MM(pO[:, 0:128], VbO[:, bprev, hl // 2, :], Eb[ei][:, 0:128], True, False), reads=["VbO", "Eb%d" % ei], writes=[pOn])
                            P.op("pe", MM(pO[:, 0:128], VbO[:, bcur, hl // 2, :], Eb[ei][:, 128:256], False, True), reads=["VbO", "Eb%d" % ei], writes=[pOn])
                            av = accO[:, hl // 2, q0:q0 + 127 * dil + 1:dil]
                            src = pO[:, 0:128]
                        an = "acc_%d_%d_%d_%d" % (g, hl, r, n)
                        if g == 0:
                            cnt["fence"] = P.op("dve", CP(av, src), reads=[pOn], writes=[an], extra=cnt.get("prevfence", []))
                        else:
                            cnt["fence"] = P.op("dve", TT(av, src, av, ALU.add), reads=[pOn], writes=[an], extra=cnt.get("prevfence", []))
                    cnt["prevfence"] = [cnt["fence"]]
                for hl in range(4 if CL >= 6 else 0):
                    pass
                for hl in range(4 if CL >= 6 else 0):
                    for tb in range(4):
                        ri = cnt["rl"] % 2
                        cnt["rl"] += 1
                        pt, pn = ps()
                        cs_ = slice(tb * 512, (tb + 1) * 512)
                        if hl % 2 == 0:
                            P.op("pe", MM(pt[0:64, :], ones_f[64:65, 0:64], accE[64:65, hl // 2, cs_]), reads=["ones_f", "accE"], writes=[pn], extra=cnt.get("prevfence", []))
                            P.op("act", ACTV(rl[ri][0:64, :], pt[0:64, :], AF.Ln), reads=[pn], writes=["rl%d" % ri])
                            P.op("act", ACTV(rl[ri][0:64, :], rl[ri][0:64, :], AF.Exp, scale=-1.0), reads=["rl%d" % ri], writes=["rl%d" % ri])
                            cnt["finfence"] = P.op("dve", TT(aoT[0:64, hh * 2 + hl // 2, cs_], accE[0:64, hl // 2, cs_], rl[ri][0:64, :], ALU.mult),
                                 reads=["accE", "rl%d" % ri], writes=["aoT"], extra=cnt.get("prevfence", []))
                        else:
                            P.op("pe", MM(pt[:, :], zo_f[0:1, :], accO[0:1, hl // 2, cs_]), reads=["zo_f", "accO"], writes=[pn], extra=cnt.get("prevfence", []))
                            P.op("act", ACTV(rl[ri][64:128, :], pt[64:128, :], AF.Ln), reads=[pn], writes=["rl%d" % ri])
                            P.op("act", ACTV(rl[ri][64:128, :], rl[ri][64:128, :], AF.Exp, scale=-1.0), reads=["rl%d" % ri], writes=["rl%d" % ri])
                            cnt["finfence"] = P.op("dve", TT(aoT[64:128, hh * 2 + hl // 2, cs_], accO[64:128, hl // 2, cs_], rl[ri][64:128, :], ALU.mult),
                                 reads=["accO", "rl%d" % ri], writes=["aoT"], extra=cnt.get("prevfence", []))
                if "finfence" in cnt:
                    cnt["prevfence"] = [cnt["finfence"]]
          P.barrier()
        sc_h.close()
        poT = sbt(sc_o, "poT", (128, 4, NTOK), BF16)

        BIG = 1.0e6
        TB = [(0, 512), (512, 512), (1024, 512), (1536, 512), (2048, 16)]
        if STAGE >= 4:
          with ExitStack() as ph:
            uT = sbt(ph, "uT", (128, 4, 16 + TOK))
            uTs = sbt(ph, "uTs", (128, 4, 16))
            sA = sbt(ph, "sA", (128, 16 + TOK))
            sB = sbt(ph, "sB", (128, 16 + TOK))
            pl = sbt(ph, "pl", (128, 4, NTOK), BF16)
            wu = sbt(ph, "wu", (128, 8, 512), BF16)
            wpm = sbt(ph, "wpm", (128, 4, 128), BF16)
            pscale = sbt(ph, "pscale", (128, 4))
            corr = sbt(ph, "corr", (128, 4, 16))
            tmpc = sbt(ph, "tmpc", (128, 16))
            spb = sbt(ph, "spb", (16, 26, 128))
            unew = sbt(ph, "unew", (16, 512))
            ulast = sbt(ph, "ulast", (16, 512))
            ssum = sbt(ph, "ssum", (16, 128))
            pls = sbt(ph, "pls", (16, 512))
            P.dma("pool", DMA(wu[:], I["w_in"][:, 4608:5120].rearrange("(k p) c -> p k c", p=128)), "wu", writes=["wu"])
            P.dma("pool", DMA(wpm[:], I["w_pool_mix"].rearrange("g c d -> c g d")), "wpm", writes=["wpm"])
            P.dma("sp", DMA(pscale[:], I["pool_scale"].rearrange("o (g p) -> p (o g)", p=128), allow_slow_non_contiguous=True), "pscale", writes=["pscale"])
            P.dma("sp", DMA(corr[:], C["poolcorr"].rearrange("p (g t) -> p g t", t=16)), "corr", writes=["corr"])
            r0 = 0
            sp_off = []
            for gi, w in enumerate((2, 4, 8, 16)):
                P.dma("sp", DMA(spb[:, r0:r0 + w - 1, :], I["spool"][:, 15 - (w - 1):15, gi * 128:(gi + 1) * 128]), "spb", writes=["spb"])
                sp_off.append(r0)
                r0 += w - 1
            finals.append(P.dma("sp", DMA(O["pools"][:, 0:14, :], I["spool"][:, 1:15, :]), "pools_cp", writes=["o_pools_a"]))
            for gi in range(4):
                for (c0, nn, dst) in [(OWN0 - 16, 16, uT[:, gi, 0:16])] + [(OWN0 + tb * 512, 512, uT[:, gi, 16 + tb * 512:16 + (tb + 1) * 512]) for tb in range(4)] + [(SMP0, 16, uTs[:, gi, :])]:
                    hb, hbn, cc = hcols(c0, nn) if c0 >= OWN0 else (None, None, None)
                    pt, pn = ps()
                    if c0 < OWN0:
                        hb, hbn, cc = hlast, "hlast", 0
                    for kc in range(8):
                        P.op("pe", MM(pt[:, 0:nn], wu[:, kc, gi * 128:(gi + 1) * 128], hb[:, kc, cc:cc + nn], kc == 0, kc == 7), reads=["wu", hbn], writes=[pn])
                    if c0 < OWN0:
                        P.op("dve", TS(dst, pt[:, 0:nn], hpt[:, 0:1], None, ALU.mult), reads=[pn, "hpt"], writes=["uT"])
                    else:
                        P.op("act", ACTV(dst, pt[:, 0:nn], AF.Copy), reads=[pn], writes=["uT" if c0 < SMP0 else "uTs"])
            for (srcT, sn, dstt, dn) in ((uT, "uT", ulast, "ulast"), (uTs, "uTs", unew, "unew")):
                pt, pn = ps()
                for gi in range(4):
                    sl = srcT[:, gi, 16 + TOK - 16:16 + TOK] if sn == "uT" else srcT[:, gi, :]
                    P.op("pe", TR(pt[0:16, gi * 128:(gi + 1) * 128], sl, ident[:]), reads=[sn, "ident"], writes=[pn])
                P.op("act", ACTV(dstt[:], pt[0:16, :], AF.Copy), reads=[pn], writes=[dn])
            finals.append(P.dma("sp", DMA(O["poolp"], ulast[:]), "poolp", reads=["ulast"], writes=["o_poolp"]))
            finals.append(P.dma("sp", DMA(O["pools"][:, 14, :], unew[:]), "pools_n", reads=["unew"], writes=["o_pools_b"]))
            for gi, w in enumerate((2, 4, 8, 16)):
                cur, curn = uT[:, gi, :], "uT"
                bufs = [(sA, "sA"), (sB, "sB")]
                lo = 0
                for step in range(gi + 1):
                    sh = 1 << step
                    dstb, dstn = bufs[step % 2]
                    P.op("dve", TT(dstb[:, lo + sh:16 + TOK], cur[:, lo + sh:16 + TOK], cur[:, lo:16 + TOK - sh], ALU.add),
                         reads=[curn], writes=[dstn])
                    cur, curn = dstb[:, :], dstn
                    lo += sh
                P.op("dve", STT(pl[:, gi, 0:TOK], cur[:, 16:16 + TOK], 1.0 / w, uT[:, gi, 16:16 + TOK], ALU.mult, ALU.subtract),
                     reads=[curn, "uT"], writes=["pl"])
                P.op("dve", TT(tmpc[:], cur[:, 16:32], corr[:, gi, :], ALU.mult), reads=[curn, "corr"], writes=["tmpc"])
                P.op("dve", TT(pl[:, gi, 0:16], tmpc[:], uT[:, gi, 16:32], ALU.subtract), reads=["tmpc", "uT"], writes=["pl"])
                P.op("dve", lambda E, gi=gi, w=w: E.tensor_reduce(out=ssum[:], in_=spb[:, sp_off[gi]:sp_off[gi] + w - 1, :].rearrange("p r c -> p c r"),
                                                                 axis=AX.X, op=ALU.add), reads=["spb"], writes=["ssum"])
                P.op("dve", TT(ssum[:], ssum[:], unew[:, gi * 128:(gi + 1) * 128], ALU.add), reads=["ssum", "unew"], writes=["ssum"])
                P.op("dve", STT(pls[:, gi * 128:(gi + 1) * 128], ssum[:], 1.0 / w, unew[:, gi * 128:(gi + 1) * 128], ALU.mult, ALU.subtract),
                     reads=["ssum", "unew"], writes=["pls"])
            pt, pn = ps()
            for gi in range(4):
                P.op("pe", TR(pt[:, gi * 16:(gi + 1) * 16], pls[0:16, gi * 128:(gi + 1) * 128], ident[0:16, 0:16]), reads=["pls", "ident"], writes=[pn])
            P.op("act", ACTV(pl[:, :, TOK:TOK + 16], pt[:, 0:64].rearrange("p (c t) -> p c t", t=16), AF.Copy), reads=[pn], writes=["pl"])
            for gi in range(4):
                for (c0, nn) in TB:
                    pt, pn = ps()
                    P.op("pe", MM(pt[:, 0:nn], wpm[:, gi, :], pl[:, gi, c0:c0 + nn]), reads=["wpm", "pl"], writes=[pn])
                    P.op("act", ACTV(poT[:, gi, c0:c0 + nn], pt[:, 0:nn], AF.Copy, scale=pscale[:, gi:gi + 1]), reads=[pn, "pscale"], writes=["poT"])
          P.barrier()

        if STAGE >= 4:
          mgT = sbt(sc_o, "mgT", (128, 8, NTOK), BF16)
          with ExitStack() as ph:
            wua = sbt(ph, "wua", (128, 4, D), BF16)
            wup = sbt(ph, "wup", (128, 4, D), BF16)
            wga = sbt(ph, "wga", (128, 8, D), BF16)
            wgp = sbt(ph, "wgp", (128, 8, D), BF16)
            sa = [sbt(ph, "sa%d" % i, (128, 512)) for i in range(2)]
            sp_ = [sbt(ph, "sp_%d" % i, (128, 512)) for i in range(2)]
            m1 = [sbt(ph, "m1%d" % i, (128, 512)) for i in range(2)]
            m2 = [sbt(ph, "m2%d" % i, (128, 512)) for i in range(2)]
            P.dma("pool", DMA(wua[:], I["w_up_attn"].rearrange("(k p) c -> p k c", p=128)), "wua", writes=["wua"])
            P.dma("pool", DMA(wup[:], I["w_up_pool"].rearrange("(k p) c -> p k c", p=128)), "wup", writes=["wup"])
            P.dma("pool", DMA(wga[:], I["w_in"][:, 5120:6144].rearrange("(k p) c -> p k c", p=128)), "wga", writes=["wga"])
            P.dma("pool", DMA(wgp[:], I["w_in"][:, 6144:7168].rearrange("(k p) c -> p k c", p=128)), "wgp", writes=["wgp"])
            it = 0
            for (c0, nn) in TB:
                for j in range(8):
                    i2 = it % 2
                    it += 1
                    js = slice(j * 128, (j + 1) * 128)
                    pA, pAn = ps()
                    pB, pBn = ps()
                    pGa, pGan = ps()
                    pGp, pGpn = ps()
                    for kc in range(4):
                        P.op("pe", MM(pA[:, 0:nn], wua[:, kc, js], aoT[:, kc, c0:c0 + nn], kc == 0, kc == 3), reads=["wua", "aoT", "aoT_s"], writes=[pAn])
                    for kc in range(4):
                        P.op("pe", MM(pB[:, 0:nn], wup[:, kc, js], poT[:, kc, c0:c0 + nn], kc == 0, kc == 3), reads=["wup", "poT"], writes=[pBn])
                    for kc in range(8):
                        P.op("pe", MM(pGa[:, 0:nn], wga[:, kc, js], hTo[:, kc, c0:c0 + nn], kc == 0, kc == 7), reads=["wga", "hTo"], writes=[pGan])
                    for kc in range(8):
                        P.op("pe", MM(pGp[:, 0:nn], wgp[:, kc, js], hTo[:, kc, c0:c0 + nn], kc == 0, kc == 7), reads=["wgp", "hTo"], writes=[pGpn])
                    P.op("act", ACTV(sa[i2][:, 0:nn], pGa[:, 0:nn], AF.Sigmoid), reads=[pGan], writes=["sa%d" % i2])
                    P.op("act", ACTV(sp_[i2][:, 0:nn], pGp[:, 0:nn], AF.Sigmoid), reads=[pGpn], writes=["sp_%d" % i2])
                    P.op("dve", TT(m1[i2][:, 0:nn], pA[:, 0:nn], sa[i2][:, 0:nn], ALU.mult), reads=[pAn, "sa%d" % i2], writes=["m1%d" % i2])
                    P.op("dve", TT(m2[i2][:, 0:nn], pB[:, 0:nn], sp_[i2][:, 0:nn], ALU.mult), reads=[pBn, "sp_%d" % i2], writes=["m2%d" % i2])
                    P.op("dve", TT(mgT[:, j, c0:c0 + nn], m1[i2][:, 0:nn], m2[i2][:, 0:nn], ALU.add), reads=["m1%d" % i2, "m2%d" % i2], writes=["mgT"])
          P.barrier()

        if STAGE >= 4:
          with ExitStack() as ph:
            wo = sbt(ph, "wo", (128, 8, D), BF16)
            wr = sbt(ph, "wr", (128, 8, 32))
            brb = sbt(ph, "brb", (128, 32))
            iot = sbt(ph, "iot", (128, 32))
            gt1b = sbt(ph, "gt1b", (128, D))
            a2b = sbt(ph, "a2b", (128, D))
            b2b = sbt(ph, "b2b", (128, D))
            gt1s = sbt(ph, "gt1s", (16, D))
            a2s = sbt(ph, "a2s", (16, D))
            b2s = sbt(ph, "b2s", (16, D))
            xts = [sbt(ph, "xd%d" % i, (128, D)) for i in range(2)]
            x1t = [sbt(ph, "x1t%d" % i, (128, D)) for i in range(2)]
            h2t = [sbt(ph, "h2t%d" % i, (128, D)) for i in range(2)]
            h2b = [sbt(ph, "h2b%d" % i, (128, D), BF16) for i in range(2)]
            junk = sbt(ph, "junk2", (128, D), BF16)
            h2T = sbt(ph, "h2T", (128, 8, 128))
            ssq = sbt(ph, "ssq2", (128, 1))
            rs = sbt(ph, "rs2", (128, 1))
            carry = sbt(ph, "carry", (128, 32))
            lg = sbt(ph, "lg", (128, 32))
            t8 = sbt(ph, "t8", (128, 8))
            mk = sbt(ph, "mk", (128, 32))
            mkb = sbt(ph, "mkb", (128, 32), BF16)
            nm = sbt(ph, "nm", (128, 1))
            ex = sbt(ph, "ex", (128, 32))
            den = sbt(ph, "den", (128, 1))
            G = sbt(ph, "G", (128, 32))
            posf = sbt(ph, "posf", (128, 32))
            v2 = sbt(ph, "v2", (128, 32))
            rowsel = sbt(ph, "rowsel", (128, 32))
            ohk = sbt(ph, "ohk", (128, 32))
            ohk2 = sbt(ph, "ohk2", (128, 32))
            posTs = sbt(ph, "posTs", (32, 128))
            P.dma("pool", DMA(wo[:], I["w_out"].rearrange("(k p) c -> p k c", p=128)), "wo", writes=["wo"])
            P.dma("sp", DMA(wr[:], I["w_router"].rearrange("(k p) c -> p k c", p=128)), "wr", writes=["wr"])
            P.dma("sp", DMA(brb[:], I["b_router"].partition_broadcast(128)), "brb", writes=["brb"])
            P.dma("sp", DMA(iot[:], C["iota32"]), "iot", writes=["iot"])
            load_mod("sp", gt1b, "gt1b", MGT1, False, "gt1b")
            load_mod("sp", a2b, "a2b", MA2, False, "a2b")
            load_mod("sp", b2b, "b2b", MSH2, False, "b2b")
            load_mod("sp", gt1s, "gt1s", MGT1, True, "gt1s")
            load_mod("sp", a2s, "a2s", MA2, True, "a2s")
            load_mod("sp", b2s, "b2s", MSH2, True, "b2s")
            P.op("dve", MSET(carry[:], 0.0), writes=["carry"])
            def dc_stage_a(ti):
                smp = ti == 16
                npart = 16 if smp else 128
                c0 = TOK if smp else ti * 128
                i2 = ti % 2
                xt, xn = xts[i2], "xd%d" % i2
                x1, x1n = x1t[i2], "x1t%d" % i2
                h2, h2n = h2t[i2], "h2t%d" % i2
                hb_, hbn_ = h2b[i2], "h2b%d" % i2
                P.dma("sp", DMA(xt[0:npart, :], I["xs"] if smp else I["xh"][HALO + ti * 128:HALO + (ti + 1) * 128, :]), xn, writes=[xn])
                gt, gtn = (gt1s, "gt1s") if smp else (gt1b, "gt1b")
                for half in range(2):
                    hs = slice(half * 512, (half + 1) * 512)
                    pt, pn = ps()
                    for kc in range(8):
                        P.op("pe", MM(pt[0:npart, :], mgT[:, kc, c0:c0 + npart], wo[:, kc, hs], kc == 0, kc == 7), reads=["mgT", "wo"], writes=[pn])
                    P.op("dve", TT(x1[0:npart, hs], pt[0:npart, :], gt[0:npart, hs], ALU.mult), reads=[pn, gtn], writes=[x1n])
                P.op("dve", TT(x1[0:npart, :], x1[0:npart, :], xt[0:npart, :], ALU.add), reads=[x1n, xn], writes=[x1n])
                P.dma("sp", DMA(x1d[c0:c0 + npart, :], x1[0:npart, :]), "x1d_%d" % i2, reads=[x1n], writes=["x1d"])
                norm_tile(x1, x1n, npart, a2s if smp else a2b, "a2s" if smp else "a2b", b2s if smp else b2b, "b2s" if smp else "b2b",
                          h2, h2n, junk, "junk2", ssq, rs, "D")
                P.op("act", ACTV(hb_[0:npart, :], h2[0:npart, :], AF.Copy), reads=[h2n], writes=[hbn_])

            def dc_stage_b(ti):
                smp = ti == 16
                npart = 16 if smp else 128
                c0 = TOK if smp else ti * 128
                i2 = ti % 2
                xt, xn = xts[i2], "xd%d" % i2
                x1, x1n = x1t[i2], "x1t%d" % i2
                h2, h2n = h2t[i2], "h2t%d" % i2
                hb_, hbn_ = h2b[i2], "h2b%d" % i2
                p0, p0n = ps()
                p1, p1n = ps()
                for kc in range(8):
                    pp, ppn = (p0, p0n) if kc < 4 else (p1, p1n)
                    P.op("pe", TR(pp[:, (kc % 4) * 128:(kc % 4) * 128 + npart], h2[0:npart, kc * 128:(kc + 1) * 128], ident[0:npart, 0:npart]),
                         reads=[h2n, "ident"], writes=[ppn])
                for half, (pp, ppn) in enumerate(((p0, p0n), (p1, p1n))):
                    P.op("dve", CP(h2T[:, half * 4:(half + 1) * 4, 0:npart], pp[:].rearrange("p (k t) -> p k t", t=128)[:, :, 0:npart]),
                         reads=[ppn], writes=["h2T"])
                pr_, prn = ps()
                for kc in range(8):
                    P.op("pe", MM(pr_[0:npart, 0:32], h2T[:, kc, 0:npart], wr[:, kc, :], kc == 0, kc == 7), reads=["h2T", "wr"], writes=[prn])
                NP = slice(0, npart)
                P.op("dve", TT(lg[NP, :], pr_[NP, 0:32], brb[NP, :], ALU.add), reads=[prn, "brb"], writes=["lg"])
                P.op("dve", lambda E, NP=NP: E.max(out=t8[NP, :], in_=lg[NP, :]), reads=["lg"], writes=["t8"])
                P.op("dve", TS(mk[NP, :], lg[NP, :], t8[NP, 3:4], None, ALU.is_ge), reads=["lg", "t8"], writes=["mk"])
                P.op("pool", CP(mkb[NP, :], mk[NP, :]), reads=["mk"], writes=["mkb"])
                P.op("dve", TS(nm[NP, :], t8[NP, 0:1], -1.0, None, ALU.mult), reads=["t8"], writes=["nm"])
                P.op("act", ACTV(ex[NP, :], lg[NP, :], AF.Exp, bias=nm[NP, 0:1]), reads=["lg", "nm"], writes=["ex"])
                P.op("dve", TT(ex[NP, :], ex[NP, :], mk[NP, :], ALU.mult), reads=["ex", "mk"], writes=["ex"])
                P.op("dve", lambda E, NP=NP: E.tensor_reduce(out=den[NP, :], in_=ex[NP, :], axis=AX.X, op=ALU.add), reads=["ex"], writes=["den"])
                P.op("dve", RCP(den[NP, :], den[NP, :]), reads=["den"], writes=["den"])
                P.op("dve", TS(G[NP, :], ex[NP, :], den[NP, 0:1], None, ALU.mult), reads=["ex", "den"], writes=["G"])
                pp_, ppn_ = ps()
                P.op("pe", MM(pp_[0:npart, 0:32], ustr_b[0:npart, 0:npart], mkb[0:npart, :]), reads=["ustr_b", "mkb"], writes=[ppn_])
                P.op("dve", TT(posf[NP, :], pp_[NP, 0:32], carry[NP, :], ALU.add), reads=[ppn_, "carry"], writes=["posf"])
                pc_, pcn_ = ps()
                P.op("pe", MM(pc_[:, 0:32], ones_b[0:npart, :], mkb[0:npart, :]), reads=["ones_b", "mkb"], writes=[pcn_])
                P.op("dve", TT(carry[:], carry[:], pc_[:, 0:32], ALU.add), reads=[pcn_, "carry", "posf"], writes=["carry"])
                P.op("dve", TS(v2[NP, :], posf[NP, :], float(CG), None, ALU.is_lt), reads=["posf"], writes=["v2"])
                P.op("dve", TT(v2[NP, :], v2[NP, :], mk[NP, :], ALU.mult), reads=["v2", "mk"], writes=["v2"])
                P.op("dve", TS(rowsel[NP, :], posf[NP, :], 1.0, None, ALU.add), reads=["posf"], writes=["rowsel"])
                P.op("dve", TT(rowsel[NP, :], rowsel[NP, :], v2[NP, :], ALU.mult), reads=["rowsel", "v2"], writes=["rowsel"])
                P.op("dve", TS(rowsel[NP, :], rowsel[NP, :], -1.0, None, ALU.add), reads=["rowsel"], writes=["rowsel"])
                P.dma("sp", DMA(posd[c0:c0 + npart, :], rowsel[NP, :]), "posd", reads=["rowsel"], writes=["posd"])
                P.dma("sp", DMA(gd[c0:c0 + npart, :], G[NP, :]), "gd", reads=["G"], writes=["gd"])
                P.dma("sp", DMA(h2d[c0:c0 + npart, :], hb_[NP, :]), "h2d_%d" % i2, reads=[hbn_], writes=["h2d"])
                pq, pqn = ps()
                P.op("pe", TR(pq[0:32, 0:npart], rowsel[NP, :], ident[0:npart, 0:npart]), reads=["rowsel", "ident"], writes=[pqn])
                P.op("act", ACTV(posTs[:, 0:npart], pq[0:32, 0:npart], AF.Copy), reads=[pqn], writes=["posTs"])
                P.dma("sp", DMA(posTd[:, c0:c0 + npart], posTs[:, 0:npart]), "posTd", reads=["posTs"], writes=["posTd"])
                if ti == 7:
                    P.op("dve", MSET(carry[:], 0.0), reads=["carry"], writes=["carry"])

            for tt_ in range(18):
                if tt_ < 17:
                    dc_stage_a(tt_)
                if tt_ >= 1:
                    dc_stage_b(tt_ - 1)
          P.barrier()
        sc_o.close()

        if STAGE >= 5:
          with ExitStack() as ph:
            bguT = sbt(ph, "bguT", (128, 16, 32))
            Ghl = sbt(ph, "Ghl", (128, 17, 32, 2), BF16)
            with ExitStack() as ph2:
                bgu = sbt(ph2, "bgu", (32, 2 * D))
                G_tm = sbt(ph2, "G_tm", (128, 17, 32))
                Gf = sbt(ph2, "Gf", (128, 17, 32))
                P.op("dve", MSET(G_tm[:], 0.0), writes=["G_tm"])
                P.dma("sp", DMA(G_tm[:, 0:16, :], gd[0:TOK, :].rearrange("(t p) e -> p t e", p=128)), "G_tm", reads=["G_tm"], writes=["G_tm"])
                P.dma("sp", DMA(G_tm[0:16, 16, :], gd[TOK:NTOK, :]), "G_tm", reads=["G_tm"], writes=["G_tm"])
                P.op("dve", CP(Ghl[:, :, :, 0], G_tm[:]), reads=["G_tm"], writes=["Ghl"])
                P.op("dve", CP(Gf[:], Ghl[:, :, :, 0]), reads=["Ghl"], writes=["Gf"])
                P.op("dve", TT(Gf[:], G_tm[:], Gf[:], ALU.subtract), reads=["G_tm", "Gf"], writes=["Gf"])
                P.op("dve", CP(Ghl[:, :, :, 1], Gf[:]), reads=["Gf"], writes=["Ghl"])
                P.dma("sp", DMA(bgu[:], I["b_gate_up"]), "bgu", writes=["bgu"])
                pt, pn = ps()
                for fc in range(16):
                    P.op("pe", TR(pt[:, fc * 32:(fc + 1) * 32], bgu[0:32, fc * 128:(fc + 1) * 128], ident[0:32, 0:32]), reads=["bgu", "ident"], writes=[pn])
                P.op("dve", CP(bguT[:].rearrange("p f e -> p (f e)"), pt[:, :]), reads=[pn], writes=["bguT"])
            P.barrier()
            wpc = [sbt(ph, "wpc%d" % i, (128, 8, 512), BF16) for i in range(2)]
            wd = sbt(ph, "wd", (128, 8, D), BF16)
            bdb = sbt(ph, "bdb", (128, D))
            bdbg = sbt(ph, "bdbg", (128, 512))
            h2bf = sbt(ph, "h2bf", (128, 17, D), BF16)
            oacc = sbt(ph, "oacc", (128, 17, D))
            Pms = [sbt(ph, "Pm%d_" % i, (128, 9, CG), BF16) for i in range(2)]
            xsT = [sbt(ph, "xsT%d" % i, (128, 8, CG), BF16) for i in range(2)]
            actT = [sbt(ph, "actT%d" % i, (128, 8, CG), BF16) for i in range(2)]
            posbc = sbt(ph, "posbc", (128, 1040))
            STm = sbt(ph, "STm", (128, 3, 1040), BF16)
            ygt = sbt(ph, "ygt", (128, 3, D), BF16)
            gs = [sbt(ph, "gs%d" % i, (128, 4)) for i in range(2)]
            gs2 = sbt(ph, "gs2", (128, 2))
            hgc = [sbt(ph, "hgc%d" % i, (128, CG)) for i in range(2)]
            sg = [sbt(ph, "sg%d" % i, (128, CG)) for i in range(2)]
            hu = [sbt(ph, "hu%d" % i, (128, CG)) for i in range(2)]
            pos_tm = sbt(ph, "pos_tm", (128, 17, 32))
            iotac = sbt(ph, "iotac", (128, CG))
            iotaps = sbt(ph, "iotaps", (128, 3))
            P.dma("sp", DMA(iotac[:], C["iotac"]), "iotac", writes=["iotac"])
            P.dma("sp", DMA(iotaps[:], C["iotaps"]), "iotaps", writes=["iotaps"])
            P.op("dve", MSET(pos_tm[:], -1.0), writes=["pos_tm"])
            P.op("pool", MSET(h2bf[:, 16, :], 0.0), writes=["h2bf"])
            P.dma("sp", DMA(pos_tm[:, 0:16, :], posd[0:TOK, :].rearrange("(t p) e -> p t e", p=128)), "pos_tm", reads=["pos_tm"], writes=["pos_tm"])
            P.dma("sp", DMA(pos_tm[0:16, 16, :], posd[TOK:NTOK, :]), "pos_tm", reads=["pos_tm"], writes=["pos_tm"])
            P.dma("sp", DMA(h2bf[:, 0:16, :], h2d[0:TOK, :].rearrange("(t p) d -> p t d", p=128)), "h2bf", reads=["h2bf"], writes=["h2bf"])
            P.dma("sp", DMA(h2bf[0:16, 16, :], h2d[TOK:NTOK, :]), "h2bf", reads=["h2bf"], writes=["h2bf"])
            HALVES = ((0, 8), (8, 17))
            npc = 0
            for e in range(NEXP):
                P.dma("pool", DMA(wd[:], I["w_down"][e].rearrange("(k p) f -> p k f", p=128)), "wd", writes=["wd"])
                P.dma("sp", DMA(bdb[:], I["b_down"][e:e + 1, :].partition_broadcast(128)), "bdb", writes=["bdb"])
                for hf, (t_lo, t_hi) in enumerate(HALVES):
                    T0 = t_lo * 128
                    ntok = (t_hi - t_lo) * 128 if t_hi < 17 else (16 - t_lo) * 128 + 16
                    Pm = Pms[hf]
                    for tl, ti in enumerate(range(t_lo, t_hi)):
                        npart = 16 if ti == 16 else 128
                        nw = min(CG, 128 * (tl + 1))
                        P.op("dve", TS(Pm[0:npart, tl, 0:nw], iotac[0:npart, 0:nw], pos_tm[0:npart, ti, e:e + 1], None, ALU.is_equal),
                             reads=["iotac", "pos_tm"], writes=["Pm%d_%d" % (hf, tl)])
                    for kc in range(8):
                        pt, pn = ps()
                        order = list(range(t_hi - t_lo))[::-1]
                        for oi, tl in enumerate(order):
                            ti = t_lo + tl
                            npart = 16 if ti == 16 else 128
                            nw = min(CG, 128 * (tl + 1))
                            P.op("pe", MM(pt[:, 0:nw], h2bf[0:npart, ti, kc * 128:(kc + 1) * 128], Pm[0:npart, tl, 0:nw], oi == 0, oi == len(order) - 1),
                                 reads=["h2bf", "Pm%d_%d" % (hf, tl)], writes=[pn])
                        P.op("act", ACTV(xsT[hf][:, kc, :], pt[:, 0:CG], AF.Copy), reads=[pn], writes=["xsT%d" % hf])
                    for sc, (s0, sn) in enumerate(SCH):
                        pt, pn = ps()
                        tls = [tl for tl in range(t_hi - t_lo) if 128 * (tl + 1) > s0]
                        for oi, tl in enumerate(tls):
                            ti = t_lo + tl
                            npart = 16 if ti == 16 else 128
                            P.op("pe", MM(pt[0:sn, 0:2], Pm[0:npart, tl, s0:s0 + sn], Ghl[0:npart, ti, e, :], oi == 0, oi == len(tls) - 1),
                                 reads=["Ghl", "Pm%d_%d" % (hf, tl)], writes=[pn])
                        P.op("dve", CP(gs2[0:sn, :], pt[0:sn, 0:2]), reads=[pn], writes=["gs2"])
                        P.op("dve", TT(gs[hf][0:sn, sc:sc + 1], gs2[0:sn, 0:1], gs2[0:sn, 1:2], ALU.add), reads=["gs2"], writes=["gs%d" % hf])
                for pc in range(4):
                    w = wpc[npc % 2]
                    wn = "wpc%d" % (npc % 2)
                    npc += 1
                    P.dma("pool", DMA(w[:, :, 0:256], I["w_gate_up"][e][:, pc * 256:(pc + 1) * 256].rearrange("(k p) f -> p k f", p=128)), wn, writes=[wn])
                    P.dma("pool", DMA(w[:, :, 256:512], I["w_gate_up"][e][:, D + pc * 256:D + (pc + 1) * 256].rearrange("(k p) f -> p k f", p=128)), wn, writes=[wn])
                    for hf in range(2):
                        for j in range(2):
                            fc = pc * 2 + j
                            i2 = (fc + hf) % 2
                            pg, pgn = ps()
                            pu, pun = ps()
                            for kc in range(8):
                                P.op("pe", MM(pg[:, 0:CG], w[:, kc, j * 128:(j + 1) * 128], xsT[hf][:, kc, :], kc == 0, kc == 7), reads=[wn, "xsT%d" % hf], writes=[pgn])
                            for kc in range(8):
                                P.op("pe", MM(pu[:, 0:CG], w[:, kc, 256 + j * 128:256 + (j + 1) * 128], xsT[hf][:, kc, :], kc == 0, kc == 7), reads=[wn, "xsT%d" % hf], writes=[pun])
                            P.op("dve", TS(hgc[i2][:], pg[:, 0:CG], bguT[:, fc, e:e + 1], 7.0, ALU.add, ALU.min), reads=[pgn, "bguT"], writes=["hgc%d" % i2])
                            P.op("act", ACTV(sg[i2][:], hgc[i2][:], AF.Sigmoid, scale=1.702), reads=["hgc%d" % i2], writes=["sg%d" % i2])
                            P.op("act", ACTV(hu[i2][:], pu[:, 0:CG], AF.Identity, bias=bguT[:, 8 + fc, e:e + 1]), reads=[pun, "bguT"], writes=["hu%d" % i2])
                            P.op("dve", TS(hu[i2][:], hu[i2][:], 7.0, -7.0, ALU.min, ALU.max), reads=["hu%d" % i2], writes=["hu%d" % i2])
                            P.op("dve", TT(hgc[i2][:], hgc[i2][:], sg[i2][:], ALU.mult), reads=["hgc%d" % i2, "sg%d" % i2], writes=["hgc%d" % i2])
                            P.op("dve", STT(actT[hf][:, fc, :], hu[i2][:], 1.0, hgc[i2][:], ALU.add, ALU.mult), reads=["hgc%d" % i2, "hu%d" % i2], writes=["actT%d" % hf])
                for hf, (t_lo, t_hi) in enumerate(HALVES):
                    T0 = t_lo * 128
                    ntok = (t_hi - t_lo) * 128 if t_hi < 17 else (16 - t_lo) * 128 + 16
                    P.dma("sp", DMA(posbc[:, 0:ntok], posTd[e:e + 1, T0:T0 + ntok].partition_broadcast(128)), "posbc", writes=["posbc"])
                    for sc, (s0, sn) in enumerate(SCH):
                        P.op("dve", TS(STm[0:sn, sc, 0:ntok], posbc[0:sn, 0:ntok], iotaps[0:sn, sc:sc + 1], None, ALU.is_equal),
                             reads=["posbc", "iotaps"], writes=["STm"])
                    for sc, (s0, sn) in enumerate(SCH):
                        for h2_ in range(2):
                            hs = slice(h2_ * 512, (h2_ + 1) * 512)
                            P.op("act", ACTV(bdbg[0:sn, :], bdb[0:sn, hs], AF.Copy, scale=gs[hf][0:sn, sc:sc + 1]), reads=["bdb", "gs%d" % hf], writes=["bdbg"])
                            pt, pn = ps()
                            for fc in range(8):
                                P.op("pe", MM(pt[0:sn, :], actT[hf][:, fc, s0:s0 + sn], wd[:, fc, hs], fc == 0, fc == 7), reads=["actT%d" % hf, "wd"], writes=[pn])
                            P.op("dve", STT(ygt[0:sn, sc, hs], pt[0:sn, :], gs[hf][0:sn, sc:sc + 1], bdbg[0:sn, :], ALU.mult, ALU.add),
                                 reads=[pn, "gs%d" % hf, "bdbg"], writes=["ygt"])
                    for tl, ti in enumerate(range(t_lo, t_hi)):
                        npart = 16 if ti == 16 else 128
                        for h2_ in range(2):
                            hs = slice(h2_ * 512, (h2_ + 1) * 512)
                            pt, pn = ps()
                            scs = [(sc, s0, sn) for sc, (s0, sn) in enumerate(SCH) if 128 * (tl + 1) > s0]
                            for oi, (sc, s0, sn) in enumerate(scs):
                                P.op("pe", MM(pt[0:npart, :], STm[0:sn, sc, tl * 128:tl * 128 + npart], ygt[0:sn, sc, hs], oi == 0, oi == len(scs) - 1),
                                     reads=["STm", "ygt"], writes=[pn])
                            on = "oacc%d_%d" % (ti, h2_)
                            if e == 0:
                                P.op("act", ACTV(oacc[0:npart, ti, hs], pt[0:npart, :], AF.Copy), reads=[pn], writes=[on])
                            else:
                                P.op("dve", TT(oacc[0:npart, ti, hs], pt[0:npart, :], oacc[0:npart, ti, hs], ALU.add), reads=[pn, on], writes=[on])
            P.barrier()
            wdf = wd[:].rearrange("p k f -> p (k f)").bitcast(F32)
            gt2b = wdf[:, 0:D]
            gt2s = wdf[:, D:2 * D]
            P.dma("sp", DMA(gt2b, modd[0, MGT2:MGT2 + 1, :].partition_broadcast(128)), "gt2b", writes=["gt2b"])
            P.dma("sp", DMA(gt2s[0:16, :], modd[1:17, MGT2, :]), "gt2s", writes=["gt2s"])
            for ti in range(17):
                smp = ti == 16
                npart = 16 if smp else 128
                NP = slice(0, npart)
                c0 = TOK if smp else ti * 128
                xr = wdf[:, (2 + ti % 2) * D:(3 + ti % 2) * D]
                xrn = "xr%d" % (ti % 2)
                P.dma("sp", DMA(xr[NP, :], x1d[c0:c0 + npart, :]), xrn, writes=[xrn])
                gt = gt2s if smp else gt2b
                on = "oaccf%d" % ti
                P.op("dve", TT(oacc[NP, ti, :], oacc[NP, ti, :], gt[NP, :], ALU.mult), reads=["gt2b", "gt2s"], writes=[on])
                P.op("dve", TT(oacc[NP, ti, :], oacc[NP, ti, :], xr[NP, :], ALU.add), reads=[on, xrn], writes=[on])
                finals.append(P.dma("sp", DMA(O["y"][c0:c0 + npart, :], oacc[NP, ti, :]), "yout%d" % (ti % 4), reads=[on], writes=["o_y"]))

    P.emit(nc, finals)
    return nc


_NC_CACHE = {}


def kernel(**inputs):
    inp = {k: np.ascontiguousarray(np.asarray(v, dtype=np.float32)) for k, v in inputs.items()}
    xp = inp["x_prompt"][0]
    in_maps = []
    for c in range(NCORES):
        m = {}
        xh = np.zeros((HALO + TOK, D), np.float32)
        if c > 0:
            xh[:HALO] = xp[(c - 1) * TOK:c * TOK]
        xh[HALO:] = xp[c * TOK:(c + 1) * TOK]
        m["xh"] = xh
        s0, s1 = c * NS, (c + 1) * NS
        m["xs"] = inp["x_sample"][s0:s1, 0]
        m["cp"] = inp["c_prompt"]
        m["cs"] = inp["c_sample"][s0:s1]
        m["kv1"] = inp["cache_kv_w128"][0, s0:s1].reshape(NS, 128, 1024)
        m["kv2"] = inp["cache_kv_w512"][0, s0:s1, 0::4].reshape(NS, 128, 1024)
        m["kv3"] = inp["cache_kv_w2048"][0, s0:s1, 0::16].reshape(NS, 128, 1024)
        m["spool"] = inp["state_pool"][0, s0:s1]
        m["w_ada"] = inp["w_ada"][0]
        m["b_ada"] = inp["b_ada"]
        m["g1"] = inp["norm_mix_g"]
        m["g2"] = inp["norm_ffn_g"]
        m["w_in"] = inp["w_in"][0]
        m["qg"] = inp["q_norm_g"]
        m["kg"] = inp["k_norm_g"]
        m["rel_bias"] = inp["rel_bias"]
        m["w_pool_mix"] = inp["w_pool_mix"][0]
        m["pool_scale"] = inp["pool_scale"]
        m["w_up_attn"] = inp["w_up_attn"][0]
        m["w_up_pool"] = inp["w_up_pool"][0]
        m["w_out"] = inp["w_out"][0]
        m["w_router"] = inp["w_router"][0]
        m["b_router"] = inp["b_router"]
        m["w_gate_up"] = inp["w_gate_up"][0]
        m["b_gate_up"] = inp["b_gate_up"][0]
        m["w_down"] = inp["w_down"][0]
        m["b_down"] = inp["b_down"][0]
        for k, v in static_consts(c).items():
            m["c_" + k] = v
        m = {k: np.ascontiguousarray(v, dtype=np.float32) for k, v in m.items() if (k in IN_SHAPES or k.startswith("c_"))}
        in_maps.append(m)
    if "nc" not in _NC_CACHE:
        _NC_CACHE["nc"] = build()
    res = run_bass_kernel_spmd(_NC_CACHE["nc"], in_maps, core_ids=list(range(NCORES)))
    R = res.results
    if os.environ.get("MK_DBG"):
        _NC_CACHE["res"] = R
    y_prompt = np.concatenate([R[c]["y"][:TOK] for c in range(NCORES)], 0)[None]
    y_sample = np.concatenate([R[c]["y"][TOK:] for c in range(NCORES)], 0)[:, None, :]
    L = R[NCORES - 1]
    kv128 = L["kvp1"].reshape(1, 1, 128, 2, 8, 64)
    kv512 = L["kvp2"].reshape(1, 1, 512, 2, 8, 64)
    kv2048 = L["kvp3"].reshape(1, 1, 2048, 2, 8, 64)
    pool_p = L["poolp"][1:16].reshape(1, 1, 15, 512)
    kvs = np.concatenate([R[c]["kvs"] for c in range(NCORES)], 0)
    ks = [kvs[:, g].reshape(1, 128, 1, 2, 8, 64) for g in range(3)]
    pool_s = np.concatenate([R[c]["pools"] for c in range(NCORES)], 0)[None]
    return (y_prompt.astype(np.float32), y_sample.astype(np.float32), kv128, kv512, kv2048, pool_p,
            ks[0], ks[1], ks[2], pool_s.astype(np.float32))
```

```python
import os
from contextlib import ExitStack
import numpy as np
import concourse.bass as bass
import concourse.mybir as mybir
from concourse.bass_utils import run_bass_kernel_spmd

F32 = mybir.dt.float32
BF16 = mybir.dt.bfloat16
I32 = mybir.dt.int32
AF = mybir.ActivationFunctionType
ALU = mybir.AluOpType
AX = mybir.AxisListType

NCORES = 8
D = 1024
TOK = 2048
HALO = 2048
NS = 16
OWN0 = HALO
SMP0 = HALO + TOK
TCOL = HALO + TOK + NS
NTOK = TOK + NS
CAP = 512
CG = 320
SCH = ((0, 128), (128, 128), (256, 64))
NEXP = 32
NROWS = NEXP * CAP
EPS = 1e-6
DILS = (1, 4, 16)
STAGE = int(os.environ.get("MK_STAGE", "9"))
CL = int(os.environ.get("MK_CL", "99"))
CONE = int(os.environ.get("MK_ONE", "0"))
VL = int(os.environ.get("MK_VL", "9"))


class Prog:
    ENG = ("pe", "act", "dve", "pool", "sp")

    def __init__(self):
        self.q = {e: [] for e in self.ENG}
        self.lastw = {}
        self.readers = {}
        self.dmakeys = {}
        self.keymap = {}
        self.psn = 0

    def _collect(self, reads, writes):
        deps = []
        for r in reads:
            if r in self.lastw:
                deps.append(self.lastw[r])
        for w in writes:
            if w in self.lastw:
                deps.append(self.lastw[w])
            deps.extend(self.readers.get(w, ()))
        return deps

    def _commit(self, rec, reads, writes):
        for r in reads:
            self.readers.setdefault(r, []).append(rec)
        for w in writes:
            self.lastw[w] = rec
            self.readers[w] = []

    def op(self, eng, fn, reads=(), writes=(), extra=()):
        writes = list(writes) + [r for r in reads if r.startswith("ps") and r[2:3] in "01234567OL"]
        reads = [r for r in reads if not (r.startswith("ps") and r[2:3] in "01234567OL")]
        rec = {"eng": eng, "fn": fn, "deps": self._collect(reads, writes) + list(extra), "dma": None, "needed": False}
        self._commit(rec, reads, writes)
        self.q[eng].append(rec)
        return rec

    def dma(self, eng, fn, key, reads=(), writes=(), extra=()):
        if (eng, key) not in self.keymap:
            self.keymap[(eng, key)] = (eng, sum(1 for k in self.keymap if k[0] == eng))
        key = self.keymap[(eng, key)]
        cnt = self.dmakeys.get(key, 0) + 16
        self.dmakeys[key] = cnt
        rec = {"eng": eng, "fn": fn, "deps": self._collect(reads, writes) + list(extra), "dma": (key, cnt), "needed": False}
        self._commit(rec, reads, writes)
        self.q[eng].append(rec)
        return rec

    def barrier(self):
        last = []
        for e in self.ENG:
            for rec in reversed(self.q[e]):
                if rec["dma"] is None and rec["fn"] is not None:
                    last.append(rec)
                    break
        seen = {}
        for e in self.ENG:
            for rec in self.q[e]:
                if rec["dma"] is not None:
                    seen[rec["dma"][0]] = rec
        deps = last + list(seen.values())
        for e in self.ENG:
            rec = {"eng": e, "fn": None, "deps": list(deps), "dma": None, "needed": False}
            self.q[e].append(rec)
        self.lastw = {}
        self.readers = {}
        self.keymap = {}

    def emit(self, nc, final_deps):
        for e in self.ENG:
            for rec in self.q[e]:
                for d in rec["deps"]:
                    if d["dma"] is None and not (d["eng"] == "pe" and rec["eng"] == "pe"):
                        d["needed"] = True
        for e in self.ENG:
            c = 0
            for rec in self.q[e]:
                if rec["dma"] is None and rec["needed"]:
                    c += 1
                    rec["cnt"] = c
        keys = sorted(self.dmakeys.keys(), key=str)
        with ExitStack() as st:
            esem = {e: st.enter_context(nc.semaphore("es_" + e)) for e in self.ENG}
            dsem = {k: st.enter_context(nc.semaphore("ds%d" % i)) for i, k in enumerate(keys)}
            block = st.enter_context(nc.Block())

            def run(e, E):
                waited = {}
                recs = list(self.q[e])
                if e == "sp":
                    recs.append({"eng": "sp", "fn": None, "deps": list(final_deps), "dma": None, "needed": False})
                for rec in recs:
                    for d in rec["deps"]:
                        if d["dma"] is not None:
                            k = ("d", d["dma"][0])
                            sem, val = dsem[d["dma"][0]], d["dma"][1]
                        else:
                            if d["eng"] == "pe" and e == "pe":
                                continue
                            k = ("c", d["eng"])
                            sem, val = esem[d["eng"]], d["cnt"]
                        if waited.get(k, 0) >= val:
                            continue
                        E.wait_ge(sem, val)
                        waited[k] = val
                    if rec["fn"] is None:
                        continue
                    ins = rec["fn"](E)
                    if rec["dma"] is not None:
                        ins.then_inc(dsem[rec["dma"][0]], 16)
                    elif rec["needed"]:
                        ins.then_inc(esem[e], 1)

            @block.tensor
            def _(E):
                run("pe", E)

            @block.scalar
            def _(E):
                run("act", E)

            @block.vector
            def _(E):
                run("dve", E)

            @block.gpsimd
            def _(E):
                run("pool", E)

            @block.sync
            def _(E):
                run("sp", E)


def MM(out, lhsT, rhs, start=True, stop=True):
    return lambda E: E.matmul(out, lhsT, rhs, start=start, stop=stop)


def TR(out, in_, ident):
    return lambda E: E.transpose(out, in_, ident)


def ACTV(out, in_, func, **kw):
    return lambda E: E.activation(out=out, in_=in_, func=func, **kw)


def TT(out, in0, in1, op):
    return lambda E: E.tensor_tensor(out=out, in0=in0, in1=in1, op=op)


def TS(out, in0, s1, s2, op0, op1=None, **kw):
    if op1 is None:
        return lambda E: E.tensor_scalar(out=out, in0=in0, scalar1=s1, scalar2=None, op0=op0, **kw)
    return lambda E: E.tensor_scalar(out=out, in0=in0, scalar1=s1, scalar2=s2, op0=op0, op1=op1, **kw)


def STT(out, in0, scalar, in1, op0, op1):
    return lambda E: E.scalar_tensor_tensor(out=out, in0=in0, scalar=scalar, in1=in1, op0=op0, op1=op1)


def CP(out, in_):
    return lambda E: E.tensor_copy(out=out, in_=in_)


def RCP(out, in_):
    return lambda E: E.reciprocal(out=out, in_=in_)


def RCPF(out, in_):
    return lambda E: E.reciprocal_approx_fast(out=out, in_=in_)


def MSET(ap, v):
    return lambda E: E.memset(ap, v)


def DMA(out, in_, **kw):
    return lambda E: E.dma_start(out=out, in_=in_, **kw)


def t5_bucket_np(d):
    d = np.asarray(d, np.int64)
    ratio = np.log(np.maximum(d, 1).astype(np.float32) / np.float32(16)) / np.float32(np.log(2048 / 16))
    large = np.minimum(16 + (ratio.astype(np.float32) * np.float32(16)).astype(np.int32), 31)
    return np.where(d < 16, d, large)


def static_consts(core):
    c = {}
    c["ident"] = np.eye(128, dtype=np.float32)
    c["jflip"] = np.eye(128, dtype=np.float32)[::-1].copy()
    bo = np.zeros((128, 128), np.float32)
    bo[:64, :64] = 1.0 / 64
    bo[64:, 64:] = 1.0 / 64
    c["blockones"] = bo
    us = np.triu(np.ones((128, 128), np.float32), 1)
    c["ustrict"] = us
    ohc = np.zeros((3, 32, 256), np.float32)
    ohp = np.zeros((3, 32, 256), np.float32)
    val = np.zeros((2, 8, 256), np.float32)
    for g, dil in enumerate(DILS):
        for m in range(255):
            u = m - 127
            if u >= 0:
                ohc[g, t5_bucket_np(u * dil), m] = 1.0
            if u <= 0:
                ohp[g, t5_bucket_np((u + 128) * dil), m] = 1.0
    for m in range(255):
        u = m - 127
        val[1, :, m] = 1.0 if u >= 0 else 0.0
        val[0, :, m] = 1.0 if u <= 0 else 0.0
    c["ohc"] = ohc
    c["ohp"] = ohp
    c["valid"] = val
    ohs = np.zeros((3, 32, 128), np.float32)
    for g, dil in enumerate(DILS):
        for i in range(128):
            ohs[g, t5_bucket_np(dil * (128 - i)), i] = 1.0
    c["ohs"] = ohs
    selbc = np.zeros((16, 16, 128), np.float32)
    for n in range(16):
        selbc[n, n, :] = 1.0
    c["selbc"] = selbc.reshape(16, 16 * 128)
    selcol = np.zeros((128, 16, 16), np.float32)
    for n in range(16):
        selcol[:, n, n] = 1.0
    c["selcol"] = selcol.reshape(128, 256)
    c["hp"] = np.full((128, 1), 0.0 if core == 0 else 1.0, np.float32)
    corr = np.ones((128, 4, 16), np.float32)
    for gi, w in enumerate((2, 4, 8, 16)):
        for t in range(16):
            cnt = min(t + 1, w) if core == 0 else w
            corr[:, gi, t] = 1.0 / cnt
    c["poolcorr"] = corr.reshape(128, 64)
    c["iota32"] = np.tile(np.arange(32, dtype=np.float32)[None, :] * CAP, (128, 1))
    c["iotac"] = np.tile(np.arange(CG, dtype=np.float32)[None, :], (128, 1))
    c["iotaps"] = (np.arange(128, dtype=np.float32)[:, None] + 128.0 * np.arange(3, dtype=np.float32)[None, :])
    return c


CONST_SHAPES = {
    "ident": (128, 128), "jflip": (128, 128), "blockones": (128, 128), "ustrict": (128, 128),
    "ohc": (3, 32, 256), "ohp": (3, 32, 256), "valid": (2, 8, 256), "ohs": (3, 32, 128),
    "selbc": (16, 2048), "selcol": (128, 256), "hp": (128, 1), "poolcorr": (128, 64), "iota32": (128, 32), "iotac": (128, CG), "iotaps": (128, 3),
}

IN_SHAPES = {
    "xh": (HALO + TOK, D), "xs": (NS, D), "cp": (1, D), "cs": (NS, D),
    "kv1": (NS, 128, 1024), "kv2": (NS, 128, 1024), "kv3": (NS, 128, 1024), "spool": (NS, 15, 512),
    "w_ada": (D, 6 * D), "b_ada": (1, 6 * D), "g1": (1, D), "g2": (1, D), "w_in": (D, 7168),
    "qg": (1, 64), "kg": (1, 64), "rel_bias": (32, 24), "w_pool_mix": (4, 128, 128), "pool_scale": (1, 512),
    "w_up_attn": (512, D), "w_up_pool": (512, D), "w_out": (D, D), "w_router": (D, 32), "b_router": (1, 32),
    "w_gate_up": (NEXP, D, 2 * D), "b_gate_up": (NEXP, 2 * D), "w_down": (NEXP, D, D), "b_down": (NEXP, D),
}
if STAGE < 5:
    for _k in ("w_gate_up", "b_gate_up", "w_down", "b_down"):
        IN_SHAPES.pop(_k)
OUT_SHAPES = {
    "y": (NTOK, D), "kvp1": (128, 1024), "kvp2": (512, 1024), "kvp3": (2048, 1024), "poolp": (16, 512),
    "kvs": (NS, 3, 1024), "pools": (NS, 15, 512),
}


def build():
    nc = bass.Bass("TRN2", target_bir_lowering=False)
    P = Prog()
    I = {k: nc.dram_tensor(k, list(s), F32, kind="ExternalInput").ap() for k, s in IN_SHAPES.items()}
    C = {k: nc.dram_tensor("c_" + k, list(s), F32, kind="ExternalInput").ap() for k, s in CONST_SHAPES.items()}
    O = {k: nc.dram_tensor(k, list(s), F32, kind="ExternalOutput").ap() for k, s in OUT_SHAPES.items()}

    def scr(name, shape, dt=F32):
        return nc.dram_tensor(name, list(shape), dt, kind="Internal").ap()

    modd = scr("modd", (17, 6, D))
    evd = scr("evd", (3, 2, 8, 256))
    x1d = scr("x1d", (NTOK, D))
    xsd = scr("xsd", (NROWS, D), BF16)
    ysd = scr("ysd", (NROWS, D))
    posd = scr("posd", (NTOK, 32))
    gd = scr("gd", (NTOK, 32))
    posTd = scr("posTd", (32, NTOK))
    h2d = scr("h2d", (NTOK, D), BF16)
    finals = []
    DBGO = {}

    def dbg(name, ap_sb, shape, dt, reads):
        if not os.environ.get("MK_DBG"):
            return
        t = nc.dram_tensor("dbg_" + name, list(shape), dt, kind="ExternalOutput").ap()
        DBGO[name] = t
        finals.append(P.dma("sp", DMA(t, ap_sb), "dbg_" + name, reads=reads, writes=["dbg_" + name]))

    with ExitStack() as top:
        def sbt(st, name, shape, dt=F32):
            return st.enter_context(nc.sbuf_tensor(name, list(shape), dt))

        PS = [top.enter_context(nc.psum_tensor("ps%d" % i, [128, 512], F32)) for i in range(8)]

        P.psmod = 8

        def ps():
            i = P.psn % P.psmod
            P.psn += 1
            return PS[i], "ps%d" % i

        ident = sbt(top, "ident", (128, 128))
        jflip = sbt(top, "jflip", (128, 128))
        ones_f = sbt(top, "ones_f", (128, 128))
        zo_f = sbt(top, "zo_f", (1, 128))
        ones_b = sbt(top, "ones_b", (128, 128), BF16)
        bones_b = sbt(top, "bones_b", (128, 128), BF16)
        ustr_b = sbt(top, "ustr_b", (128, 128), BF16)
        epst = sbt(top, "epst", (128, 1))
        hpt = sbt(top, "hpt", (128, 1))
        gq = sbt(top, "gq", (128, 1))
        gk = sbt(top, "gk", (128, 1))
        ident_b = sbt(top, "ident_b", (128, 128), BF16)
        hlast = sbt(top, "hlast", (128, 8, 16), BF16)
        P.dma("pool", DMA(ident_b[:], C["ident"]), "ident_b", writes=["ident_b"])
        P.dma("sp", DMA(ident[:], C["ident"]), "ident", writes=["ident"])
        P.dma("sp", DMA(jflip[:], C["jflip"]), "jflip", writes=["jflip"])
        P.dma("sp", DMA(hpt[:], C["hp"]), "hpt", writes=["hpt"])
        P.dma("pool", DMA(bones_b[:], C["blockones"]), "bones_b", writes=["bones_b"])
        P.dma("pool", DMA(ustr_b[:], C["ustrict"]), "ustr_b", writes=["ustr_b"])
        P.op("dve", MSET(ones_f[:], 1.0), writes=["ones_f"])
        P.op("dve", MSET(zo_f[:, 0:64], 0.0), writes=["zo_f"])
        P.op("dve", MSET(zo_f[:, 64:128], 1.0), writes=["zo_f"])
        P.op("dve", MSET(ones_b[:], 1.0), writes=["ones_b"])
        P.op("dve", MSET(epst[:], EPS), writes=["epst"])
        for (t, src, nm) in ((gq, I["qg"], "gq"), (gk, I["kg"], "gk")):
            srcT = src.rearrange("o e -> e o")
            with nc.allow_non_contiguous_dma(reason="tiny gain vector"):
                P.dma("sp", DMA(t[0:64, :], srcT), nm, writes=[nm])
                P.dma("sp", DMA(t[64:128, :], srcT), nm, writes=[nm])
        P.op("dve", TS(gq[:], gq[:], 0.125, None, ALU.mult), reads=["gq"], writes=["gq"])

        with ExitStack() as ph:
            csb = sbt(ph, "csb", (16, D))
            cpb = sbt(ph, "cpb", (1, D))
            scT = sbt(ph, "scT", (128, 8, 17), BF16)
            modp = sbt(ph, "modp", (1, 6 * D))
            mods = sbt(ph, "mods", (16, 6 * D))
            badas = sbt(ph, "badas", (16, 6 * D))
            g1s = sbt(ph, "g1s", (16, 2, D))
            wad = [sbt(ph, "wad%d" % i, (128, 8, 1024), BF16) for i in range(2)]
            P.dma("sp", DMA(csb[:], I["cs"]), "csb", writes=["csb"])
            P.dma("sp", DMA(cpb[:], I["cp"]), "cpb", writes=["cpb"])
            P.dma("sp", DMA(badas[:], I["b_ada"].partition_broadcast(16)), "badas", writes=["badas"])
            P.dma("sp", DMA(g1s[:, 0, :], I["g1"].partition_broadcast(16)), "g1s", writes=["g1s"])
            P.dma("sp", DMA(g1s[:, 1, :], I["g2"].partition_broadcast(16)), "g1s", writes=["g1s"])
            P.op("act", ACTV(csb[:], csb[:], AF.Silu), reads=["csb"], writes=["csb"])
            P.op("act", ACTV(cpb[:], cpb[:], AF.Silu), reads=["cpb"], writes=["cpb"])
            pt, pn = ps()
            for kc in range(8):
                P.op("pe", TR(pt[:, kc * 17:kc * 17 + 1], cpb[0:1, kc * 128:(kc + 1) * 128], ident[0:1, 0:1]),
                     reads=["cpb", "ident"], writes=[pn])
                P.op("pe", TR(pt[:, kc * 17 + 1:kc * 17 + 17], csb[0:16, kc * 128:(kc + 1) * 128], ident[0:16, 0:16]),
                     reads=["csb", "ident"], writes=[pn])
            P.op("dve", CP(scT[:].rearrange("p k c -> p (k c)"), pt[:, 0:136]), reads=[pn], writes=["scT"])
            for blk in range(6):
                w = wad[blk % 2]
                wn = "wad%d" % (blk % 2)
                P.dma("pool", DMA(w[:], I["w_ada"][:, blk * 1024:(blk + 1) * 1024].rearrange("(k p) c -> p k c", p=128)),
                      wn, writes=[wn])
                for half in range(2):
                    c0 = blk * 1024 + half * 512
                    pa, pan = ps()
                    pb, pbn = ps()
                    for kc in range(8):
                        P.op("pe", MM(pa[0:1, :], scT[:, kc, 0:1], w[:, kc, half * 512:(half + 1) * 512], kc == 0, kc == 7),
                             reads=["scT", wn], writes=[pan])
                    for kc in range(8):
                        P.op("pe", MM(pb[0:16, :], scT[:, kc, 1:17], w[:, kc, half * 512:(half + 1) * 512], kc == 0, kc == 7),
                             reads=["scT", wn], writes=[pbn])
                    P.op("dve", TT(modp[:, c0:c0 + 512], pa[0:1, :], badas[0:1, c0:c0 + 512], ALU.add),
                         reads=[pan, "badas"], writes=["modp"])
                    P.op("dve", TT(mods[:, c0:c0 + 512], pb[0:16, :], badas[:, c0:c0 + 512], ALU.add),
                         reads=[pbn, "badas"], writes=["mods"])
            for (m, npart, eng) in ((modp, 1, "dve"), (mods, 16, "dve")):
                mn = "modp" if npart == 1 else "mods"
                for (sc, gi) in ((1, 0), (4, 1)):
                    P.op(eng, STT(m[0:npart, sc * D:(sc + 1) * D], m[0:npart, sc * D:(sc + 1) * D], 1.0, g1s[0:npart, gi, :],
                                  ALU.add, ALU.mult), reads=[mn, "g1s"], writes=[mn])
            P.dma("sp", DMA(modd[0:1].rearrange("o j d -> o (j d)"), modp[:]), "modd", reads=["modp"], writes=["modd"])
            P.dma("sp", DMA(modd[1:17].rearrange("o j d -> o (j d)"), mods[:]), "modd", reads=["mods"], writes=["modd"])
        P.barrier()
        MSH1, MA1, MGT1, MSH2, MA2, MGT2 = range(6)

        def load_mod(eng, dst, dname, j, sample, key):
            if sample:
                return P.dma(eng, DMA(dst[0:16, :], modd[1:17, j, :]), key, reads=["modd"], writes=[dname])
            return P.dma(eng, DMA(dst[:, :], modd[0, j:j + 1, :].partition_broadcast(128)), key, reads=["modd"], writes=[dname])

        sc_o = top.enter_context(ExitStack())
        aoT = sbt(sc_o, "aoT", (128, 4, NTOK), BF16)
        hTo = sbt(sc_o, "hTo", (128, 8, NTOK), BF16)
        sc_h = sc_o.enter_context(ExitStack())
        hTh = sbt(sc_h, "hTh", (128, 8, HALO), BF16)

        def hcols(c0, n, step=1):
            if c0 < OWN0:
                assert c0 + (n - 1) * step < OWN0
                return hTh, "hTh", c0
            return hTo, "hTo", c0 - OWN0

        def norm_tile(xt, xn, npart, a_t, a_n, b_t, b_n, out_t, out_n, tmp, tmpn, ssq, rs, tag):
            P.op("act", ACTV(tmp[0:npart, :], xt[0:npart, :], AF.Square, accum_out=ssq[0:npart, 0:1]),
                 reads=[xn], writes=[tmpn, tag + "ssq"])
            P.op("dve", TS(rs[0:npart, 0:1], ssq[0:npart, 0:1], 1.0 / D, EPS, ALU.mult, ALU.add),
                 reads=[tag + "ssq"], writes=[tag + "rs"])
            P.op("act", ACTV(rs[0:npart, 0:1], rs[0:npart, 0:1], AF.Ln), reads=[tag + "rs"], writes=[tag + "rs"])
            P.op("act", ACTV(rs[0:npart, 0:1], rs[0:npart, 0:1], AF.Exp, scale=-0.5), reads=[tag + "rs"], writes=[tag + "rs"])
            P.op("dve", STT(out_t[0:npart, :], xt[0:npart, :], rs[0:npart, 0:1], a_t[0:npart, :], ALU.mult, ALU.mult),
                 reads=[xn, tag + "rs", a_n], writes=[out_n])
            P.op("dve", TT(out_t[0:npart, :], out_t[0:npart, :], b_t[0:npart, :], ALU.add), reads=[out_n, b_n], writes=[out_n])

        with ExitStack() as ph:
            a1b = sbt(ph, "a1b", (128, D))
            b1b = sbt(ph, "b1b", (128, D))
            a1s = sbt(ph, "a1s", (16, D))
            b1s = sbt(ph, "b1s", (16, D))
            xts = [sbt(ph, "xt%d" % i, (128, D)) for i in range(3)]
            hts = [sbt(ph, "ht%d" % i, (128, D)) for i in range(2)]
            junk = sbt(ph, "junk", (128, D), BF16)
            ssq = sbt(ph, "ssq", (128, 1))
            rs = sbt(ph, "rs", (128, 1))
            load_mod("sp", a1b, "a1b", MA1, False, "a1b")
            load_mod("sp", b1b, "b1b", MSH1, False, "b1b")
            load_mod("sp", a1s, "a1s", MA1, True, "a1s")
            load_mod("sp", b1s, "b1s", MSH1, True, "b1s")
            def b_stage_a(ti):
                smp = ti == 32
                npart = 16 if smp else 128
                xt = xts[ti % 3]
                xn = "xt%d" % (ti % 3)
                ht = hts[ti % 2]
                hn = "ht%d" % (ti % 2)
                src = I["xs"] if smp else I["xh"][ti * 128:(ti + 1) * 128, :]
                P.dma("sp", DMA(xt[0:npart, :], src), xn, writes=[xn])
                norm_tile(xt, xn, npart, a1s if smp else a1b, "a1s" if smp else "a1b", b1s if smp else b1b,
                          "b1s" if smp else "b1b", ht, hn, junk, "junk", ssq, rs, "B")

            def b_stage_b(ti):
                smp = ti == 32
                npart = 16 if smp else 128
                xt = xts[ti % 3]
                xn = "xt%d" % (ti % 3)
                ht = hts[ti % 2]
                hn = "ht%d" % (ti % 2)
                p0, p0n = ps()
                p1, p1n = ps()
                for kc in range(8):
                    pp, ppn = (p0, p0n) if kc < 4 else (p1, p1n)
                    P.op("pe", TR(pp[:, (kc % 4) * 128:(kc % 4) * 128 + npart], ht[0:npart, kc * 128:(kc + 1) * 128],
                                  ident[0:npart, 0:npart]), reads=[hn, "ident"], writes=[ppn])
                c0 = SMP0 if smp else ti * 128
                hb, hbn, cc = hcols(c0, npart)
                for half, (pp, ppn) in enumerate(((p0, p0n), (p1, p1n))):
                    P.op("act", ACTV(hb[:, half * 4:(half + 1) * 4, cc:cc + npart],
                                     pp[:].rearrange("p (k t) -> p k t", t=128)[:, :, 0:npart], AF.Copy),
                         reads=[ppn], writes=[hbn])

            for tt_ in range(34):
                if tt_ < 33:
                    b_stage_a(tt_)
                if tt_ >= 1:
                    b_stage_b(tt_ - 1)
        P.op("pool", CP(hlast[:], hTh[:, :, HALO - 16:HALO]), reads=["hTh"], writes=["hlast"])
        P.barrier()

        if STAGE >= 2:
          with ExitStack() as ph:
            zs = sbt(ph, "zs", (16, 4608))
            sqs = sbt(ph, "sqs", (16, 3072))
            wsb = [sbt(ph, "wsb%d" % i, (128, 8, 512), BF16) for i in range(2)]
            kvt = [sbt(ph, "kvt%d" % i, (128, 1024)) for i in range(3)]
            prod = [sbt(ph, "prod%d" % i, (128, 512)) for i in range(2)]
            pvb = [sbt(ph, "pvb%d" % i, (128, 520), BF16) for i in range(2)]
            lgs = [sbt(ph, "lgs%d" % i, (128, 8)) for i in range(2)]
            ems = [sbt(ph, "ems%d" % i, (128, 8)) for i in range(2)]
            selbc = sbt(ph, "selbc", (16, 2048))
            selcol = sbt(ph, "selcol", (128, 256), BF16)
            ohs = sbt(ph, "ohs", (32, 3, 128))
            relb = sbt(ph, "relb", (32, 24))
            expbS = sbt(ph, "expbS", (128, 24))
            expb0 = sbt(ph, "expb0", (16, 24))
            g16 = sbt(ph, "g16", (16, 2, 64))
            mss = sbt(ph, "mss", (16, 48))
            l0 = sbt(ph, "l0", (16, 24))
            osum = sbt(ph, "osum", (16, 512))
            lsum = sbt(ph, "lsum", (16, 8))
            tmp5 = sbt(ph, "tmp5", (16, 512))
            P.dma("sp", DMA(selbc[:], C["selbc"]), "selbc", writes=["selbc"])
            P.dma("pool", DMA(selcol[:], C["selcol"]), "selcol", writes=["selcol"])
            P.dma("sp", DMA(ohs[:], C["ohs"].rearrange("g b i -> b g i")), "ohs", writes=["ohs"])
            P.dma("sp", DMA(relb[:], I["rel_bias"]), "relb", writes=["relb"])
            P.dma("sp", DMA(expb0[:], I["rel_bias"][0:1, :].partition_broadcast(16)), "expb0", writes=["expb0"])
            P.dma("sp", DMA(g16[:, 0, :], I["qg"].partition_broadcast(16)), "g16", writes=["g16"])
            P.dma("sp", DMA(g16[:, 1, :], I["kg"].partition_broadcast(16)), "g16", writes=["g16"])
            P.op("dve", TS(g16[:, 0, :], g16[:, 0, :], 0.125, None, ALU.mult), reads=["g16"], writes=["g16"])
            P.op("act", ACTV(expb0[:], expb0[:], AF.Exp), reads=["expb0"], writes=["expb0"])
            pt, pn = ps()
            for g in range(3):
                P.op("pe", MM(pt[:, g * 8:(g + 1) * 8], ohs[:, g, :], relb[:, g * 8:(g + 1) * 8]), reads=["ohs", "relb"], writes=[pn])
            P.op("act", ACTV(expbS[:], pt[:, 0:24], AF.Exp), reads=[pn], writes=["expbS"])
            for cb in range(9):
                w = wsb[cb % 2]
                wn = "wsb%d" % (cb % 2)
                P.dma("pool", DMA(w[:], I["w_in"][:, cb * 512:(cb + 1) * 512].rearrange("(k p) c -> p k c", p=128)), wn, writes=[wn])
                pt, pn = ps()
                for kc in range(8):
                    P.op("pe", MM(pt[0:16, :], hTo[:, kc, TOK:TOK + 16], w[:, kc, :], kc == 0, kc == 7), reads=["hTo", wn], writes=[pn])
                P.op("act", ACTV(zs[:, cb * 512:(cb + 1) * 512], pt[0:16, :], AF.Copy), reads=[pn], writes=["zs"])
            P.op("dve", TT(sqs[:], zs[:, 0:3072], zs[:, 0:3072], ALU.mult), reads=["zs"], writes=["sqs"])
            P.op("dve", lambda E: E.tensor_reduce(out=mss[:], in_=sqs[:].rearrange("p (h e) -> p h e", e=64), axis=AX.X, op=ALU.add),
                 reads=["sqs"], writes=["mss"])
            P.op("dve", TS(mss[:], mss[:], 1.0 / 64, EPS, ALU.mult, ALU.add), reads=["mss"], writes=["mss"])
            P.op("act", ACTV(mss[:], mss[:], AF.Sqrt), reads=["mss"], writes=["mss"])
            P.op("dve", RCP(mss[:], mss[:]), reads=["mss"], writes=["mss"])
            zqk = zs[:, 0:3072].rearrange("p (h e) -> p h e", e=64)
            P.op("dve", TT(zqk, zqk, mss[:].unsqueeze(2).to_broadcast([16, 48, 64]), ALU.mult), reads=["zs", "mss"], writes=["zs"])
            for qk in range(2):
                zz = zs[:, qk * 1536:(qk + 1) * 1536].rearrange("p (h e) -> p h e", e=64)
                P.op("dve", TT(zz, zz, g16[:, qk, :].unsqueeze(1).to_broadcast([16, 24, 64]), ALU.mult), reads=["zs", "g16"], writes=["zs"])
            finals.append(P.dma("sp", DMA(O["kvs"][:, :, 0:512], zs[:, 1536:3072].rearrange("p (g c) -> p g c", g=3)), "kvs_k", reads=["zs"], writes=["o_kvs_k"]))
            finals.append(P.dma("sp", DMA(O["kvs"][:, :, 512:1024], zs[:, 3072:4608].rearrange("p (g c) -> p g c", g=3)), "kvs_v", reads=["zs"], writes=["o_kvs_v"]))
            psO, psL = PS[6], PS[7]
            P.psmod = 6
            caches = (I["kv1"], I["kv2"], I["kv3"])
            sits = [(n, g) for n in range(16) for g in range(3)]
            SD = 2
            sst = {}
            for it2 in range(len(sits) + SD):
                if it2 < len(sits):
                    n, g = sits[it2]
                    kt = kvt[it2 % 3]
                    ktn = "kvt%d" % (it2 % 3)
                    P.dma("sp", DMA(kt[:], caches[g][n]), ktn, writes=[ktn])
                    pt, pn = ps()
                    P.op("pe", MM(pt[:, :], selbc[0:16, n * 128:(n + 1) * 128], zs[0:16, g * 512:(g + 1) * 512]), reads=["selbc", "zs"], writes=[pn])
                    sst[it2] = (pt, pn)
                it = it2 - SD
                if it < 0:
                    continue
                n, g = sits[it]
                pt, pn = sst.pop(it)
                kt = kvt[it % 3]
                ktn = "kvt%d" % (it % 3)
                pr = prod[it % 2]
                prn = "prod%d" % (it % 2)
                pv = pvb[it % 2]
                pvn = "pvb%d" % (it % 2)
                lg = lgs[it % 2]
                lgn = "lgs%d" % (it % 2)
                em = ems[it % 2]
                emn = "ems%d" % (it % 2)
                P.op("dve", TT(pr[:], kt[:, 0:512], pt[:, :], ALU.mult), reads=[ktn, pn], writes=[prn])
                P.op("dve", lambda E, lg=lg, pr=pr: E.tensor_reduce(out=lg[:], in_=pr[:].rearrange("p (h e) -> p h e", e=64), axis=AX.X, op=ALU.add),
                     reads=[prn], writes=[lgn])
                P.op("act", ACTV(lg[:], lg[:], AF.Exp), reads=[lgn], writes=[lgn])
                P.op("dve", TT(em[:], lg[:], expbS[:, g * 8:(g + 1) * 8], ALU.mult), reads=[lgn, "expbS"], writes=[emn])
                P.op("act", ACTV(pv[:, 512:520], em[:], AF.Copy), reads=[emn], writes=[pvn])
                P.op("dve", TT(pv[:, 0:512].rearrange("p (h e) -> p h e", e=64), kt[:, 512:1024].rearrange("p (h e) -> p h e", e=64),
                               em[:].unsqueeze(2).to_broadcast([128, 8, 64]), ALU.mult), reads=[ktn, emn], writes=[pvn])
                first, last = it == 0, it == 47
                P.op("pe", MM(psO[0:16, :], selcol[:, n * 16:(n + 1) * 16], pv[:, 0:512], first, last), reads=["selcol", pvn], writes=["psO"])
                P.op("pe", MM(psL[0:16, 0:8], selcol[:, n * 16:(n + 1) * 16], pv[:, 512:520], first, last), reads=["selcol", pvn], writes=["psL"])
            qv = zs[:, 0:1536].rearrange("p (h e) -> p h e", e=64)
            kv_ = zs[:, 1536:3072].rearrange("p (h e) -> p h e", e=64)
            sq3 = sqs[:, 0:1536].rearrange("p (h e) -> p h e", e=64)
            P.op("dve", TT(sq3, qv, kv_, ALU.mult), reads=["zs"], writes=["sqs"])
            P.op("dve", lambda E: E.tensor_reduce(out=l0[:], in_=sq3, axis=AX.X, op=ALU.add), reads=["sqs"], writes=["l0"])
            P.op("act", ACTV(l0[:], l0[:], AF.Exp), reads=["l0"], writes=["l0"])
            P.op("dve", TT(l0[:], l0[:], expb0[:], ALU.mult), reads=["l0", "expb0"], writes=["l0"])
            P.op("dve", CP(osum[:], psO[0:16, :]), reads=["psO"], writes=["osum"])
            P.op("dve", CP(lsum[:], psL[0:16, 0:8]), reads=["psL"], writes=["lsum"])
            for g in range(3):
                vg = zs[:, 3072 + g * 512:3072 + (g + 1) * 512].rearrange("p (h e) -> p h e", e=64)
                e0 = l0[:, g * 8:(g + 1) * 8]
                P.op("dve", TT(tmp5[:].rearrange("p (h e) -> p h e", e=64), vg, e0.unsqueeze(2).to_broadcast([16, 8, 64]), ALU.mult),
                     reads=["zs", "l0"], writes=["tmp5"])
                P.op("dve", TT(osum[:], osum[:], tmp5[:], ALU.add), reads=["tmp5", "osum"], writes=["osum"])
                P.op("dve", TT(lsum[:], lsum[:], e0, ALU.add), reads=["l0", "lsum"], writes=["lsum"])
            P.op("dve", RCP(lsum[:], lsum[:]), reads=["lsum"], writes=["lsum"])
            o3 = osum[:].rearrange("p (h e) -> p h e", e=64)
            P.op("dve", TT(o3, o3, lsum[:].unsqueeze(2).to_broadcast([16, 8, 64]), ALU.mult), reads=["osum", "lsum"], writes=["osum"])
            pt, pn = ps()
            for c in range(4):
                P.op("pe", TR(pt[:, c * 16:(c + 1) * 16], osum[0:16, c * 128:(c + 1) * 128], ident[0:16, 0:16]), reads=["osum", "ident"], writes=[pn])
            P.op("act", ACTV(aoT[:, :, TOK:TOK + 16], pt[:, 0:64].rearrange("p (c t) -> p c t", t=16), AF.Copy), reads=[pn], writes=["aoT_s"])
            P.psmod = 8
          P.barrier()

        if STAGE >= 3:
          with ExitStack() as ph0:
            relb = sbt(ph0, "relb2", (32, 24))
            ohcp = sbt(ph0, "ohcp", (32, 6, 256))
            valt = sbt(ph0, "valt", (8, 2, 256))
            evs = sbt(ph0, "evs", (8, 6, 256))
            P.dma("sp", DMA(relb[:], I["rel_bias"]), "relb2", writes=["relb2"])
            P.dma("sp", DMA(ohcp[:, 0:3, :], C["ohp"].rearrange("g b m -> b g m")), "ohcp", writes=["ohcp"])
            P.dma("sp", DMA(ohcp[:, 3:6, :], C["ohc"].rearrange("g b m -> b g m")), "ohcp", writes=["ohcp"])
            P.dma("sp", DMA(valt[:], C["valid"].rearrange("c h m -> h c m")), "valt", writes=["valt"])
            for g in range(3):
                for cp_ in range(2):
                    pt, pn = ps()
                    P.op("pe", MM(pt[0:8, 0:256], relb[:, g * 8:(g + 1) * 8], ohcp[:, cp_ * 3 + g, :]), reads=["relb2", "ohcp"], writes=[pn])
                    P.op("act", ACTV(evs[:, g * 2 + cp_, :], pt[0:8, 0:256], AF.Exp), reads=[pn], writes=["evs"])
                    P.op("dve", TT(evs[:, g * 2 + cp_, :], evs[:, g * 2 + cp_, :], valt[:, cp_, :], ALU.mult), reads=["evs", "valt"], writes=["evs"])
            P.dma("sp", DMA(evd.rearrange("g c h m -> h (g c) m"), evs[:]), "evd", reads=["evs"], writes=["evd"])
          P.barrier()
        if STAGE >= 3:
          with ExitStack() as ph:
            accE = sbt(ph, "accE", (65, 2, TOK))
            accO = sbt(ph, "accO", (128, 2, TOK))
            qT = sbt(ph, "qT", (128, 2, TOK), BF16)
            kT = sbt(ph, "kT", (128, 2, HALO + TOK), BF16)
            VbE = sbt(ph, "VbE", (128, 32, 2, 66), BF16)
            VbO = sbt(ph, "VbO", (128, 32, 2, 128), BF16)
            wq = sbt(ph, "wq", (128, 8, 256), BF16)
            wk = sbt(ph, "wk", (128, 8, 256), BF16)
            wv = sbt(ph, "wv", (128, 8, 256), BF16)
            mtab = sbt(ph, "mtab", (128, 4, 4, 128))
            Ef = [sbt(ph, "Ef%d" % i, (128, 256)) for i in range(2)]
            Eb = [sbt(ph, "Eb%d" % i, (128, 256), BF16) for i in range(2)]
            knf = [sbt(ph, "knf%d" % i, (128, 512)) for i in range(2)]
            sqb = [sbt(ph, "sqb%d" % i, (128, 512), BF16) for i in range(2)]
            rt = [sbt(ph, "rt%d" % i, (128, 512)) for i in range(2)]
            vst = [sbt(ph, "vst%d" % i, (128, 256)) for i in range(2)]
            kst = [sbt(ph, "kst%d" % i, (128, 128)) for i in range(2)]
            rl = [sbt(ph, "rl%d" % i, (128, 512)) for i in range(2)]
            P.op("pool", MSET(VbE[:, :, :, 64:65], 1.0), writes=["VbE"])
            P.op("pool", MSET(VbO[:, :, :, 0:64], 0.0), writes=["VbO"])
            P.op("pool", MSET(VbO[:, :, :, 0:1], 1.0), writes=["VbO"])
            kvouts = (O["kvp1"], O["kvp2"], O["kvp3"])
            cnt = {"p": 0, "e": 0, "s": 0, "hk": 0, "rl": 0, "pS": 0, "pO": 0, "pp": 0, "pv": 0}

            def psp():
                i_ = cnt["pp"] % 6
                cnt["pp"] += 1
                return PS[i_], "ps%d" % i_

            def psv():
                i_ = 6 + cnt["pv"] % 2
                cnt["pv"] += 1
                return PS[i_], "ps%d" % i_

            for hh in range(2 if CL >= 2 else 0):
                for g in range(3):
                    if CONE and (hh, g) != (0, CONE - 1):
                        continue
                    dil = DILS[g]
                    halo_g = 128 * dil
                    keep = halo_g
                    nb = 16 // dil
                    colq = g * 512 + hh * 256
                    for (wt, wnm, c0) in ((wq, "wq", colq), (wk, "wk", 1536 + colq), (wv, "wv", 3072 + colq)):
                        P.dma("pool", DMA(wt[:], I["w_in"][:, c0:c0 + 256].rearrange("(k p) c -> p k c", p=128)), wnm, writes=[wnm])
                    for hl in range(4):
                        for cp_ in range(2):
                            mi_ = hl * 2 + cp_
                            hkt = rl[mi_ // 4][:, (mi_ % 4) * 128:(mi_ % 4 + 1) * 128]
                            base = evd[g, cp_, hh * 4 + hl, 0:128]
                            hank = bass.AP(tensor=base.tensor, offset=base.offset, ap=[[1, 128], [1, 128]])
                            P.dma("sp", DMA(hkt, hank), "hk%d" % mi_, reads=["evd"], writes=["rl%d" % (mi_ // 4)])
                    if CL < 3:
                        continue
                    def emit_vblock(r, n):
                        bi = r * (nb + 1) + (n + 1)
                        c0 = OWN0 + n * 128 * dil + r
                        hb, hbn, cc = hcols(c0, 128, dil)
                        pv_, pvn_ = psv()
                        for kc in range(8):
                            P.op("pe", MM(pv_[:, 0:256], hb[:, kc, cc:cc + 127 * dil + 1:dil], wv[:, kc, :], kc == 0, kc == 7),
                                 reads=[hbn, "wv"], writes=[pvn_])
                        pv4 = pv_[:, 0:256].rearrange("p (h e) -> p h e", e=64)
                        if VL >= 2:
                            P.op("act", ACTV(VbE[:, bi, :, 0:64], pv4[:, 0:4:2, :], AF.Copy), reads=[pvn_], writes=["VbE"])
                        if VL >= 3:
                            P.op("act", ACTV(VbO[:, bi, :, 64:128], pv4[:, 1:4:2, :], AF.Copy), reads=[pvn_], writes=["VbO"])
                        trel = n * 128 * dil
                        if VL >= 4 and n >= 0 and trel >= TOK - keep:
                            vs_i = cnt["s"] % 2
                            cnt["s"] += 1
                            P.op("dve", CP(vst[vs_i][:], pv_[:, 0:256]), reads=[pvn_], writes=["vst%d" % vs_i])
                            r0 = trel - (TOK - keep) + r
                            dst = kvouts[g].rearrange("(j r) c -> r j c", r=dil)[r, r0 // dil:r0 // dil + 128, 512 + hh * 256:512 + (hh + 1) * 256]
                            finals.append(P.dma("sp", DMA(dst, vst[vs_i][:]), "vst%d" % vs_i, reads=["vst%d" % vs_i], writes=["o_v"]))
                    vlist = [(r, n) for r in range(dil) for n in range(-1, nb)]
                    pj = []
                    for isk in range(2):
                        c_lo = OWN0 - halo_g if isk else OWN0
                        blocks = [(cb, min(512, OWN0 + TOK - cb)) for cb in range(c_lo, OWN0 + TOK, 512)]
                        if isk and halo_g < 512:
                            blocks = [(c_lo, halo_g)] + [(cb, 512) for cb in range(OWN0, OWN0 + TOK, 512)]
                        for c in range(2):
                            for (cb, nn) in blocks:
                                pj.append((isk, c, cb, nn))
                    stp = {}
                    for i in range(len(pj) + 1):
                        if i < len(pj):
                            isk, c, cb, nn = pj[i]
                            wt, wnm = (wk, "wk") if isk else (wq, "wq")
                            hb, hbn, cc = hcols(cb, nn)
                            i2 = cnt["p"] % 2
                            cnt["p"] += 1
                            pz, pzn = psp()
                            for kc in range(8):
                                P.op("pe", MM(pz[:, 0:nn], wt[:, kc, c * 128:(c + 1) * 128], hb[:, kc, cc:cc + nn], kc == 0, kc == 7),
                                     reads=[wnm, hbn], writes=[pzn])
                            P.op("act", ACTV(sqb[i2][:, 0:nn], pz[:, 0:nn], AF.Square), reads=[pzn], writes=["sqb%d" % i2])
                            stp[i] = (pz, pzn, i2)
                            if vlist and CL >= 4:
                                emit_vblock(*vlist.pop(0))
                        j = i - 1
                        if j < 0:
                            continue
                        isk, c, cb, nn = pj[j]
                        pz, pzn, i2 = stp.pop(j)
                        gcol = gk if isk else gq
                        gn = "gk" if isk else "gq"
                        pm, pmn = psp()
                        P.op("pe", MM(pm[:, 0:nn], bones_b[:], sqb[i2][:, 0:nn]), reads=["bones_b", "sqb%d" % i2], writes=[pmn])
                        P.op("act", ACTV(rt[i2][:, 0:nn], pm[:, 0:nn], AF.Ln, bias=epst[:, 0:1]), reads=[pmn, "epst"], writes=["rt%d" % i2])
                        P.op("act", ACTV(rt[i2][:, 0:nn], rt[i2][:, 0:nn], AF.Exp, scale=-0.5), reads=["rt%d" % i2], writes=["rt%d" % i2])
                        if not isk:
                            P.op("dve", STT(qT[:, c, cb - OWN0:cb - OWN0 + nn], pz[:, 0:nn], gcol[:, 0:1], rt[i2][:, 0:nn], ALU.mult, ALU.mult),
                                 reads=[pzn, gn, "rt%d" % i2], writes=["qT"])
                        else:
                            P.op("dve", STT(knf[i2][:, 0:nn], pz[:, 0:nn], gcol[:, 0:1], rt[i2][:, 0:nn], ALU.mult, ALU.mult),
                                 reads=[pzn, gn, "rt%d" % i2], writes=["knf%d" % i2])
                            P.op("act", ACTV(kT[:, c, cb:cb + nn], knf[i2][:, 0:nn], AF.Copy), reads=["knf%d" % i2], writes=["kT"])
                            pk, pkn = None, None
                            for t0 in range(cb, cb + nn, 128):
                                trel = t0 - OWN0
                                if trel < TOK - keep or trel < 0:
                                    continue
                                ks_i = cnt["s"] % 2
                                cnt["s"] += 1
                                if pk is None:
                                    pk, pkn = psp()
                                qo = ((t0 - cb) // 128) * 128
                                P.op("pe", TR(pk[:, qo:qo + 128], knf[i2][:, t0 - cb:t0 - cb + 128], ident[:]), reads=["knf%d" % i2, "ident"], writes=[pkn])
                                P.op("act", ACTV(kst[ks_i][:], pk[:, qo:qo + 128], AF.Copy), reads=[pkn], writes=["kst%d" % ks_i])
                                r0 = trel - (TOK - keep)
                                finals.append(P.dma("sp", DMA(kvouts[g][r0:r0 + 128, hh * 256 + c * 128:hh * 256 + (c + 1) * 128], kst[ks_i][:]),
                                                    "kst%d" % ks_i, reads=["kst%d" % ks_i], writes=["o_k"]))
                    while vlist:
                        emit_vblock(*vlist.pop(0))
                    if CL < 5:
                        continue
                    for hl in range(4):
                        for cp_ in range(2):
                            mi_ = hl * 2 + cp_
                            hkt = rl[mi_ // 4][:, (mi_ % 4) * 128:(mi_ % 4 + 1) * 128]
                            pt, pn = ps()
                            P.op("pe", MM(pt[:, 0:128], jflip[:], hkt), reads=["jflip", "rl%d" % (mi_ // 4)], writes=[pn])
                            P.op("act", ACTV(mtab[:, hl, cp_, :], pt[:, 0:128], AF.Copy), reads=[pn], writes=["mtab"])
                            if cp_ == 0:
                                P.op("dve", TS(mtab[:, hl, 2, :], pt[:, 0:128], hpt[:, 0:1], None, ALU.mult), reads=[pn, "hpt"], writes=["mtab"])
                            else:
                                P.op("dve", CP(mtab[:, hl, 3, :], pt[:, 0:128]), reads=[pn], writes=["mtab"])
                    its = [(r, n, hl) for r in range(dil) for n in range(nb) for hl in range(4)]
                    DEPTH = 3
                    stg = {}
                    for i in range(len(its) + DEPTH):
                        if i < len(its):
                            r, n, hl = its[i]
                            q0 = n * 128 * dil + r
                            kc0 = OWN0 + q0
                            kp0 = kc0 - 128 * dil
                            c = hl // 2
                            pb = (hl % 2) * 64
                            pS, pSn = PS[cnt["pS"] % 5], "ps%d" % (cnt["pS"] % 5)
                            cnt["pS"] += 1
                            qsl = qT[pb:pb + 64, c, q0:q0 + 127 * dil + 1:dil]
                            P.op("pe", MM(pS[:, 0:128], kT[pb:pb + 64, c, kp0:kp0 + 127 * dil + 1:dil], qsl), reads=["kT", "qT"], writes=[pSn])
                            P.op("pe", MM(pS[:, 128:256], kT[pb:pb + 64, c, kc0:kc0 + 127 * dil + 1:dil], qsl), reads=["kT", "qT"], writes=[pSn])
                            stg[i] = (pS, pSn)
                        j = i - DEPTH
                        if j < 0:
                            continue
                        r, n, hl = its[j]
                        pS, pSn = stg.pop(j)
                        bprev = r * (nb + 1) + n
                        bcur = bprev + 1
                        q0 = n * 128 * dil + r
                        ei = cnt["e"] % 2
                        cnt["e"] += 1
                        P.op("act", ACTV(Ef[ei][:], pS[:, 0:256], AF.Exp), reads=[pSn], writes=["Ef%d" % ei])
                        mi = 2 if n == 0 else 0
                        P.op("dve", TT(Eb[ei][:].rearrange("p (a k) -> p a k", a=2), Ef[ei][:].rearrange("p (a k) -> p a k", a=2),
                                       mtab[:, hl, mi:mi + 2, :], ALU.mult), reads=["Ef%d" % ei, "mtab"], writes=["Eb%d" % ei])
                        pO, pOn = PS[5 + cnt["pO"] % 3], "ps%d" % (5 + cnt["pO"] % 3)
                        cnt["pO"] += 1
                        if hl % 2 == 0:
                            P.op("pe", MM(pO[0:65, 0:128], VbE[:, bprev, hl // 2, 0:65], Eb[ei][:, 0:128], True, False), reads=["VbE", "Eb%d" % ei], writes=[pOn])
                            P.op("pe", MM(pO[0:65, 0:128], VbE[:, bcur, hl // 2, 0:65], Eb[ei][:, 128:256], False, True), reads=["VbE", "Eb%d" % ei], writes=[pOn])
                            av = accE[0:65, hl // 2, q0:q0 + 127 * dil + 1:dil]
                            src = pO[0:65, 0:128]
                        else:
                            P.op("pe", MM(pO[:, 0:128], VbO[:, bprev, hl // 2, :], Eb[ei][:, 0:128], True, False), reads=["VbO", "Eb%d" % ei], writes=[pOn])
                            P.op("pe", MM(pO[:, 0:128], VbO[:, bcur, hl // 2, :], Eb[ei][:, 128:256], False, True), reads=["VbO", "Eb%d" % ei], writes=[pOn])
                            av = accO[:, hl // 2, q0:q0 + 127 * dil + 1:dil]
                            src = pO[:, 0:128]
                        an = "acc_%d_%d_%d_%d" % (g, hl, r, n)
                        if g == 0:
                            cnt["fence"] = P.op("dve", CP(av, src), reads=[pOn], writes=[an], extra=cnt.get("prevfence", []))
                        else:
                            cnt["fence"] = P.op("dve", TT(av, src, av, ALU.add), reads=[pOn], writes=[an], extra=cnt.get("prevfence", []))
                    cnt["prevfence"] = [cnt["fence"]]
                for hl in range(4 if CL >= 6 else 0):
                    pass
                for hl in range(4 if CL >= 6 else 0):
                    for tb in range(4):
                        ri = cnt["rl"] % 2
                        cnt["rl"] += 1
                        pt, pn = ps()
                        cs_ = slice(tb * 512, (tb + 1) * 512)
                        if hl % 2 == 0:
                            P.op("pe", MM(pt[0:64, :], ones_f[64:65, 0:64], accE[64:65, hl // 2, cs_]), reads=["ones_f", "accE"], writes=[pn], extra=cnt.get("prevfence", []))
                            P.op("act", ACTV(rl[ri][0:64, :], pt[0:64, :], AF.Ln), reads=[pn], writes=["rl%d" % ri])
                            P.op("act", ACTV(rl[ri][0:64, :], rl[ri][0:64, :], AF.Exp, scale=-1.0), reads=["rl%d" % ri], writes=["rl%d" % ri])
                            cnt["finfence"] = P.op("dve", TT(aoT[0:64, hh * 2 + hl // 2, cs_], accE[0:64, hl // 2, cs_], rl[ri][0:64, :], ALU.mult),
                                 reads=["accE", "rl%d" % ri], writes=["aoT"], extra=cnt.get("prevfence", []))
                        else:
                            P.op("pe", MM(pt[:, :], zo_f[0:1, :], accO[0:1, hl // 2, cs_]), reads=["zo_f", "accO"], writes=[pn], extra=cnt.get("prevfence", []))
                            P.op("act", ACTV(rl[ri][64:128, :], pt[64:128, :], AF.Ln), reads=[pn], writes=["rl%d" % ri])
                            P.op("act", ACTV(rl[ri][64:128, :], rl[ri][64:128, :], AF.Exp, scale=-1.0), reads=["rl%d" % ri], writes=["rl%d" % ri])
                            cnt["finfence"] = P.op("dve", TT(aoT[64:128, hh * 2 + hl // 2, cs_], accO[64:128, hl // 2, cs_], rl[ri][64:128, :], ALU.mult),
                                 reads=["accO", "rl%d" % ri], writes=["aoT"], extra=cnt.get("prevfence", []))
                if "finfence" in cnt:
                    cnt["prevfence"] = [cnt["finfence"]]
          P.barrier()
        sc_h.close()
        poT = sbt(sc_o, "poT", (128, 4, NTOK), BF16)

        BIG = 1.0e6
        TB = [(0, 512), (512, 512), (1024, 512), (1536, 512), (2048, 16)]
        if STAGE >= 4:
          with ExitStack() as ph:
            uT = sbt(ph, "uT", (128, 4, 16 + TOK))
            uTs = sbt(ph, "uTs", (128, 4, 16))
            sA = sbt(ph, "sA", (128, 16 + TOK))
            sB = sbt(ph, "sB", (128, 16 + TOK))
            pl = sbt(ph, "pl", (128, 4, NTOK), BF16)
            wu = sbt(ph, "wu", (128, 8, 512), BF16)
            wpm = sbt(ph, "wpm", (128, 4, 128), BF16)
            pscale = sbt(ph, "pscale", (128, 4))
            corr = sbt(ph, "corr", (128, 4, 16))
            tmpc = sbt(ph, "tmpc", (128, 16))
            spb = sbt(ph, "spb", (16, 26, 128))
            unew = sbt(ph, "unew", (16, 512))
            ulast = sbt(ph, "ulast", (16, 512))
            ssum = sbt(ph, "ssum", (16, 128))
            pls = sbt(ph, "pls", (16, 512))
            P.dma("pool", DMA(wu[:], I["w_in"][:, 4608:5120].rearrange("(k p) c -> p k c", p=128)), "wu", writes=["wu"])
            P.dma("pool", DMA(wpm[:], I["w_pool_mix"].rearrange("g c d -> c g d")), "wpm", writes=["wpm"])
            P.dma("sp", DMA(pscale[:], I["pool_scale"].rearrange("o (g p) -> p (o g)", p=128), allow_slow_non_contiguous=True), "pscale", writes=["pscale"])
            P.dma("sp", DMA(corr[:], C["poolcorr"].rearrange("p (g t) -> p g t", t=16)), "corr", writes=["corr"])
            r0 = 0
            sp_off = []
            for gi, w in enumerate((2, 4, 8, 16)):
                P.dma("sp", DMA(spb[:, r0:r0 + w - 1, :], I["spool"][:, 15 - (w - 1):15, gi * 128:(gi + 1) * 128]), "spb", writes=["spb"])
                sp_off.append(r0)
                r0 += w - 1
            finals.append(P.dma("sp", DMA(O["pools"][:, 0:14, :], I["spool"][:, 1:15, :]), "pools_cp", writes=["o_pools_a"]))
            for gi in range(4):
                for (c0, nn, dst) in [(OWN0 - 16, 16, uT[:, gi, 0:16])] + [(OWN0 + tb * 512, 512, uT[:, gi, 16 + tb * 512:16 + (tb + 1) * 512]) for tb in range(4)] + [(SMP0, 16, uTs[:, gi, :])]:
                    hb, hbn, cc = hcols(c0, nn) if c0 >= OWN0 else (None, None, None)
                    pt, pn = ps()
                    if c0 < OWN0:
                        hb, hbn, cc = hlast, "hlast", 0
                    for kc in range(8):
                        P.op("pe", MM(pt[:, 0:nn], wu[:, kc, gi * 128:(gi + 1) * 128], hb[:, kc, cc:cc + nn], kc == 0, kc == 7), reads=["wu", hbn], writes=[pn])
                    if c0 < OWN0:
                        P.op("dve", TS(dst, pt[:, 0:nn], hpt[:, 0:1], None, ALU.mult), reads=[pn, "hpt"], writes=["uT"])
                    else:
                        P.op("act", ACTV(dst, pt[:, 0:nn], AF.Copy), reads=[pn], writes=["uT" if c0 < SMP0 else "uTs"])
            for (srcT, sn, dstt, dn) in ((uT, "uT", ulast, "ulast"), (uTs, "uTs", unew, "unew")):
                pt, pn = ps()
                for gi in range(4):
                    sl = srcT[:, gi, 16 + TOK - 16:16 + TOK] if sn == "uT" else srcT[:, gi, :]
                    P.op("pe", TR(pt[0:16, gi * 128:(gi + 1) * 128], sl, ident[:]), reads=[sn, "ident"], writes=[pn])
                P.op("act", ACTV(dstt[:], pt[0:16, :], AF.Copy), reads=[pn], writes=[dn])
            finals.append(P.dma("sp", DMA(O["poolp"], ulast[:]), "poolp", reads=["ulast"], writes=["o_poolp"]))
            finals.append(P.dma("sp", DMA(O["pools"][:, 14, :], unew[:]), "pools_n", reads=["unew"], writes=["o_pools_b"]))
            for gi, w in enumerate((2, 4, 8, 16)):
                cur, curn = uT[:, gi, :], "uT"
                bufs = [(sA, "sA"), (sB, "sB")]
                lo = 0
                for step in range(gi + 1):
                    sh = 1 << step
                    dstb, dstn = bufs[step % 2]
                    P.op("dve", TT(dstb[:, lo + sh:16 + TOK], cur[:, lo + sh:16 + TOK], cur[:, lo:16 + TOK - sh], ALU.add),
                         reads=[curn], writes=[dstn])
                    cur, curn = dstb[:, :], dstn
                    lo += sh
                P.op("dve", STT(pl[:, gi, 0:TOK], cur[:, 16:16 + TOK], 1.0 / w, uT[:, gi, 16:16 + TOK], ALU.mult, ALU.subtract),
                     reads=[curn, "uT"], writes=["pl"])
                P.op("dve", TT(tmpc[:], cur[:, 16:32], corr[:, gi, :], ALU.mult), reads=[curn, "corr"], writes=["tmpc"])
                P.op("dve", TT(pl[:, gi, 0:16], tmpc[:], uT[:, gi, 16:32], ALU.subtract), reads=["tmpc", "uT"], writes=["pl"])
                P.op("dve", lambda E, gi=gi, w=w: E.tensor_reduce(out=ssum[:], in_=spb[:, sp_off[gi]:sp_off[gi] + w - 1, :].rearrange("p r c -> p c r"),
                                                                 axis=AX.X, op=ALU.add), reads=["spb"], writes=["ssum"])
                P.op("dve", TT(ssum[:], ssum[:], unew[:, gi * 128:(gi + 1) * 128], ALU.add), reads=["ssum", "unew"], writes=["ssum"])
                P.op("dve", STT(pls[:, gi * 128:(gi + 1) * 128], ssum[:], 1.0 / w, unew[:, gi * 128:(gi + 1) * 128], ALU.mult, ALU.subtract),
                     reads=["ssum", "unew"], writes=["pls"])
            pt, pn = ps()
            for gi in range(4):
                P.op("pe", TR(pt[:, gi * 16:(gi + 1) * 16], pls[0:16, gi * 128:(gi + 1) * 128], ident[0:16, 0:16]), reads=["pls", "ident"], writes=[pn])
            P.op("act", ACTV(pl[:, :, TOK:TOK + 16], pt[:, 0:64].rearrange("p (c t) -> p c t", t=16), AF.Copy), reads=[pn], writes=["pl"])
            for gi in range(4):
                for (c0, nn) in TB:
                    pt, pn = ps()
                    P.op("pe", MM(pt[:, 0:nn], wpm[:, gi, :], pl[:, gi, c0:c0 + nn]), reads=["wpm", "pl"], writes=[pn])
                    P.op("act", ACTV(poT[:, gi, c0:c0 + nn], pt[:, 0:nn], AF.Copy, scale=pscale[:, gi:gi + 1]), reads=[pn, "pscale"], writes=["poT"])
          P.barrier()

        if STAGE >= 4:
          mgT = sbt(sc_o, "mgT", (128, 8, NTOK), BF16)
          with ExitStack() as ph:
            wua = sbt(ph, "wua", (128, 4, D), BF16)
            wup = sbt(ph, "wup", (128, 4, D), BF16)
            wga = sbt(ph, "wga", (128, 8, D), BF16)
            wgp = sbt(ph, "wgp", (128, 8, D), BF16)
            sa = [sbt(ph, "sa%d" % i, (128, 512)) for i in range(2)]
            sp_ = [sbt(ph, "sp_%d" % i, (128, 512)) for i in range(2)]
            m1 = [sbt(ph, "m1%d" % i, (128, 512)) for i in range(2)]
            m2 = [sbt(ph, "m2%d" % i, (128, 512)) for i in range(2)]
            P.dma("pool", DMA(wua[:], I["w_up_attn"].rearrange("(k p) c -> p k c", p=128)), "wua", writes=["wua"])
            P.dma("pool", DMA(wup[:], I["w_up_pool"].rearrange("(k p) c -> p k c", p=128)), "wup", writes=["wup"])
            P.dma("pool", DMA(wga[:], I["w_in"][:, 5120:6144].rearrange("(k p) c -> p k c", p=128)), "wga", writes=["wga"])
            P.dma("pool", DMA(wgp[:], I["w_in"][:, 6144:7168].rearrange("(k p) c -> p k c", p=128)), "wgp", writes=["wgp"])
            it = 0
            for (c0, nn) in TB:
                for j in range(8):
                    i2 = it % 2
                    it += 1
                    js = slice(j * 128, (j + 1) * 128)
                    pA, pAn = ps()
                    pB, pBn = ps()
                    pGa, pGan = ps()
                    pGp, pGpn = ps()
                    for kc in range(4):
                        P.op("pe", MM(pA[:, 0:nn], wua[:, kc, js], aoT[:, kc, c0:c0 + nn], kc == 0, kc == 3), reads=["wua", "aoT", "aoT_s"], writes=[pAn])
                    for kc in range(4):
                        P.op("pe", MM(pB[:, 0:nn], wup[:, kc, js], poT[:, kc, c0:c0 + nn], kc == 0, kc == 3), reads=["wup", "poT"], writes=[pBn])
                    for kc in range(8):
                        P.op("pe", MM(pGa[:, 0:nn], wga[:, kc, js], hTo[:, kc, c0:c0 + nn], kc == 0, kc == 7), reads=["wga", "hTo"], writes=[pGan])
                    for kc in range(8):
                        P.op("pe", MM(pGp[:, 0:nn], wgp[:, kc, js], hTo[:, kc, c0:c0 + nn], kc == 0, kc == 7), reads=["wgp", "hTo"], writes=[pGpn])
                    P.op("act", ACTV(sa[i2][:, 0:nn], pGa[:, 0:nn], AF.Sigmoid), reads=[pGan], writes=["sa%d" % i2])
                    P.op("act", ACTV(sp_[i2][:, 0:nn], pGp[:, 0:nn], AF.Sigmoid), reads=[pGpn], writes=["sp_%d" % i2])
                    P.op("dve", TT(m1[i2][:, 0:nn], pA[:, 0:nn], sa[i2][:, 0:nn], ALU.mult), reads=[pAn, "sa%d" % i2], writes=["m1%d" % i2])
                    P.op("dve", TT(m2[i2][:, 0:nn], pB[:, 0:nn], sp_[i2][:, 0:nn], ALU.mult), reads=[pBn, "sp_%d" % i2], writes=["m2%d" % i2])
                    P.op("dve", TT(mgT[:, j, c0:c0 + nn], m1[i2][:, 0:nn], m2[i2][:, 0:nn], ALU.add), reads=["m1%d" % i2, "m2%d" % i2], writes=["mgT"])
          P.barrier()

        if STAGE >= 4:
          with ExitStack() as ph:
            wo = sbt(ph, "wo", (128, 8, D), BF16)
            wr = sbt(ph, "wr", (128, 8, 32))
            brb = sbt(ph, "brb", (128, 32))
            iot = sbt(ph, "iot", (128, 32))
            gt1b = sbt(ph, "gt1b", (128, D))
            a2b = sbt(ph, "a2b", (128, D))
            b2b = sbt(ph, "b2b", (128, D))
            gt1s = sbt(ph, "gt1s", (16, D))
            a2s = sbt(ph, "a2s", (16, D))
            b2s = sbt(ph, "b2s", (16, D))
            xts = [sbt(ph, "xd%d" % i, (128, D)) for i in range(2)]
            x1t = [sbt(ph, "x1t%d" % i, (128, D)) for i in range(2)]
            h2t = [sbt(ph, "h2t%d" % i, (128, D)) for i in range(2)]
            h2b = [sbt(ph, "h2b%d" % i, (128, D), BF16) for i in range(2)]
            junk = sbt(ph, "junk2", (128, D), BF16)
            h2T = sbt(ph, "h2T", (128, 8, 128))
            ssq = sbt(ph, "ssq2", (128, 1))
            rs = sbt(ph, "rs2", (128, 1))
            carry = sbt(ph, "carry", (128, 32))
            lg = sbt(ph, "lg", (128, 32))
            t8 = sbt(ph, "t8", (128, 8))
            mk = sbt(ph, "mk", (128, 32))
            mkb = sbt(ph, "mkb", (128, 32), BF16)
            nm = sbt(ph, "nm", (128, 1))
            ex = sbt(ph, "ex", (128, 32))
            den = sbt(ph, "den", (128, 1))
            G = sbt(ph, "G", (128, 32))
            posf = sbt(ph, "posf", (128, 32))
            v2 = sbt(ph, "v2", (128, 32))
            rowsel = sbt(ph, "rowsel", (128, 32))
            ohk = sbt(ph, "ohk", (128, 32))
            ohk2 = sbt(ph, "ohk2", (128, 32))
            posTs = sbt(ph, "posTs", (32, 128))
            P.dma("pool", DMA(wo[:], I["w_out"].rearrange("(k p) c -> p k c", p=128)), "wo", writes=["wo"])
            P.dma("sp", DMA(wr[:], I["w_router"].rearrange("(k p) c -> p k c", p=128)), "wr", writes=["wr"])
            P.dma("sp", DMA(brb[:], I["b_router"].partition_broadcast(128)), "brb", writes=["brb"])
            P.dma("sp", DMA(iot[:], C["iota32"]), "iot", writes=["iot"])
            load_mod("sp", gt1b, "gt1b", MGT1, False, "gt1b")
            load_mod("sp", a2b, "a2b", MA2, False, "a2b")
            load_mod("sp", b2b, "b2b", MSH2, False, "b2b")
            load_mod("sp", gt1s, "gt1s", MGT1, True, "gt1s")
            load_mod("sp", a2s, "a2s", MA2, True, "a2s")
            load_mod("sp", b2s, "b2s", MSH2, True, "b2s")
            P.op("dve", MSET(carry[:], 0.0), writes=["carry"])
            def dc_stage_a(ti):
                smp = ti == 16
                npart = 16 if smp else 128
                c0 = TOK if smp else ti * 128
                i2 = ti % 2
                xt, xn = xts[i2], "xd%d" % i2
                x1, x1n = x1t[i2], "x1t%d" % i2
                h2, h2n = h2t[i2], "h2t%d" % i2
                hb_, hbn_ = h2b[i2], "h2b%d" % i2
                P.dma("sp", DMA(xt[0:npart, :], I["xs"] if smp else I["xh"][HALO + ti * 128:HALO + (ti + 1) * 128, :]), xn, writes=[xn])
                gt, gtn = (gt1s, "gt1s") if smp else (gt1b, "gt1b")
                for half in range(2):
                    hs = slice(half * 512, (half + 1) * 512)
                    pt, pn = ps()
                    for kc in range(8):
                        P.op("pe", MM(pt[0:npart, :], mgT[:, kc, c0:c0 + npart], wo[:, kc, hs], kc == 0, kc == 7), reads=["mgT", "wo"], writes=[pn])
                    P.op("dve", TT(x1[0:npart, hs], pt[0:npart, :], gt[0:npart, hs], ALU.mult), reads=[pn, gtn], writes=[x1n])
                P.op("dve", TT(x1[0:npart, :], x1[0:npart, :], xt[0:npart, :], ALU.add), reads=[x1n, xn], writes=[x1n])
                P.dma("sp", DMA(x1d[c0:c0 + npart, :], x1[0:npart, :]), "x1d_%d" % i2, reads=[x1n], writes=["x1d"])
                norm_tile(x1, x1n, npart, a2s if smp else a2b, "a2s" if smp else "a2b", b2s if smp else b2b, "b2s" if smp else "b2b",
                          h2, h2n, junk, "junk2", ssq, rs, "D")
                P.op("act", ACTV(hb_[0:npart, :], h2[0:npart, :], AF.Copy), reads=[h2n], writes=[hbn_])

            def dc_stage_b(ti):
                smp = ti == 16
                npart = 16 if smp else 128
                c0 = TOK if smp else ti * 128
                i2 = ti % 2
                xt, xn = xts[i2], "xd%d" % i2
                x1, x1n = x1t[i2], "x1t%d" % i2
                h2, h2n = h2t[i2], "h2t%d" % i2
                hb_, hbn_ = h2b[i2], "h2b%d" % i2
                p0, p0n = ps()
                p1, p1n = ps()
                for kc in range(8):
                    pp, ppn = (p0, p0n) if kc < 4 else (p1, p1n)
                    P.op("pe", TR(pp[:, (kc % 4) * 128:(kc % 4) * 128 + npart], h2[0:npart, kc * 128:(kc + 1) * 128], ident[0:npart, 0:npart]),
                         reads=[h2n, "ident"], writes=[ppn])
                for half, (pp, ppn) in enumerate(((p0, p0n), (p1, p1n))):
                    P.op("dve", CP(h2T[:, half * 4:(half + 1) * 4, 0:npart], pp[:].rearrange("p (k t) -> p k t", t=128)[:, :, 0:npart]),
                         reads=[ppn], writes=["h2T"])
                pr_, prn = ps()
                for kc in range(8):
                    P.op("pe", MM(pr_[0:npart, 0:32], h2T[:, kc, 0:npart], wr[:, kc, :], kc == 0, kc == 7), reads=["h2T", "wr"], writes=[prn])
                NP = slice(0, npart)
                P.op("dve", TT(lg[NP, :], pr_[NP, 0:32], brb[NP, :], ALU.add), reads=[prn, "brb"], writes=["lg"])
                P.op("dve", lambda E, NP=NP: E.max(out=t8[NP, :], in_=lg[NP, :]), reads=["lg"], writes=["t8"])
                P.op("dve", TS(mk[NP, :], lg[NP, :], t8[NP, 3:4], None, ALU.is_ge), reads=["lg", "t8"], writes=["mk"])
                P.op("dve", TS(mkb[NP, :], lg[NP, :], t8[NP, 3:4], None, ALU.is_ge), reads=["lg", "t8"], writes=["mkb"])
                P.op("dve", TS(nm[NP, :], t8[NP, 0:1], -1.0, None, ALU.mult), reads=["t8"], writes=["nm"])
                P.op("act", ACTV(ex[NP, :], lg[NP, :], AF.Exp, bias=nm[NP, 0:1]), reads=["lg", "nm"], writes=["ex"])
                P.op("dve", TT(ex[NP, :], ex[NP, :], mk[NP, :], ALU.mult), reads=["ex", "mk"], writes=["ex"])
                P.op("dve", lambda E, NP=NP: E.tensor_reduce(out=den[NP, :], in_=ex[NP, :], axis=AX.X, op=ALU.add), reads=["ex"], writes=["den"])
                P.op("dve", RCP(den[NP, :], den[NP, :]), reads=["den"], writes=["den"])
                P.op("dve", TS(G[NP, :], ex[NP, :], den[NP, 0:1], None, ALU.mult), reads=["ex", "den"], writes=["G"])
                pp_, ppn_ = ps()
                P.op("pe", MM(pp_[0:npart, 0:32], ustr_b[0:npart, 0:npart], mkb[0:npart, :]), reads=["ustr_b", "mkb"], writes=[ppn_])
                P.op("dve", TT(posf[NP, :], pp_[NP, 0:32], carry[NP, :], ALU.add), reads=[ppn_, "carry"], writes=["posf"])
                pc_, pcn_ = ps()
                P.op("pe", MM(pc_[:, 0:32], ones_b[0:npart, :], mkb[0:npart, :]), reads=["ones_b", "mkb"], writes=[pcn_])
                P.op("dve", TT(carry[:], carry[:], pc_[:, 0:32], ALU.add), reads=[pcn_, "carry", "posf"], writes=["carry"])
                P.op("dve", TS(v2[NP, :], posf[NP, :], float(CG), None, ALU.is_lt), reads=["posf"], writes=["v2"])
                P.op("dve", TT(v2[NP, :], v2[NP, :], mk[NP, :], ALU.mult), reads=["v2", "mk"], writes=["v2"])
                P.op("dve", TS(rowsel[NP, :], posf[NP, :], 1.0, None, ALU.add), reads=["posf"], writes=["rowsel"])
                P.op("dve", TT(rowsel[NP, :], rowsel[NP, :], v2[NP, :], ALU.mult), reads=["rowsel", "v2"], writes=["rowsel"])
                P.op("dve", TS(rowsel[NP, :], rowsel[NP, :], -1.0, None, ALU.add), reads=["rowsel"], writes=["rowsel"])
                P.dma("sp", DMA(posd[c0:c0 + npart, :], rowsel[NP, :]), "posd", reads=["rowsel"], writes=["posd"])
                P.dma("sp", DMA(gd[c0:c0 + npart, :], G[NP, :]), "gd", reads=["G"], writes=["gd"])
                P.dma("sp", DMA(h2d[c0:c0 + npart, :], hb_[NP, :]), "h2d_%d" % i2, reads=[hbn_], writes=["h2d"])
                pq, pqn = ps()
                P.op("pe", TR(pq[0:32, 0:npart], rowsel[NP, :], ident[0:npart, 0:npart]), reads=["rowsel", "ident"], writes=[pqn])
                P.op("act", ACTV(posTs[:, 0:npart], pq[0:32, 0:npart], AF.Copy), reads=[pqn], writes=["posTs"])
                P.dma("sp", DMA(posTd[:, c0:c0 + npart], posTs[:, 0:npart]), "posTd", reads=["posTs"], writes=["posTd"])
                if ti == 7:
                    P.op("dve", MSET(carry[:], 0.0), reads=["carry"], writes=["carry"])

            for tt_ in range(18):
                if tt_ < 17:
                    dc_stage_a(tt_)
                if tt_ >= 1:
                    dc_stage_b(tt_ - 1)
          P.barrier()
        sc_o.close()

        if STAGE >= 5:
          with ExitStack() as ph:
            bguT = sbt(ph, "bguT", (128, 16, 32))
            Ghl = sbt(ph, "Ghl", (128, 17, 32, 2), BF16)
            with ExitStack() as ph2:
                bgu = sbt(ph2, "bgu", (32, 2 * D))
                G_tm = sbt(ph2, "G_tm", (128, 17, 32))
                Gf = sbt(ph2, "Gf", (128, 17, 32))
                P.op("dve", MSET(G_tm[:], 0.0), writes=["G_tm"])
                P.dma("sp", DMA(G_tm[:, 0:16, :], gd[0:TOK, :].rearrange("(t p) e -> p t e", p=128)), "G_tm", reads=["G_tm"], writes=["G_tm"])
                P.dma("sp", DMA(G_tm[0:16, 16, :], gd[TOK:NTOK, :]), "G_tm", reads=["G_tm"], writes=["G_tm"])
                P.op("dve", CP(Ghl[:, :, :, 0], G_tm[:]), reads=["G_tm"], writes=["Ghl"])
                P.op("dve", CP(Gf[:], Ghl[:, :, :, 0]), reads=["Ghl"], writes=["Gf"])
                P.op("dve", TT(Gf[:], G_tm[:], Gf[:], ALU.subtract), reads=["G_tm", "Gf"], writes=["Gf"])
                P.op("dve", CP(Ghl[:, :, :, 1], Gf[:]), reads=["Gf"], writes=["Ghl"])
                P.dma("sp", DMA(bgu[:], I["b_gate_up"]), "bgu", writes=["bgu"])
                pt, pn = ps()
                for fc in range(16):
                    P.op("pe", TR(pt[:, fc * 32:(fc + 1) * 32], bgu[0:32, fc * 128:(fc + 1) * 128], ident[0:32, 0:32]), reads=["bgu", "ident"], writes=[pn])
                P.op("dve", CP(bguT[:].rearrange("p f e -> p (f e)"), pt[:, :]), reads=[pn], writes=["bguT"])
            P.barrier()
            wpc = [sbt(ph, "wpc%d" % i, (128, 8, 512), BF16) for i in range(2)]
            wd = sbt(ph, "wd", (128, 8, D), BF16)
            bdb = sbt(ph, "bdb", (128, D))
            bdbg = sbt(ph, "bdbg", (128, 512))
            h2bf = sbt(ph, "h2bf", (128, 17, D), BF16)
            oacc = sbt(ph, "oacc", (128, 17, D))
            Pms = [sbt(ph, "Pm%d_" % i, (128, 9, CG), BF16) for i in range(2)]
            xsT = [sbt(ph, "xsT%d" % i, (128, 8, CG), BF16) for i in range(2)]
            actT = [sbt(ph, "actT%d" % i, (128, 8, CG), BF16) for i in range(2)]
            posbc = sbt(ph, "posbc", (128, 1040))
            STm = sbt(ph, "STm", (128, 3, 1040), BF16)
            ygt = sbt(ph, "ygt", (128, 3, D), BF16)
            gs = [sbt(ph, "gs%d" % i, (128, 4)) for i in range(2)]
            gs2 = sbt(ph, "gs2", (128, 2))
            hgc = [sbt(ph, "hgc%d" % i, (128, CG)) for i in range(2)]
            sg = [sbt(ph, "sg%d" % i, (128, CG)) for i in range(2)]
            hu = [sbt(ph, "hu%d" % i, (128, CG)) for i in range(2)]
            pos_tm = sbt(ph, "pos_tm", (128, 17, 32))
            iotac = sbt(ph, "iotac", (128, CG))
            iotaps = sbt(ph, "iotaps", (128, 3))
            P.dma("sp", DMA(iotac[:], C["iotac"]), "iotac", writes=["iotac"])
            P.dma("sp", DMA(iotaps[:], C["iotaps"]), "iotaps", writes=["iotaps"])
            P.op("dve", MSET(pos_tm[:], -1.0), writes=["pos_tm"])
            P.op("pool", MSET(h2bf[:, 16, :], 0.0), writes=["h2bf"])
            P.dma("sp", DMA(pos_tm[:, 0:16, :], posd[0:TOK, :].rearrange("(t p) e -> p t e", p=128)), "pos_tm", reads=["pos_tm"], writes=["pos_tm"])
            P.dma("sp", DMA(pos_tm[0:16, 16, :], posd[TOK:NTOK, :]), "pos_tm", reads=["pos_tm"], writes=["pos_tm"])
            P.dma("sp", DMA(h2bf[:, 0:16, :], h2d[0:TOK, :].rearrange("(t p) d -> p t d", p=128)), "h2bf", reads=["h2bf"], writes=["h2bf"])
            P.dma("sp", DMA(h2bf[0:16, 16, :], h2d[TOK:NTOK, :]), "h2bf", reads=["h2bf"], writes=["h2bf"])
            HALVES = ((0, 8), (8, 17))
            npc = 0
            for e in range(NEXP):
                P.dma("pool", DMA(wd[:], I["w_down"][e].rearrange("(k p) f -> p k f", p=128)), "wd", writes=["wd"])
                P.dma("sp", DMA(bdb[:], I["b_down"][e:e + 1, :].partition_broadcast(128)), "bdb", writes=["bdb"])
                for hf, (t_lo, t_hi) in enumerate(HALVES):
                    T0 = t_lo * 128
                    ntok = (t_hi - t_lo) * 128 if t_hi < 17 else (16 - t_lo) * 128 + 16
                    Pm = Pms[hf]
                    for tl, ti in enumerate(range(t_lo, t_hi)):
                        npart = 16 if ti == 16 else 128
                        nw = min(CG, 128 * (tl + 1))
                        P.op("dve", TS(Pm[0:npart, tl, 0:nw], iotac[0:npart, 0:nw], pos_tm[0:npart, ti, e:e + 1], None, ALU.is_equal),
                             reads=["iotac", "pos_tm"], writes=["Pm%d_%d" % (hf, tl)])
                    for kc in range(8):
                        pt, pn = ps()
                        order = list(range(t_hi - t_lo))[::-1]
                        for oi, tl in enumerate(order):
                            ti = t_lo + tl
                            npart = 16 if ti == 16 else 128
                            nw = min(CG, 128 * (tl + 1))
                            P.op("pe", MM(pt[:, 0:nw], h2bf[0:npart, ti, kc * 128:(kc + 1) * 128], Pm[0:npart, tl, 0:nw], oi == 0, oi == len(order) - 1),
                                 reads=["h2bf", "Pm%d_%d" % (hf, tl)], writes=[pn])
                        P.op("act", ACTV(xsT[hf][:, kc, :], pt[:, 0:CG], AF.Copy), reads=[pn], writes=["xsT%d" % hf])
                    for sc, (s0, sn) in enumerate(SCH):
                        pt, pn = ps()
                        tls = [tl for tl in range(t_hi - t_lo) if 128 * (tl + 1) > s0]
                        for oi, tl in enumerate(tls):
                            ti = t_lo + tl
                            npart = 16 if ti == 16 else 128
                            P.op("pe", MM(pt[0:sn, 0:2], Pm[0:npart, tl, s0:s0 + sn], Ghl[0:npart, ti, e, :], oi == 0, oi == len(tls) - 1),
                                 reads=["Ghl", "Pm%d_%d" % (hf, tl)], writes=[pn])
                        P.op("dve", CP(gs2[0:sn, :], pt[0:sn, 0:2]), reads=[pn], writes=["gs2"])
                        P.op("dve", TT(gs[hf][0:sn, sc:sc + 1], gs2[0:sn, 0:1], gs2[0:sn, 1:2], ALU.add), reads=["gs2"], writes=["gs%d" % hf])
                for pc in range(4):
                    w = wpc[npc % 2]
                    wn = "wpc%d" % (npc % 2)
                    npc += 1
                    P.dma("pool", DMA(w[:, :, 0:256], I["w_gate_up"][e][:, pc * 256:(pc + 1) * 256].rearrange("(k p) f -> p k f", p=128)), wn, writes=[wn])
                    P.dma("pool", DMA(w[:, :, 256:512], I["w_gate_up"][e][:, D + pc * 256:D + (pc + 1) * 256].rearrange("(k p) f -> p k f", p=128)), wn, writes=[wn])
                    for hf in range(2):
                        for j in range(2):
                            fc = pc * 2 + j
                            i2 = (fc + hf) % 2
                            pg, pgn = ps()
                            pu, pun = ps()
                            for kc in range(8):
                                P.op("pe", MM(pg[:, 0:CG], w[:, kc, j * 128:(j + 1) * 128], xsT[hf][:, kc, :], kc == 0, kc == 7), reads=[wn, "xsT%d" % hf], writes=[pgn])
                            for kc in range(8):
                                P.op("pe", MM(pu[:, 0:CG], w[:, kc, 256 + j * 128:256 + (j + 1) * 128], xsT[hf][:, kc, :], kc == 0, kc == 7), reads=[wn, "xsT%d" % hf], writes=[pun])
                            P.op("dve", TS(hgc[i2][:], pg[:, 0:CG], bguT[:, fc, e:e + 1], 7.0, ALU.add, ALU.min), reads=[pgn, "bguT"], writes=["hgc%d" % i2])
                            P.op("act", ACTV(sg[i2][:], hgc[i2][:], AF.Sigmoid, scale=1.702), reads=["hgc%d" % i2], writes=["sg%d" % i2])
                            P.op("act", ACTV(hu[i2][:], pu[:, 0:CG], AF.Identity, bias=bguT[:, 8 + fc, e:e + 1]), reads=[pun, "bguT"], writes=["hu%d" % i2])
                            P.op("dve", TS(hu[i2][:], hu[i2][:], 7.0, -7.0, ALU.min, ALU.max), reads=["hu%d" % i2], writes=["hu%d" % i2])
                            P.op("dve", TT(hgc[i2][:], hgc[i2][:], sg[i2][:], ALU.mult), reads=["hgc%d" % i2, "sg%d" % i2], writes=["hgc%d" % i2])
                            P.op("dve", STT(actT[hf][:, fc, :], hu[i2][:], 1.0, hgc[i2][:], ALU.add, ALU.mult), reads=["hgc%d" % i2, "hu%d" % i2], writes=["actT%d" % hf])
                for hf, (t_lo, t_hi) in enumerate(HALVES):
                    T0 = t_lo * 128
                    ntok = (t_hi - t_lo) * 128 if t_hi < 17 else (16 - t_lo) * 128 + 16
                    P.dma("sp", DMA(posbc[:, 0:ntok], posTd[e:e + 1, T0:T0 + ntok].partition_broadcast(128)), "posbc", writes=["posbc"])
                    for sc, (s0, sn) in enumerate(SCH):
                        P.op("dve", TS(STm[0:sn, sc, 0:ntok], posbc[0:sn, 0:ntok], iotaps[0:sn, sc:sc + 1], None, ALU.is_equal),
                             reads=["posbc", "iotaps"], writes=["STm"])
                    for sc, (s0, sn) in enumerate(SCH):
                        for h2_ in range(2):
                            hs = slice(h2_ * 512, (h2_ + 1) * 512)
                            P.op("act", ACTV(bdbg[0:sn, :], bdb[0:sn, hs], AF.Copy, scale=gs[hf][0:sn, sc:sc + 1]), reads=["bdb", "gs%d" % hf], writes=["bdbg"])
                            pt, pn = ps()
                            for fc in range(8):
                                P.op("pe", MM(pt[0:sn, :], actT[hf][:, fc, s0:s0 + sn], wd[:, fc, hs], fc == 0, fc == 7), reads=["actT%d" % hf, "wd"], writes=[pn])
                            P.op("dve", STT(ygt[0:sn, sc, hs], pt[0:sn, :], gs[hf][0:sn, sc:sc + 1], bdbg[0:sn, :], ALU.mult, ALU.add),
                                 reads=[pn, "gs%d" % hf, "bdbg"], writes=["ygt"])
                    for tl, ti in enumerate(range(t_lo, t_hi)):
                        npart = 16 if ti == 16 else 128
                        for h2_ in range(2):
                            hs = slice(h2_ * 512, (h2_ + 1) * 512)
                            pt, pn = ps()
                            scs = [(sc, s0, sn) for sc, (s0, sn) in enumerate(SCH) if 128 * (tl + 1) > s0]
                            for oi, (sc, s0, sn) in enumerate(scs):
                                P.op("pe", MM(pt[0:npart, :], STm[0:sn, sc, tl * 128:tl * 128 + npart], ygt[0:sn, sc, hs], oi == 0, oi == len(scs) - 1),
                                     reads=["STm", "ygt"], writes=[pn])
                            on = "oacc%d_%d" % (ti, h2_)
                            if e == 0:
                                P.op("act", ACTV(oacc[0:npart, ti, hs], pt[0:npart, :], AF.Copy), reads=[pn], writes=[on])
                            else:
                                P.op("dve", TT(oacc[0:npart, ti, hs], pt[0:npart, :], oacc[0:npart, ti, hs], ALU.add), reads=[pn, on], writes=[on])
            P.barrier()
            wdf = wd[:].rearrange("p k f -> p (k f)").bitcast(F32)
            gt2b = wdf[:, 0:D]
            gt2s = wdf[:, D:2 * D]
            P.dma("sp", DMA(gt2b, modd[0, MGT2:MGT2 + 1, :].partition_broadcast(128)), "gt2b", writes=["gt2b"])
            P.dma("sp", DMA(gt2s[0:16, :], modd[1:17, MGT2, :]), "gt2s", writes=["gt2s"])
            for ti in range(17):
                smp = ti == 16
                npart = 16 if smp else 128
                NP = slice(0, npart)
                c0 = TOK if smp else ti * 128
                xr = wdf[:, (2 + ti % 2) * D:(3 + ti % 2) * D]
                xrn = "xr%d" % (ti % 2)
                P.dma("sp", DMA(xr[NP, :], x1d[c0:c0 + npart, :]), xrn, writes=[xrn])
                gt = gt2s if smp else gt2b
                on = "oaccf%d" % ti
                P.op("dve", TT(oacc[NP, ti, :], oacc[NP, ti, :], gt[NP, :], ALU.mult), reads=["gt2b", "gt2s"], writes=[on])
                P.op("dve", TT(oacc[NP, ti, :], oacc[NP, ti, :], xr[NP, :], ALU.add), reads=[on, xrn], writes=[on])
                finals.append(P.dma("sp", DMA(O["y"][c0:c0 + npart, :], oacc[NP, ti, :]), "yout%d" % (ti % 4), reads=[on], writes=["o_y"]))

    P.emit(nc, finals)
    return nc


_NC_CACHE = {}


def kernel(**inputs):
    inp = {k: np.ascontiguousarray(np.asarray(v, dtype=np.float32)) for k, v in inputs.items()}
    xp = inp["x_prompt"][0]
    in_maps = []
    for c in range(NCORES):
        m = {}
        xh = np.zeros((HALO + TOK, D), np.float32)
        if c > 0:
            xh[:HALO] = xp[(c - 1) * TOK:c * TOK]
        xh[HALO:] = xp[c * TOK:(c + 1) * TOK]
        m["xh"] = xh
        s0, s1 = c * NS, (c + 1) * NS
        m["xs"] = inp["x_sample"][s0:s1, 0]
        m["cp"] = inp["c_prompt"]
        m["cs"] = inp["c_sample"][s0:s1]
        m["kv1"] = inp["cache_kv_w128"][0, s0:s1].reshape(NS, 128, 1024)
        m["kv2"] = inp["cache_kv_w512"][0, s0:s1, 0::4].reshape(NS, 128, 1024)
        m["kv3"] = inp["cache_kv_w2048"][0, s0:s1, 0::16].reshape(NS, 128, 1024)
        m["spool"] = inp["state_pool"][0, s0:s1]
        m["w_ada"] = inp["w_ada"][0]
        m["b_ada"] = inp["b_ada"]
        m["g1"] = inp["norm_mix_g"]
        m["g2"] = inp["norm_ffn_g"]
        m["w_in"] = inp["w_in"][0]
        m["qg"] = inp["q_norm_g"]
        m["kg"] = inp["k_norm_g"]
        m["rel_bias"] = inp["rel_bias"]
        m["w_pool_mix"] = inp["w_pool_mix"][0]
        m["pool_scale"] = inp["pool_scale"]
        m["w_up_attn"] = inp["w_up_attn"][0]
        m["w_up_pool"] = inp["w_up_pool"][0]
        m["w_out"] = inp["w_out"][0]
        m["w_router"] = inp["w_router"][0]
        m["b_router"] = inp["b_router"]
        m["w_gate_up"] = inp["w_gate_up"][0]
        m["b_gate_up"] = inp["b_gate_up"][0]
        m["w_down"] = inp["w_down"][0]
        m["b_down"] = inp["b_down"][0]
        for k, v in static_consts(c).items():
            m["c_" + k] = v
        m = {k: np.ascontiguousarray(v, dtype=np.float32) for k, v in m.items() if (k in IN_SHAPES or k.startswith("c_"))}
        in_maps.append(m)
    if "nc" not in _NC_CACHE:
        _NC_CACHE["nc"] = build()
    res = run_bass_kernel_spmd(_NC_CACHE["nc"], in_maps, core_ids=list(range(NCORES)))
    R = res.results
    if os.environ.get("MK_DBG"):
        _NC_CACHE["res"] = R
    y_prompt = np.concatenate([R[c]["y"][:TOK] for c in range(NCORES)], 0)[None]
    y_sample = np.concatenate([R[c]["y"][TOK:] for c in range(NCORES)], 0)[:, None, :]
    L = R[NCORES - 1]
    kv128 = L["kvp1"].reshape(1, 1, 128, 2, 8, 64)
    kv512 = L["kvp2"].reshape(1, 1, 512, 2, 8, 64)
    kv2048 = L["kvp3"].reshape(1, 1, 2048, 2, 8, 64)
    pool_p = L["poolp"][1:16].reshape(1, 1, 15, 512)
    kvs = np.concatenate([R[c]["kvs"] for c in range(NCORES)], 0)
    ks = [kvs[:, g].reshape(1, 128, 1, 2, 8, 64) for g in range(3)]
    pool_s = np.concatenate([R[c]["pools"] for c in range(NCORES)], 0)[None]
    return (y_prompt.astype(np.float32), y_sample.astype(np.float32), kv128, kv512, kv2048, pool_p,
            ks[0], ks[1], ks[2], pool_s.astype(np.float32))
```

```python
import os
from contextlib import ExitStack
import numpy as np
import concourse.bass as bass
import concourse.mybir as mybir
from concourse.bass_utils import run_bass_kernel_spmd

F32 = mybir.dt.float32
BF16 = mybir.dt.bfloat16
I32 = mybir.dt.int32
AF = mybir.ActivationFunctionType
ALU = mybir.AluOpType
AX = mybir.AxisListType

NCORES = 8
D = 1024
TOK = 2048
HALO = 2048
NS = 16
OWN0 = HALO
SMP0 = HALO + TOK
TCOL = HALO + TOK + NS
NTOK = TOK + NS
CAP = 512
CG = 320
SCH = ((0, 128), (128, 128), (256, 64))
NEXP = 32
NROWS = NEXP * CAP
EPS = 1e-6
DILS = (1, 4, 16)
STAGE = int(os.environ.get("MK_STAGE", "9"))
CL = int(os.environ.get("MK_CL", "99"))
CONE = int(os.environ.get("MK_ONE", "0"))
VL = int(os.environ.get("MK_VL", "9"))


class Prog:
    ENG = ("pe", "act", "dve", "pool", "sp")

    def __init__(self):
        self.q = {e: [] for e in self.ENG}
        self.lastw = {}
        self.readers = {}
        self.dmakeys = {}
        self.keymap = {}
        self.psn = 0

    def _collect(self, reads, writes):
        deps = []
        for r in reads:
            if r in self.lastw:
                deps.append(self.lastw[r])
        for w in writes:
            if w in self.lastw:
                deps.append(self.lastw[w])
            deps.extend(self.readers.get(w, ()))
        return deps

    def _commit(self, rec, reads, writes):
        for r in reads:
            self.readers.setdefault(r, []).append(rec)
        for w in writes:
            self.lastw[w] = rec
            self.readers[w] = []

    def op(self, eng, fn, reads=(), writes=(), extra=()):
        writes = list(writes) + [r for r in reads if r.startswith("ps") and r[2:3] in "01234567OL"]
        reads = [r for r in reads if not (r.startswith("ps") and r[2:3] in "01234567OL")]
        rec = {"eng": eng, "fn": fn, "deps": self._collect(reads, writes) + list(extra), "dma": None, "needed": False}
        self._commit(rec, reads, writes)
        self.q[eng].append(rec)
        return rec

    def dma(self, eng, fn, key, reads=(), writes=(), extra=()):
        if (eng, key) not in self.keymap:
            self.keymap[(eng, key)] = (eng, sum(1 for k in self.keymap if k[0] == eng))
        key = self.keymap[(eng, key)]
        cnt = self.dmakeys.get(key, 0) + 16
        self.dmakeys[key] = cnt
        rec = {"eng": eng, "fn": fn, "deps": self._collect(reads, writes) + list(extra), "dma": (key, cnt), "needed": False}
        self._commit(rec, reads, writes)
        self.q[eng].append(rec)
        return rec

    def barrier(self):
        last = []
        for e in self.ENG:
            for rec in reversed(self.q[e]):
                if rec["dma"] is None and rec["fn"] is not None:
                    last.append(rec)
                    break
        seen = {}
        for e in self.ENG:
            for rec in self.q[e]:
                if rec["dma"] is not None:
                    seen[rec["dma"][0]] = rec
        deps = last + list(seen.values())
        for e in self.ENG:
            rec = {"eng": e, "fn": None, "deps": list(deps), "dma": None, "needed": False}
            self.q[e].append(rec)
        self.lastw = {}
        self.readers = {}
        self.keymap = {}

    def emit(self, nc, final_deps):
        for e in self.ENG:
            for rec in self.q[e]:
                for d in rec["deps"]:
                    if d["dma"] is None and not (d["eng"] == "pe" and rec["eng"] == "pe"):
                        d["needed"] = True
        for e in self.ENG:
            c = 0
            for rec in self.q[e]:
                if rec["dma"] is None and rec["needed"]:
                    c += 1
                    rec["cnt"] = c
        keys = sorted(self.dmakeys.keys(), key=str)
        with ExitStack() as st:
            esem = {e: st.enter_context(nc.semaphore("es_" + e)) for e in self.ENG}
            dsem = {k: st.enter_context(nc.semaphore("ds%d" % i)) for i, k in enumerate(keys)}
            block = st.enter_context(nc.Block())

            def run(e, E):
                waited = {}
                recs = list(self.q[e])
                if e == "sp":
                    recs.append({"eng": "sp", "fn": None, "deps": list(final_deps), "dma": None, "needed": False})
                for rec in recs:
                    for d in rec["deps"]:
                        if d["dma"] is not None:
                            k = ("d", d["dma"][0])
                            sem, val = dsem[d["dma"][0]], d["dma"][1]
                        else:
                            if d["eng"] == "pe" and e == "pe":
                                continue
                            k = ("c", d["eng"])
                            sem, val = esem[d["eng"]], d["cnt"]
                        if waited.get(k, 0) >= val:
                            continue
                        E.wait_ge(sem, val)
                        waited[k] = val
                    if rec["fn"] is None:
                        continue
                    ins = rec["fn"](E)
                    if rec["dma"] is not None:
                        ins.then_inc(dsem[rec["dma"][0]], 16)
                    elif rec["needed"]:
                        ins.then_inc(esem[e], 1)

            @block.tensor
            def _(E):
                run("pe", E)

            @block.scalar
            def _(E):
                run("act", E)

            @block.vector
            def _(E):
                run("dve", E)

            @block.gpsimd
            def _(E):
                run("pool", E)

            @block.sync
            def _(E):
                run("sp", E)


def MM(out, lhsT, rhs, start=True, stop=True):
    return lambda E: E.matmul(out, lhsT, rhs, start=start, stop=stop)


def TR(out, in_, ident):
    return lambda E: E.transpose(out, in_, ident)


def ACTV(out, in_, func, **kw):
    return lambda E: E.activation(out=out, in_=in_, func=func, **kw)


def TT(out, in0, in1, op):
    return lambda E: E.tensor_tensor(out=out, in0=in0, in1=in1, op=op)


def TS(out, in0, s1, s2, op0, op1=None, **kw):
    if op1 is None:
        return lambda E: E.tensor_scalar(out=out, in0=in0, scalar1=s1, scalar2=None, op0=op0, **kw)
    return lambda E: E.tensor_scalar(out=out, in0=in0, scalar1=s1, scalar2=s2, op0=op0, op1=op1, **kw)


def STT(out, in0, scalar, in1, op0, op1):
    return lambda E: E.scalar_tensor_tensor(out=out, in0=in0, scalar=scalar, in1=in1, op0=op0, op1=op1)


def CP(out, in_):
    return lambda E: E.tensor_copy(out=out, in_=in_)


def RCP(out, in_):
    return lambda E: E.reciprocal(out=out, in_=in_)


def RCPF(out, in_):
    return lambda E: E.reciprocal_approx_fast(out=out, in_=in_)


def MSET(ap, v):
    return lambda E: E.memset(ap, v)


def DMA(out, in_, **kw):
    return lambda E: E.dma_start(out=out, in_=in_, **kw)


def t5_bucket_np(d):
    d = np.asarray(d, np.int64)
    ratio = np.log(np.maximum(d, 1).astype(np.float32) / np.float32(16)) / np.float32(np.log(2048 / 16))
    large = np.minimum(16 + (ratio.astype(np.float32) * np.float32(16)).astype(np.int32), 31)
    return np.where(d < 16, d, large)


def static_consts(core):
    c = {}
    c["ident"] = np.eye(128, dtype=np.float32)
    c["jflip"] = np.eye(128, dtype=np.float32)[::-1].copy()
    bo = np.zeros((128, 128), np.float32)
    bo[:64, :64] = 1.0 / 64
    bo[64:, 64:] = 1.0 / 64
    c["blockones"] = bo
    us = np.triu(np.ones((128, 128), np.float32), 1)
    c["ustrict"] = us
    ohc = np.zeros((3, 32, 256), np.float32)
    ohp = np.zeros((3, 32, 256), np.float32)
    val = np.zeros((2, 8, 256), np.float32)
    for g, dil in enumerate(DILS):
        for m in range(255):
            u = m - 127
            if u >= 0:
                ohc[g, t5_bucket_np(u * dil), m] = 1.0
            if u <= 0:
                ohp[g, t5_bucket_np((u + 128) * dil), m] = 1.0
    for m in range(255):
        u = m - 127
        val[1, :, m] = 1.0 if u >= 0 else 0.0
        val[0, :, m] = 1.0 if u <= 0 else 0.0
    c["ohc"] = ohc
    c["ohp"] = ohp
    c["valid"] = val
    ohs = np.zeros((3, 32, 128), np.float32)
    for g, dil in enumerate(DILS):
        for i in range(128):
            ohs[g, t5_bucket_np(dil * (128 - i)), i] = 1.0
    c["ohs"] = ohs
    selbc = np.zeros((16, 16, 128), np.float32)
    for n in range(16):
        selbc[n, n, :] = 1.0
    c["selbc"] = selbc.reshape(16, 16 * 128)
    selcol = np.zeros((128, 16, 16), np.float32)
    for n in range(16):
        selcol[:, n, n] = 1.0
    c["selcol"] = selcol.reshape(128, 256)
    c["hp"] = np.full((128, 1), 0.0 if core == 0 else 1.0, np.float32)
    corr = np.ones((128, 4, 16), np.float32)
    for gi, w in enumerate((2, 4, 8, 16)):
        for t in range(16):
            cnt = min(t + 1, w) if core == 0 else w
            corr[:, gi, t] = 1.0 / cnt
    c["poolcorr"] = corr.reshape(128, 64)
    c["iota32"] = np.tile(np.arange(32, dtype=np.float32)[None, :] * CAP, (128, 1))
    c["iotac"] = np.tile(np.arange(CG, dtype=np.float32)[None, :], (128, 1))
    c["iotaps"] = (np.arange(128, dtype=np.float32)[:, None] + 128.0 * np.arange(3, dtype=np.float32)[None, :])
    return c


CONST_SHAPES = {
    "ident": (128, 128), "jflip": (128, 128), "blockones": (128, 128), "ustrict": (128, 128),
    "ohc": (3, 32, 256), "ohp": (3, 32, 256), "valid": (2, 8, 256), "ohs": (3, 32, 128),
    "selbc": (16, 2048), "selcol": (128, 256), "hp": (128, 1), "poolcorr": (128, 64), "iota32": (128, 32), "iotac": (128, CG), "iotaps": (128, 3),
}

IN_SHAPES = {
    "xh": (HALO + TOK, D), "xs": (NS, D), "cp": (1, D), "cs": (NS, D),
    "kv1": (NS, 128, 1024), "kv2": (NS, 128, 1024), "kv3": (NS, 128, 1024), "spool": (NS, 15, 512),
    "w_ada": (D, 6 * D), "b_ada": (1, 6 * D), "g1": (1, D), "g2": (1, D), "w_in": (D, 7168),
    "qg": (1, 64), "kg": (1, 64), "rel_bias": (32, 24), "w_pool_mix": (4, 128, 128), "pool_scale": (1, 512),
    "w_up_attn": (512, D), "w_up_pool": (512, D), "w_out": (D, D), "w_router": (D, 32), "b_router": (1, 32),
    "w_gate_up": (NEXP, D, 2 * D), "b_gate_up": (NEXP, 2 * D), "w_down": (NEXP, D, D), "b_down": (NEXP, D),
}
if STAGE < 5:
    for _k in ("w_gate_up", "b_gate_up", "w_down", "b_down"):
        IN_SHAPES.pop(_k)
OUT_SHAPES = {
    "y": (NTOK, D), "kvp1": (128, 1024), "kvp2": (512, 1024), "kvp3": (2048, 1024), "poolp": (16, 512),
    "kvs": (NS, 3, 1024), "pools": (NS, 15, 512),
}


def build():
    nc = bass.Bass("TRN2", target_bir_lowering=False)
    P = Prog()
    I = {k: nc.dram_tensor(k, list(s), F32, kind="ExternalInput").ap() for k, s in IN_SHAPES.items()}
    C = {k: nc.dram_tensor("c_" + k, list(s), F32, kind="ExternalInput").ap() for k, s in CONST_SHAPES.items()}
    O = {k: nc.dram_tensor(k, list(s), F32, kind="ExternalOutput").ap() for k, s in OUT_SHAPES.items()}

    def scr(name, shape, dt=F32):
        return nc.dram_tensor(name, list(shape), dt, kind="Internal").ap()

    modd = scr("modd", (17, 6, D))
    evd = scr("evd", (3, 2, 8, 256))
    x1d = scr("x1d", (NTOK, D))
    xsd = scr("xsd", (NROWS, D), BF16)
    ysd = scr("ysd", (NROWS, D))
    posd = scr("posd", (NTOK, 32))
    gd = scr("gd", (NTOK, 32))
    posTd = scr("posTd", (32, NTOK))
    h2d = scr("h2d", (NTOK, D), BF16)
    finals = []
    DBGO = {}

    def dbg(name, ap_sb, shape, dt, reads):
        if not os.environ.get("MK_DBG"):
            return
        t = nc.dram_tensor("dbg_" + name, list(shape), dt, kind="ExternalOutput").ap()
        DBGO[name] = t
        finals.append(P.dma("sp", DMA(t, ap_sb), "dbg_" + name, reads=reads, writes=["dbg_" + name]))

    with ExitStack() as top:
        def sbt(st, name, shape, dt=F32):
            return st.enter_context(nc.sbuf_tensor(name, list(shape), dt))

        PS = [top.enter_context(nc.psum_tensor("ps%d" % i, [128, 512], F32)) for i in range(8)]

        P.psmod = 8

        def ps():
            i = P.psn % P.psmod
            P.psn += 1
            return PS[i], "ps%d" % i

        ident = sbt(top, "ident", (128, 128))
        jflip = sbt(top, "jflip", (128, 128))
        ones_f = sbt(top, "ones_f", (128, 128))
        zo_f = sbt(top, "zo_f", (1, 128))
        ones_b = sbt(top, "ones_b", (128, 128), BF16)
        bones_b = sbt(top, "bones_b", (128, 128), BF16)
        ustr_b = sbt(top, "ustr_b", (128, 128), BF16)
        epst = sbt(top, "epst", (128, 1))
        hpt = sbt(top, "hpt", (128, 1))
        gq = sbt(top, "gq", (128, 1))
        gk = sbt(top, "gk", (128, 1))
        ident_b = sbt(top, "ident_b", (128, 128), BF16)
        hlast = sbt(top, "hlast", (128, 8, 16), BF16)
        P.dma("pool", DMA(ident_b[:], C["ident"]), "ident_b", writes=["ident_b"])
        P.dma("sp", DMA(ident[:], C["ident"]), "ident", writes=["ident"])
        P.dma("sp", DMA(jflip[:], C["jflip"]), "jflip", writes=["jflip"])
        P.dma("sp", DMA(hpt[:], C["hp"]), "hpt", writes=["hpt"])
        P.dma("pool", DMA(bones_b[:], C["blockones"]), "bones_b", writes=["bones_b"])
        P.dma("pool", DMA(ustr_b[:], C["ustrict"]), "ustr_b", writes=["ustr_b"])
        P.op("dve", MSET(ones_f[:], 1.0), writes=["ones_f"])
        P.op("dve", MSET(zo_f[:, 0:64], 0.0), writes=["zo_f"])
        P.op("dve", MSET(zo_f[:, 64:128], 1.0), writes=["zo_f"])
        P.op("dve", MSET(ones_b[:], 1.0), writes=["ones_b"])
        P.op("dve", MSET(epst[:], EPS), writes=["epst"])
        for (t, src, nm) in ((gq, I["qg"], "gq"), (gk, I["kg"], "gk")):
            srcT = src.rearrange("o e -> e o")
            with nc.allow_non_contiguous_dma(reason="tiny gain vector"):
                P.dma("sp", DMA(t[0:64, :], srcT), nm, writes=[nm])
                P.dma("sp", DMA(t[64:128, :], srcT), nm, writes=[nm])
        P.op("dve", TS(gq[:], gq[:], 0.125, None, ALU.mult), reads=["gq"], writes=["gq"])

        with ExitStack() as ph:
            csb = sbt(ph, "csb", (16, D))
            cpb = sbt(ph, "cpb", (1, D))
            scT = sbt(ph, "scT", (128, 8, 17), BF16)
            modp = sbt(ph, "modp", (1, 6 * D))
            mods = sbt(ph, "mods", (16, 6 * D))
            badas = sbt(ph, "badas", (16, 6 * D))
            g1s = sbt(ph, "g1s", (16, 2, D))
            wad = [sbt(ph, "wad%d" % i, (128, 8, 1024), BF16) for i in range(2)]
            P.dma("sp", DMA(csb[:], I["cs"]), "csb", writes=["csb"])
            P.dma("sp", DMA(cpb[:], I["cp"]), "cpb", writes=["cpb"])
            P.dma("sp", DMA(badas[:], I["b_ada"].partition_broadcast(16)), "badas", writes=["badas"])
            P.dma("sp", DMA(g1s[:, 0, :], I["g1"].partition_broadcast(16)), "g1s", writes=["g1s"])
            P.dma("sp", DMA(g1s[:, 1, :], I["g2"].partition_broadcast(16)), "g1s", writes=["g1s"])
            P.op("act", ACTV(csb[:], csb[:], AF.Silu), reads=["csb"], writes=["csb"])
            P.op("act", ACTV(cpb[:], cpb[:], AF.Silu), reads=["cpb"], writes=["cpb"])
            pt, pn = ps()
            for kc in range(8):
                P.op("pe", TR(pt[:, kc * 17:kc * 17 + 1], cpb[0:1, kc * 128:(kc + 1) * 128], ident[0:1, 0:1]),
                     reads=["cpb", "ident"], writes=[pn])
                P.op("pe", TR(pt[:, kc * 17 + 1:kc * 17 + 17], csb[0:16, kc * 128:(kc + 1) * 128], ident[0:16, 0:16]),
                     reads=["csb", "ident"], writes=[pn])
            P.op("dve", CP(scT[:].rearrange("p k c -> p (k c)"), pt[:, 0:136]), reads=[pn], writes=["scT"])
            for blk in range(6):
                w = wad[blk % 2]
                wn = "wad%d" % (blk % 2)
                P.dma("pool", DMA(w[:], I["w_ada"][:, blk * 1024:(blk + 1) * 1024].rearrange("(k p) c -> p k c", p=128)),
                      wn, writes=[wn])
                for half in range(2):
                    c0 = blk * 1024 + half * 512
                    pa, pan = ps()
                    pb, pbn = ps()
                    for kc in range(8):
                        P.op("pe", MM(pa[0:1, :], scT[:, kc, 0:1], w[:, kc, half * 512:(half + 1) * 512], kc == 0, kc == 7),
                             reads=["scT", wn], writes=[pan])
                    for kc in range(8):
                        P.op("pe", MM(pb[0:16, :], scT[:, kc, 1:17], w[:, kc, half * 512:(half + 1) * 512], kc == 0, kc == 7),
                             reads=["scT", wn], writes=[pbn])
                    P.op("dve", TT(modp[:, c0:c0 + 512], pa[0:1, :], badas[0:1, c0:c0 + 512], ALU.add),
                         reads=[pan, "badas"], writes=["modp"])
                    P.op("dve", TT(mods[:, c0:c0 + 512], pb[0:16, :], badas[:, c0:c0 + 512], ALU.add),
                         reads=[pbn, "badas"], writes=["mods"])
            for (m, npart, eng) in ((modp, 1, "dve"), (mods, 16, "dve")):
                mn = "modp" if npart == 1 else "mods"
                for (sc, gi) in ((1, 0), (4, 1)):
                    P.op(eng, STT(m[0:npart, sc * D:(sc + 1) * D], m[0:npart, sc * D:(sc + 1) * D], 1.0, g1s[0:npart, gi, :],
                                  ALU.add, ALU.mult), reads=[mn, "g1s"], writes=[mn])
            P.dma("sp", DMA(modd[0:1].rearrange("o j d -> o (j d)"), modp[:]), "modd", reads=["modp"], writes=["modd"])
            P.dma("sp", DMA(modd[1:17].rearrange("o j d -> o (j d)"), mods[:]), "modd", reads=["mods"], writes=["modd"])
        P.barrier()
        MSH1, MA1, MGT1, MSH2, MA2, MGT2 = range(6)

        def load_mod(eng, dst, dname, j, sample, key):
            if sample:
                return P.dma(eng, DMA(dst[0:16, :], modd[1:17, j, :]), key, reads=["modd"], writes=[dname])
            return P.dma(eng, DMA(dst[:, :], modd[0, j:j + 1, :].partition_broadcast(128)), key, reads=["modd"], writes=[dname])

        sc_o = top.enter_context(ExitStack())
        aoT = sbt(sc_o, "aoT", (128, 4, NTOK), BF16)
        hTo = sbt(sc_o, "hTo", (128, 8, NTOK), BF16)
        sc_h = sc_o.enter_context(ExitStack())
        hTh = sbt(sc_h, "hTh", (128, 8, HALO), BF16)

        def hcols(c0, n, step=1):
            if c0 < OWN0:
                assert c0 + (n - 1) * step < OWN0
                return hTh, "hTh", c0
            return hTo, "hTo", c0 - OWN0

        def norm_tile(xt, xn, npart, a_t, a_n, b_t, b_n, out_t, out_n, tmp, tmpn, ssq, rs, tag):
            P.op("act", ACTV(tmp[0:npart, :], xt[0:npart, :], AF.Square, accum_out=ssq[0:npart, 0:1]),
                 reads=[xn], writes=[tmpn, tag + "ssq"])
            P.op("dve", TS(rs[0:npart, 0:1], ssq[0:npart, 0:1], 1.0 / D, EPS, ALU.mult, ALU.add),
                 reads=[tag + "ssq"], writes=[tag + "rs"])
            P.op("act", ACTV(rs[0:npart, 0:1], rs[0:npart, 0:1], AF.Ln), reads=[tag + "rs"], writes=[tag + "rs"])
            P.op("act", ACTV(rs[0:npart, 0:1], rs[0:npart, 0:1], AF.Exp, scale=-0.5), reads=[tag + "rs"], writes=[tag + "rs"])
            P.op("dve", STT(out_t[0:npart, :], xt[0:npart, :], rs[0:npart, 0:1], a_t[0:npart, :], ALU.mult, ALU.mult),
                 reads=[xn, tag + "rs", a_n], writes=[out_n])
            P.op("dve", TT(out_t[0:npart, :], out_t[0:npart, :], b_t[0:npart, :], ALU.add), reads=[out_n, b_n], writes=[out_n])

        with ExitStack() as ph:
            a1b = sbt(ph, "a1b", (128, D))
            b1b = sbt(ph, "b1b", (128, D))
            a1s = sbt(ph, "a1s", (16, D))
            b1s = sbt(ph, "b1s", (16, D))
            xts = [sbt(ph, "xt%d" % i, (128, D)) for i in range(3)]
            hts = [sbt(ph, "ht%d" % i, (128, D)) for i in range(2)]
            junk = sbt(ph, "junk", (128, D), BF16)
            ssq = sbt(ph, "ssq", (128, 1))
            rs = sbt(ph, "rs", (128, 1))
            load_mod("sp", a1b, "a1b", MA1, False, "a1b")
            load_mod("sp", b1b, "b1b", MSH1, False, "b1b")
            load_mod("sp", a1s, "a1s", MA1, True, "a1s")
            load_mod("sp", b1s, "b1s", MSH1, True, "b1s")
            def b_stage_a(ti):
                smp = ti == 32
                npart = 16 if smp else 128
                xt = xts[ti % 3]
                xn = "xt%d" % (ti % 3)
                ht = hts[ti % 2]
                hn = "ht%d" % (ti % 2)
                src = I["xs"] if smp else I["xh"][ti * 128:(ti + 1) * 128, :]
                P.dma("sp", DMA(xt[0:npart, :], src), xn, writes=[xn])
                norm_tile(xt, xn, npart, a1s if smp else a1b, "a1s" if smp else "a1b", b1s if smp else b1b,
                          "b1s" if smp else "b1b", ht, hn, junk, "junk", ssq, rs, "B")

            def b_stage_b(ti):
                smp = ti == 32
                npart = 16 if smp else 128
                xt = xts[ti % 3]
                xn = "xt%d" % (ti % 3)
                ht = hts[ti % 2]
                hn = "ht%d" % (ti % 2)
                p0, p0n = ps()
                p1, p1n = ps()
                for kc in range(8):
                    pp, ppn = (p0, p0n) if kc < 4 else (p1, p1n)
                    P.op("pe", TR(pp[:, (kc % 4) * 128:(kc % 4) * 128 + npart], ht[0:npart, kc * 128:(kc + 1) * 128],
                                  ident[0:npart, 0:npart]), reads=[hn, "ident"], writes=[ppn])
                c0 = SMP0 if smp else ti * 128
                hb, hbn, cc = hcols(c0, npart)
                for half, (pp, ppn) in enumerate(((p0, p0n), (p1, p1n))):
                    P.op("act", ACTV(hb[:, half * 4:(half + 1) * 4, cc:cc + npart],
                                     pp[:].rearrange("p (k t) -> p k t", t=128)[:, :, 0:npart], AF.Copy),
                         reads=[ppn], writes=[hbn])

            for tt_ in range(34):
                if tt_ < 33:
                    b_stage_a(tt_)
                if tt_ >= 1:
                    b_stage_b(tt_ - 1)
        P.op("pool", CP(hlast[:], hTh[:, :, HALO - 16:HALO]), reads=["hTh"], writes=["hlast"])
        P.barrier()

        if STAGE >= 2:
          with ExitStack() as ph:
            zs = sbt(ph, "zs", (16, 4608))
            sqs = sbt(ph, "sqs", (16, 3072))
            wsb = [sbt(ph, "wsb%d" % i, (128, 8, 512), BF16) for i in range(2)]
            kvt = [sbt(ph, "kvt%d" % i, (128, 1024)) for i in range(3)]
            prod = [sbt(ph, "prod%d" % i, (128, 512)) for i in range(2)]
            pvb = [sbt(ph, "pvb%d" % i, (128, 520), BF16) for i in range(2)]
            lgs = [sbt(ph, "lgs%d" % i, (128, 8)) for i in range(2)]
            ems = [sbt(ph, "ems%d" % i, (128, 8)) for i in range(2)]
            selbc = sbt(ph, "selbc", (16, 2048))
            selcol = sbt(ph, "selcol", (128, 256), BF16)
            ohs = sbt(ph, "ohs", (32, 3, 128))
            relb = sbt(ph, "relb", (32, 24))
            expbS = sbt(ph, "expbS", (128, 24))
            expb0 = sbt(ph, "expb0", (16, 24))
            g16 = sbt(ph, "g16", (16, 2, 64))
            mss = sbt(ph, "mss", (16, 48))
            l0 = sbt(ph, "l0", (16, 24))
            osum = sbt(ph, "osum", (16, 512))
            lsum = sbt(ph, "lsum", (16, 8))
            tmp5 = sbt(ph, "tmp5", (16, 512))
            P.dma("sp", DMA(selbc[:], C["selbc"]), "selbc", writes=["selbc"])
            P.dma("pool", DMA(selcol[:], C["selcol"]), "selcol", writes=["selcol"])
            P.dma("sp", DMA(ohs[:], C["ohs"].rearrange("g b i -> b g i")), "ohs", writes=["ohs"])
            P.dma("sp", DMA(relb[:], I["rel_bias"]), "relb", writes=["relb"])
            P.dma("sp", DMA(expb0[:], I["rel_bias"][0:1, :].partition_broadcast(16)), "expb0", writes=["expb0"])
            P.dma("sp", DMA(g16[:, 0, :], I["qg"].partition_broadcast(16)), "g16", writes=["g16"])
            P.dma("sp", DMA(g16[:, 1, :], I["kg"].partition_broadcast(16)), "g16", writes=["g16"])
            P.op("dve", TS(g16[:, 0, :], g16[:, 0, :], 0.125, None, ALU.mult), reads=["g16"], writes=["g16"])
            P.op("act", ACTV(expb0[:], expb0[:], AF.Exp), reads=["expb0"], writes=["expb0"])
            pt, pn = ps()
            for g in range(3):
                P.op("pe", MM(pt[:, g * 8:(g + 1) * 8], ohs[:, g, :], relb[:, g * 8:(g + 1) * 8]), reads=["ohs", "relb"], writes=[pn])
            P.op("act", ACTV(expbS[:], pt[:, 0:24], AF.Exp), reads=[pn], writes=["expbS"])
            for cb in range(9):
                w = wsb[cb % 2]
                wn = "wsb%d" % (cb % 2)
                P.dma("pool", DMA(w[:], I["w_in"][:, cb * 512:(cb + 1) * 512].rearrange("(k p) c -> p k c", p=128)), wn, writes=[wn])
                pt, pn = ps()
                for kc in range(8):
                    P.op("pe", MM(pt[0:16, :], hTo[:, kc, TOK:TOK + 16], w[:, kc, :], kc == 0, kc == 7), reads=["hTo", wn], writes=[pn])
                P.op("act", ACTV(zs[:, cb * 512:(cb + 1) * 512], pt[0:16, :], AF.Copy), reads=[pn], writes=["zs"])
            P.op("dve", TT(sqs[:], zs[:, 0:3072], zs[:, 0:3072], ALU.mult), reads=["zs"], writes=["sqs"])
            P.op("dve", lambda E: E.tensor_reduce(out=mss[:], in_=sqs[:].rearrange("p (h e) -> p h e", e=64), axis=AX.X, op=ALU.add),
                 reads=["sqs"], writes=["mss"])
            P.op("dve", TS(mss[:], mss[:], 1.0 / 64, EPS, ALU.mult, ALU.add), reads=["mss"], writes=["mss"])
            P.op("act", ACTV(mss[:], mss[:], AF.Sqrt), reads=["mss"], writes=["mss"])
            P.op("dve", RCP(mss[:], mss[:]), reads=["mss"], writes=["mss"])
            zqk = zs[:, 0:3072].rearrange("p (h e) -> p h e", e=64)
            P.op("dve", TT(zqk, zqk, mss[:].unsqueeze(2).to_broadcast([16, 48, 64]), ALU.mult), reads=["zs", "mss"], writes=["zs"])
            for qk in range(2):
                zz = zs[:, qk * 1536:(qk + 1) * 1536].rearrange("p (h e) -> p h e", e=64)
                P.op("dve", TT(zz, zz, g16[:, qk, :].unsqueeze(1).to_broadcast([16, 24, 64]), ALU.mult), reads=["zs", "g16"], writes=["zs"])
            finals.append(P.dma("sp", DMA(O["kvs"][:, :, 0:512], zs[:, 1536:3072].rearrange("p (g c) -> p g c", g=3)), "kvs_k", reads=["zs"], writes=["o_kvs_k"]))
            finals.append(P.dma("sp", DMA(O["kvs"][:, :, 512:1024], zs[:, 3072:4608].rearrange("p (g c) -> p g c", g=3)), "kvs_v", reads=["zs"], writes=["o_kvs_v"]))
            psO, psL = PS[6], PS[7]
            P.psmod = 6
            caches = (I["kv1"], I["kv2"], I["kv3"])
            sits = [(n, g) for n in range(16) for g in range(3)]
            SD = 2
            sst = {}
            for it2 in range(len(sits) + SD):
                if it2 < len(sits):
                    n, g = sits[it2]
                    kt = kvt[it2 % 3]
                    ktn = "kvt%d" % (it2 % 3)
                    P.dma("sp", DMA(kt[:], caches[g][n]), ktn, writes=[ktn])
                    pt, pn = ps()
                    P.op("pe", MM(pt[:, :], selbc[0:16, n * 128:(n + 1) * 128], zs[0:16, g * 512:(g + 1) * 512]), reads=["selbc", "zs"], writes=[pn])
                    sst[it2] = (pt, pn)
                it = it2 - SD
                if it < 0:
                    continue
                n, g = sits[it]
                pt, pn = sst.pop(it)
                kt = kvt[it % 3]
                ktn = "kvt%d" % (it % 3)
                pr = prod[it % 2]
                prn = "prod%d" % (it % 2)
                pv = pvb[it % 2]
                pvn = "pvb%d" % (it % 2)
                lg = lgs[it % 2]
                lgn = "lgs%d" % (it % 2)
                em = ems[it % 2]
                emn = "ems%d" % (it % 2)
                P.op("dve", TT(pr[:], kt[:, 0:512], pt[:, :], ALU.mult), reads=[ktn, pn], writes=[prn])
                P.op("dve", lambda E, lg=lg, pr=pr: E.tensor_reduce(out=lg[:], in_=pr[:].rearrange("p (h e) -> p h e", e=64), axis=AX.X, op=ALU.add),
                     reads=[prn], writes=[lgn])
                P.op("act", ACTV(lg[:], lg[:], AF.Exp), reads=[lgn], writes=[lgn])
                P.op("dve", TT(em[:], lg[:], expbS[:, g * 8:(g + 1) * 8], ALU.mult), reads=[lgn, "expbS"], writes=[emn])
                P.op("act", ACTV(pv[:, 512:520], em[:], AF.Copy), reads=[emn], writes=[pvn])
                P.op("dve", TT(pv[:, 0:512].rearrange("p (h e) -> p h e", e=64), kt[:, 512:1024].rearrange("p (h e) -> p h e", e=64),
                               em[:].unsqueeze(2).to_broadcast([128, 8, 64]), ALU.mult), reads=[ktn, emn], writes=[pvn])
                first, last = it == 0, it == 47
                P.op("pe", MM(psO[0:16, :], selcol[:, n * 16:(n + 1) * 16], pv[:, 0:512], first, last), reads=["selcol", pvn], writes=["psO"])
                P.op("pe", MM(psL[0:16, 0:8], selcol[:, n * 16:(n + 1) * 16], pv[:, 512:520], first, last), reads=["selcol", pvn], writes=["psL"])
            qv = zs[:, 0:1536].rearrange("p (h e) -> p h e", e=64)
            kv_ = zs[:, 1536:3072].rearrange("p (h e) -> p h e", e=64)
            sq3 = sqs[:, 0:1536].rearrange("p (h e) -> p h e", e=64)
            P.op("dve", TT(sq3, qv, kv_, ALU.mult), reads=["zs"], writes=["sqs"])
            P.op("dve", lambda E: E.tensor_reduce(out=l0[:], in_=sq3, axis=AX.X, op=ALU.add), reads=["sqs"], writes=["l0"])
            P.op("act", ACTV(l0[:], l0[:], AF.Exp), reads=["l0"], writes=["l0"])
            P.op("dve", TT(l0[:], l0[:], expb0[:], ALU.mult), reads=["l0", "expb0"], writes=["l0"])
            P.op("dve", CP(osum[:], psO[0:16, :]), reads=["psO"], writes=["osum"])
            P.op("dve", CP(lsum[:], psL[0:16, 0:8]), reads=["psL"], writes=["lsum"])
            for g in range(3):
                vg = zs[:, 3072 + g * 512:3072 + (g + 1) * 512].rearrange("p (h e) -> p h e", e=64)
                e0 = l0[:, g * 8:(g + 1) * 8]
                P.op("dve", TT(tmp5[:].rearrange("p (h e) -> p h e", e=64), vg, e0.unsqueeze(2).to_broadcast([16, 8, 64]), ALU.mult),
                     reads=["zs", "l0"], writes=["tmp5"])
                P.op("dve", TT(osum[:], osum[:], tmp5[:], ALU.add), reads=["tmp5", "osum"], writes=["osum"])
                P.op("dve", TT(lsum[:], lsum[:], e0, ALU.add), reads=["l0", "lsum"], writes=["lsum"])
            P.op("dve", RCP(lsum[:], lsum[:]), reads=["lsum"], writes=["lsum"])
            o3 = osum[:].rearrange("p (h e) -> p h e", e=64)
            P.op("dve", TT(o3, o3, lsum[:].unsqueeze(2).to_broadcast([16, 8, 64]), ALU.mult), reads=["osum", "lsum"], writes=["osum"])
            pt, pn = ps()
            for c in range(4):
                P.op("pe", TR(pt[:, c * 16:(c + 1) * 16], osum[0:16, c * 128:(c + 1) * 128], ident[0:16, 0:16]), reads=["osum", "ident"], writes=[pn])
            P.op("act", ACTV(aoT[:, :, TOK:TOK + 16], pt[:, 0:64].rearrange("p (c t) -> p c t", t=16), AF.Copy), reads=[pn], writes=["aoT_s"])
            P.psmod = 8
          P.barrier()

        if STAGE >= 3:
          with ExitStack() as ph0:
            relb = sbt(ph0, "relb2", (32, 24))
            ohcp = sbt(ph0, "ohcp", (32, 6, 256))
            valt = sbt(ph0, "valt", (8, 2, 256))
            evs = sbt(ph0, "evs", (8, 6, 256))
            P.dma("sp", DMA(relb[:], I["rel_bias"]), "relb2", writes=["relb2"])
            P.dma("sp", DMA(ohcp[:, 0:3, :], C["ohp"].rearrange("g b m -> b g m")), "ohcp", writes=["ohcp"])
            P.dma("sp", DMA(ohcp[:, 3:6, :], C["ohc"].rearrange("g b m -> b g m")), "ohcp", writes=["ohcp"])
            P.dma("sp", DMA(valt[:], C["valid"].rearrange("c h m -> h c m")), "valt", writes=["valt"])
            for g in range(3):
                for cp_ in range(2):
                    pt, pn = ps()
                    P.op("pe", MM(pt[0:8, 0:256], relb[:, g * 8:(g + 1) * 8], ohcp[:, cp_ * 3 + g, :]), reads=["relb2", "ohcp"], writes=[pn])
                    P.op("act", ACTV(evs[:, g * 2 + cp_, :], pt[0:8, 0:256], AF.Exp), reads=[pn], writes=["evs"])
                    P.op("dve", TT(evs[:, g * 2 + cp_, :], evs[:, g * 2 + cp_, :], valt[:, cp_, :], ALU.mult), reads=["evs", "valt"], writes=["evs"])
            P.dma("sp", DMA(evd.rearrange("g c h m -> h (g c) m"), evs[:]), "evd", reads=["evs"], writes=["evd"])
          P.barrier()
        if STAGE >= 3:
          with ExitStack() as ph:
            accE = sbt(ph, "accE", (65, 2, TOK))
            accO = sbt(ph, "accO", (128, 2, TOK))
            qT = sbt(ph, "qT", (128, 2, TOK), BF16)
            kT = sbt(ph, "kT", (128, 2, HALO + TOK), BF16)
            VbE = sbt(ph, "VbE", (128, 32, 2, 66), BF16)
            VbO = sbt(ph, "VbO", (128, 32, 2, 128), BF16)
            wq = sbt(ph, "wq", (128, 8, 256), BF16)
            wk = sbt(ph, "wk", (128, 8, 256), BF16)
            wv = sbt(ph, "wv", (128, 8, 256), BF16)
            mtab = sbt(ph, "mtab", (128, 4, 4, 128))
            Ef = [sbt(ph, "Ef%d" % i, (128, 256)) for i in range(2)]
            Eb = [sbt(ph, "Eb%d" % i, (128, 256), BF16) for i in range(2)]
            knf = [sbt(ph, "knf%d" % i, (128, 512)) for i in range(2)]
            sqb = [sbt(ph, "sqb%d" % i, (128, 512), BF16) for i in range(2)]
            rt = [sbt(ph, "rt%d" % i, (128, 512)) for i in range(2)]
            vst = [sbt(ph, "vst%d" % i, (128, 256)) for i in range(2)]
            kst = [sbt(ph, "kst%d" % i, (128, 128)) for i in range(2)]
            rl = [sbt(ph, "rl%d" % i, (128, 512)) for i in range(2)]
            P.op("pool", MSET(VbE[:, :, :, 64:65], 1.0), writes=["VbE"])
            P.op("pool", MSET(VbO[:, :, :, 0:64], 0.0), writes=["VbO"])
            P.op("pool", MSET(VbO[:, :, :, 0:1], 1.0), writes=["VbO"])
            kvouts = (O["kvp1"], O["kvp2"], O["kvp3"])
            cnt = {"p": 0, "e": 0, "s": 0, "hk": 0, "rl": 0, "pS": 0, "pO": 0, "pp": 0, "pv": 0}

            def psp():
                i_ = cnt["pp"] % 6
                cnt["pp"] += 1
                return PS[i_], "ps%d" % i_

            def psv():
                i_ = 6 + cnt["pv"] % 2
                cnt["pv"] += 1
                return PS[i_], "ps%d" % i_

            for hh in range(2 if CL >= 2 else 0):
                for g in range(3):
                    if CONE and (hh, g) != (0, CONE - 1):
                        continue
                    dil = DILS[g]
                    halo_g = 128 * dil
                    keep = halo_g
                    nb = 16 // dil
                    colq = g * 512 + hh * 256
                    for (wt, wnm, c0) in ((wq, "wq", colq), (wk, "wk", 1536 + colq), (wv, "wv", 3072 + colq)):
                        P.dma("pool", DMA(wt[:], I["w_in"][:, c0:c0 + 256].rearrange("(k p) c -> p k c", p=128)), wnm, writes=[wnm])
                    for hl in range(4):
                        for cp_ in range(2):
                            mi_ = hl * 2 + cp_
                            hkt = rl[mi_ // 4][:, (mi_ % 4) * 128:(mi_ % 4 + 1) * 128]
                            base = evd[g, cp_, hh * 4 + hl, 0:128]
                            hank = bass.AP(tensor=base.tensor, offset=base.offset, ap=[[1, 128], [1, 128]])
                            P.dma("sp", DMA(hkt, hank), "hk%d" % mi_, reads=["evd"], writes=["rl%d" % (mi_ // 4)])
                    if CL < 3:
                        continue
                    def emit_vblock(r, n):
                        bi = r * (nb + 1) + (n + 1)
                        c0 = OWN0 + n * 128 * dil + r
                        hb, hbn, cc = hcols(c0, 128, dil)
                        pv_, pvn_ = psv()
                        for kc in range(8):
                            P.op("pe", MM(pv_[:, 0:256], hb[:, kc, cc:cc + 127 * dil + 1:dil], wv[:, kc, :], kc == 0, kc == 7),
                                 reads=[hbn, "wv"], writes=[pvn_])
                        pv4 = pv_[:, 0:256].rearrange("p (h e) -> p h e", e=64)
                        if VL >= 2:
                            P.op("act", ACTV(VbE[:, bi, :, 0:64], pv4[:, 0:4:2, :], AF.Copy), reads=[pvn_], writes=["VbE"])
                        if VL >= 3:
                            P.op("act", ACTV(VbO[:, bi, :, 64:128], pv4[:, 1:4:2, :], AF.Copy), reads=[pvn_], writes=["VbO"])
                        trel = n * 128 * dil
                        if VL >= 4 and n >= 0 and trel >= TOK - keep:
                            vs_i = cnt["s"] % 2
                            cnt["s"] += 1
                            P.op("dve", CP(vst[vs_i][:], pv_[:, 0:256]), reads=[pvn_], writes=["vst%d" % vs_i])
                            r0 = trel - (TOK - keep) + r
                            dst = kvouts[g].rearrange("(j r) c -> r j c", r=dil)[r, r0 // dil:r0 // dil + 128, 512 + hh * 256:512 + (hh + 1) * 256]
                            finals.append(P.dma("sp", DMA(dst, vst[vs_i][:]), "vst%d" % vs_i, reads=["vst%d" % vs_i], writes=["o_v"]))
                    vlist = [(r, n) for r in range(dil) for n in range(-1, nb)]
                    pj = []
                    for isk in range(2):
                        c_lo = OWN0 - halo_g if isk else OWN0
                        blocks = [(cb, min(512, OWN0 + TOK - cb)) for cb in range(c_lo, OWN0 + TOK, 512)]
                        if isk and halo_g < 512:
                            blocks = [(c_lo, halo_g)] + [(cb, 512) for cb in range(OWN0, OWN0 + TOK, 512)]
                        for c in range(2):
                            for (cb, nn) in blocks:
                                pj.append((isk, c, cb, nn))
                    stp = {}
                    for i in range(len(pj) + 1):
                        if i < len(pj):
                            isk, c, cb, nn = pj[i]
                            wt, wnm = (wk, "wk") if isk else (wq, "wq")
                            hb, hbn, cc = hcols(cb, nn)
                            i2 = cnt["p"] % 2
                            cnt["p"] += 1
                            pz, pzn = psp()
                            for kc in range(8):
                                P.op("pe", MM(pz[:, 0:nn], wt[:, kc, c * 128:(c + 1) * 128], hb[:, kc, cc:cc + nn], kc == 0, kc == 7),
                                     reads=[wnm, hbn], writes=[pzn])
                            P.op("act", ACTV(sqb[i2][:, 0:nn], pz[:, 0:nn], AF.Square), reads=[pzn], writes=["sqb%d" % i2])
                            stp[i] = (pz, pzn, i2)
                            if vlist and CL >= 4:
                                emit_vblock(*vlist.pop(0))
                        j = i - 1
                        if j < 0:
                            continue
                        isk, c, cb, nn = pj[j]
                        pz, pzn, i2 = stp.pop(j)
                        gcol = gk if isk else gq
                        gn = "gk" if isk else "gq"
                        pm, pmn = psp()
                        P.op("pe", MM(pm[:, 0:nn], bones_b[:], sqb[i2][:, 0:nn]), reads=["bones_b", "sqb%d" % i2], writes=[pmn])
                        P.op("act", ACTV(rt[i2][:, 0:nn], pm[:, 0:nn], AF.Ln, bias=epst[:, 0:1]), reads=[pmn, "epst"], writes=["rt%d" % i2])
                        P.op("act", ACTV(rt[i2][:, 0:nn], rt[i2][:, 0:nn], AF.Exp, scale=-0.5), reads=["rt%d" % i2], writes=["rt%d" % i2])
                        if not isk:
                            P.op("dve", STT(qT[:, c, cb - OWN0:cb - OWN0 + nn], pz[:, 0:nn], gcol[:, 0:1], rt[i2][:, 0:nn], ALU.mult, ALU.mult),
                                 reads=[pzn, gn, "rt%d" % i2], writes=["qT"])
                        else:
                            P.op("dve", STT(knf[i2][:, 0:nn], pz[:, 0:nn], gcol[:, 0:1], rt[i2][:, 0:nn], ALU.mult, ALU.mult),
                                 reads=[pzn, gn, "rt%d" % i2], writes=["knf%d" % i2])
                            P.op("act", ACTV(kT[:, c, cb:cb + nn], knf[i2][:, 0:nn], AF.Copy), reads=["knf%d" % i2], writes=["kT"])
                            pk, pkn = None, None
                            for t0 in range(cb, cb + nn, 128):
                                trel = t0 - OWN0
                                if trel < TOK - keep or trel < 0:
                                    continue
                                ks_i = cnt["s"] % 2
                                cnt["s"] += 1
                                if pk is None:
                                    pk, pkn = psp()
                                qo = ((t0 - cb) // 128) * 128
                                P.op("pe", TR(pk[:, qo:qo + 128], knf[i2][:, t0 - cb:t0 - cb + 128], ident[:]), reads=["knf%d" % i2, "ident"], writes=[pkn])
                                P.op("act", ACTV(kst[ks_i][:], pk[:, qo:qo + 128], AF.Copy), reads=[pkn], writes=["kst%d" % ks_i])
                                r0 = trel - (TOK - keep)
                                finals.append(P.dma("sp", DMA(kvouts[g][r0:r0 + 128, hh * 256 + c * 128:hh * 256 + (c + 1) * 128], kst[ks_i][:]),
                                                    "kst%d" % ks_i, reads=["kst%d" % ks_i], writes=["o_k"]))
                    while vlist:
                        emit_vblock(*vlist.pop(0))
                    if CL < 5:
                        continue
                    for hl in range(4):
                        for cp_ in range(2):
                            mi_ = hl * 2 + cp_
                            hkt = rl[mi_ // 4][:, (mi_ % 4) * 128:(mi_ % 4 + 1) * 128]
                            pt, pn = ps()
                            P.op("pe", MM(pt[:, 0:128], jflip[:], hkt), reads=["jflip", "rl%d" % (mi_ // 4)], writes=[pn])
                            P.op("act", ACTV(mtab[:, hl, cp_, :], pt[:, 0:128], AF.Copy), reads=[pn], writes=["mtab"])
                            if cp_ == 0:
                                P.op("dve", TS(mtab[:, hl, 2, :], pt[:, 0:128], hpt[:, 0:1], None, ALU.mult), reads=[pn, "hpt"], writes=["mtab"])
                            else:
                                P.op("dve", CP(mtab[:, hl, 3, :], pt[:, 0:128]), reads=[pn], writes=["mtab"])
                    its = [(r, n, hl) for r in range(dil) for n in range(nb) for hl in range(4)]
                    DEPTH = 3
                    stg = {}
                    for i in range(len(its) + DEPTH):
                        if i < len(its):
                            r, n, hl = its[i]
                            q0 = n * 128 * dil + r
                            kc0 = OWN0 + q0
                            kp0 = kc0 - 128 * dil
                            c = hl // 2
                            pb = (hl % 2) * 64
                            pS, pSn = PS[cnt["pS"] % 5], "ps%d" % (cnt["pS"] % 5)
                            cnt["pS"] += 1
                            qsl = qT[pb:pb + 64, c, q0:q0 + 127 * dil + 1:dil]
                            P.op("pe", MM(pS[:, 0:128], kT[pb:pb + 64, c, kp0:kp0 + 127 * dil + 1:dil], qsl), reads=["kT", "qT"], writes=[pSn])
                            P.op("pe", MM(pS[:, 128:256], kT[pb:pb + 64, c, kc0:kc0 + 127 * dil + 1:dil], qsl), reads=["kT", "qT"], writes=[pSn])
                            stg[i] = (pS, pSn)
                        j = i - DEPTH
                        if j < 0:
                            continue
                        r, n, hl = its[j]
                        pS, pSn = stg.pop(j)
                        bprev = r * (nb + 1) + n
                        bcur = bprev + 1
                        q0 = n * 128 * dil + r
                        ei = cnt["e"] % 2
                        cnt["e"] += 1
                        P.op("act", ACTV(Ef[ei][:], pS[:, 0:256], AF.Exp), reads=[pSn], writes=["Ef%d" % ei])
                        mi = 2 if n == 0 else 0
                        P.op("dve", TT(Eb[ei][:].rearrange("p (a k) -> p a k", a=2), Ef[ei][:].rearrange("p (a k) -> p a k", a=2),
                                       mtab[:, hl, mi:mi + 2, :], ALU.mult), reads=["Ef%d" % ei, "mtab"], writes=["Eb%d" % ei])
                        pO, pOn = PS[5 + cnt["pO"] % 3], "ps%d" % (5 + cnt["pO"] % 3)
                        cnt["pO"] += 1
                        if hl % 2 == 0:
                            P.op("pe", MM(pO[0:65, 0:128], VbE[:, bprev, hl // 2, 0:65], Eb[ei][:, 0:128], True, False), reads=["VbE", "Eb%d" % ei], writes=[pOn])
                            P.op("pe", MM(pO[0:65, 0:128], VbE[:, bcur, hl // 2, 0:65], Eb[ei][:, 128:256], False, True), reads=["VbE", "Eb%d" % ei], writes=[pOn])
                            av = accE[0:65, hl // 2, q0:q0 + 127 * dil + 1:dil]
                            src = pO[0:65, 0:128]
                        else:
                            P.op("pe", MM(pO[:, 0:128], VbO[:, bprev, hl // 2, :], Eb[ei][:, 0:128], True, False), reads=["VbO", "Eb%d" % ei], writes=[pOn])
                            P.op("pe", MM(pO[:, 0:128], VbO[:, bcur, hl // 2, :], Eb[ei][:, 128:256], False, True), reads=["VbO", "Eb%d" % ei], writes=[pOn])
                            av = accO[:, hl // 2, q0:q0 + 127 * dil + 1:dil]
                            src = pO[:, 0:128]
                        an = "acc_%d_%d_%d_%d" % (g, hl, r, n)
                        if g == 0:
                            cnt["fence"] = P.op("dve", CP(av, src), reads=[pOn], writes=[an], extra=cnt.get("prevfence", []))
                        else:
                            cnt["fence"] = P.op("dve", TT(av, src, av, ALU.add), reads=[pOn], writes=[an], extra=cnt.get("prevfence", []))
                    cnt["prevfence"] = [cnt["fence"]]
                for hl in range(4 if CL >= 6 else 0):
                    pass
                for hl in range(4 if CL >= 6 else 0):
                    for tb in range(4):
                        ri = cnt["rl"] % 2
                        cnt["rl"] += 1
                        pt, pn = ps()
                        cs_ = slice(tb * 512, (tb + 1) * 512)
                        if hl % 2 == 0:
                            P.op("pe", MM(pt[0:64, :], ones_f[64:65, 0:64], accE[64:65, hl // 2, cs_]), reads=["ones_f", "accE"], writes=[pn], extra=cnt.get("prevfence", []))
                            P.op("act", ACTV(rl[ri][0:64, :], pt[0:64, :], AF.Ln), reads=[pn], writes=["rl%d" % ri])
                            P.op("act", ACTV(rl[ri][0:64, :], rl[ri][0:64, :], AF.Exp, scale=-1.0), reads=["rl%d" % ri], writes=["rl%d" % ri])
                            cnt["finfence"] = P.op("dve", TT(aoT[0:64, hh * 2 + hl // 2, cs_], accE[0:64, hl // 2, cs_], rl[ri][0:64, :], ALU.mult),
                                 reads=["accE", "rl%d" % ri], writes=["aoT"], extra=cnt.get("prevfence", []))
                        else:
                            P.op("pe", MM(pt[:, :], zo_f[0:1, :], accO[0:1, hl // 2, cs_]), reads=["zo_f", "accO"], writes=[pn], extra=cnt.get("prevfence", []))
                            P.op("act", ACTV(rl[ri][64:128, :], pt[64:128, :], AF.Ln), reads=[pn], writes=["rl%d" % ri])
                            P.op("act", ACTV(rl[ri][64:128, :], rl[ri][64:128, :], AF.Exp, scale=-1.0), reads=["rl%d" % ri], writes=["rl%d" % ri])
                            cnt["finfence"] = P.op("dve", TT(aoT[64:128, hh * 2 + hl // 2, cs_], accO[64:128, hl // 2, cs_], rl[ri][64:128, :], ALU.mult),
                                 reads=["accO", "rl%d" % ri], writes=["aoT"], extra=cnt.get("prevfence", []))
                if "finfence" in cnt:
                    cnt["prevfence"] = [cnt["finfence"]]
          P.barrier()
        sc_h.close()
        poT = sbt(sc_o, "poT", (128, 4, NTOK), BF16)

        BIG = 1.0e6
        TB = [(0, 512), (512, 512), (1024, 512), (1536, 512), (2048, 16)]
        if STAGE >= 4:
          with ExitStack() as ph:
            uT = sbt(ph, "uT", (128, 4, 16 + TOK))
            uTs = sbt(ph, "uTs", (128, 4, 16))
            sA = sbt(ph, "sA", (128, 16 + TOK))
            sB = sbt(ph, "sB", (128, 16 + TOK))
            pl = sbt(ph, "pl", (128, 4, NTOK), BF16)
            wu = sbt(ph, "wu", (128, 8, 512), BF16)
            wpm = sbt(ph, "wpm", (128, 4, 128), BF16)
            pscale = sbt(ph, "pscale", (128, 4))
            corr = sbt(ph, "corr", (128, 4, 16))
            tmpc = sbt(ph, "tmpc", (128, 16))
            spb = sbt(ph, "spb", (16, 26, 128))
            unew = sbt(ph, "unew", (16, 512))
            ulast = sbt(ph, "ulast", (16, 512))
            ssum = sbt(ph, "ssum", (16, 128))
            pls = sbt(ph, "pls", (16, 512))
            P.dma("pool", DMA(wu[:], I["w_in"][:, 4608:5120].rearrange("(k p) c -> p k c", p=128)), "wu", writes=["wu"])
            P.dma("pool", DMA(wpm[:], I["w_pool_mix"].rearrange("g c d -> c g d")), "wpm", writes=["wpm"])
            P.dma("sp", DMA(pscale[:], I["pool_scale"].rearrange("o (g p) -> p (o g)", p=128), allow_slow_non_contiguous=True), "pscale", writes=["pscale"])
            P.dma("sp", DMA(corr[:], C["poolcorr"].rearrange("p (g t) -> p g t", t=16)), "corr", writes=["corr"])
            r0 = 0
            sp_off = []
            for gi, w in enumerate((2, 4, 8, 16)):
                P.dma("sp", DMA(spb[:, r0:r0 + w - 1, :], I["spool"][:, 15 - (w - 1):15, gi * 128:(gi + 1) * 128]), "spb", writes=["spb"])
                sp_off.append(r0)
                r0 += w - 1
            finals.append(P.dma("sp", DMA(O["pools"][:, 0:14, :], I["spool"][:, 1:15, :]), "pools_cp", writes=["o_pools_a"]))
            for gi in range(4):
                for (c0, nn, dst) in [(OWN0 - 16, 16, uT[:, gi, 0:16])] + [(OWN0 + tb * 512, 512, uT[:, gi, 16 + tb * 512:16 + (tb + 1) * 512]) for tb in range(4)] + [(SMP0, 16, uTs[:, gi, :])]:
                    hb, hbn, cc = hcols(c0, nn) if c0 >= OWN0 else (None, None, None)
                    pt, pn = ps()
                    if c0 < OWN0:
                        hb, hbn, cc = hlast, "hlast", 0
                    for kc in range(8):
                        P.op("pe", MM(pt[:, 0:nn], wu[:, kc, gi * 128:(gi + 1) * 128], hb[:, kc, cc:cc + nn], kc == 0, kc == 7), reads=["wu", hbn], writes=[pn])
                    if c0 < OWN0:
                        P.op("dve", TS(dst, pt[:, 0:nn], hpt[:, 0:1], None, ALU.mult), reads=[pn, "hpt"], writes=["uT"])
                    else:
                        P.op("act", ACTV(dst, pt[:, 0:nn], AF.Copy), reads=[pn], writes=["uT" if c0 < SMP0 else "uTs"])
            for (srcT, sn, dstt, dn) in ((uT, "uT", ulast, "ulast"), (uTs, "uTs", unew, "unew")):
                pt, pn = ps()
                for gi in range(4):
                    sl = srcT[:, gi, 16 + TOK - 16:16 + TOK] if sn == "uT" else srcT[:, gi, :]
                    P.op("pe", TR(pt[0:16, gi * 128:(gi + 1) * 128], sl, ident[:]), reads=[sn, "ident"], writes=[pn])
                P.op("act", ACTV(dstt[:], pt[0:16, :], AF.Copy), reads=[pn], writes=[dn])
            finals.append(P.dma("sp", DMA(O["poolp"], ulast[:]), "poolp", reads=["ulast"], writes=["o_poolp"]))
            finals.append(P.dma("sp", DMA(O["pools"][:, 14, :], unew[:]), "pools_n", reads=["unew"], writes=["o_pools_b"]))
            for gi, w in enumerate((2, 4, 8, 16)):
                cur, curn = uT[:, gi, :], "uT"
                bufs = [(sA, "sA"), (sB, "sB")]
                lo = 0
                for step in range(gi + 1):
                    sh = 1 << step
                    dstb, dstn = bufs[step % 2]
                    P.op("dve", TT(dstb[:, lo + sh:16 + TOK], cur[:, lo + sh:16 + TOK], cur[:, lo:16 + TOK - sh], ALU.add),
                         reads=[curn], writes=[dstn])
                    cur, curn = dstb[:, :], dstn
                    lo += sh
                P.op("dve", STT(pl[:, gi, 0:TOK], cur[:, 16:16 + TOK], 1.0 / w, uT[:, gi, 16:16 + TOK], ALU.mult, ALU.subtract),
                     reads=[curn, "uT"], writes=["pl"])
                P.op("dve", TT(tmpc[:], cur[:, 16:32], corr[:, gi, :], ALU.mult), reads=[curn, "corr"], writes=["tmpc"])
                P.op("dve", TT(pl[:, gi, 0:16], tmpc[:], uT[:, gi, 16:32], ALU.subtract), reads=["tmpc", "uT"], writes=["pl"])
                P.op("dve", lambda E, gi=gi, w=w: E.tensor_reduce(out=ssum[:], in_=spb[:, sp_off[gi]:sp_off[gi] + w - 1, :].rearrange("p r c -> p c r"),
                                                                 axis=AX.X, op=ALU.add), reads=["spb"], writes=["ssum"])
                P.op("dve", TT(ssum[:], ssum[:], unew[:, gi * 128:(gi + 1) * 128], ALU.add), reads=["ssum", "unew"], writes=["ssum"])
                P.op("dve", STT(pls[:, gi * 128:(gi + 1) * 128], ssum[:], 1.0 / w, unew[:, gi * 128:(gi + 1) * 128], ALU.mult, ALU.subtract),
                     reads=["ssum", "unew"], writes=["pls"])
            pt, pn = ps()
            for gi in range(4):
                P.op("pe", TR(pt[:, gi * 16:(gi + 1) * 16], pls[0:16, gi * 128:(gi + 1) * 128], ident[0:16, 0:16]), reads=["pls", "ident"], writes=[pn])
            P.op("act", ACTV(pl[:, :, TOK:TOK + 16], pt[:, 0:64].rearrange("p (c t) -> p c t", t=16), AF.Copy), reads=[pn], writes=["pl"])
            for gi in range(4):
                for (c0, nn) in TB:
                    pt, pn = ps()
                    P.op("pe", MM(pt[:, 0:nn], wpm[:, gi, :], pl[:, gi, c0:c0 + nn]), reads=["wpm", "pl"], writes=[pn])
                    P.op("act", ACTV(poT[:, gi, c0:c0 + nn], pt[:, 0:nn], AF.Copy, scale=pscale[:, gi:gi + 1]), reads=[pn, "pscale"], writes=["poT"])
          P.barrier()

        if STAGE >= 4:
          mgT = sbt(sc_o, "mgT", (128, 8, NTOK), BF16)
          with ExitStack() as ph:
            wua = sbt(ph, "wua", (128, 4, D), BF16)
            wup = sbt(ph, "wup", (128, 4, D), BF16)
            wga = sbt(ph, "wga", (128, 8, D), BF16)
            wgp = sbt(ph, "wgp", (128, 8, D), BF16)
            sa = [sbt(ph, "sa%d" % i, (128, 512)) for i in range(2)]
            sp_ = [sbt(ph, "sp_%d" % i, (128, 512)) for i in range(2)]
            m1 = [sbt(ph, "m1%d" % i, (128, 512)) for i in range(2)]
            m2 = [sbt(ph, "m2%d" % i, (128, 512)) for i in range(2)]
            P.dma("pool", DMA(wua[:], I["w_up_attn"].rearrange("(k p) c -> p k c", p=128)), "wua", writes=["wua"])
            P.dma("pool", DMA(wup[:], I["w_up_pool"].rearrange("(k p) c -> p k c", p=128)), "wup", writes=["wup"])
            P.dma("pool", DMA(wga[:], I["w_in"][:, 5120:6144].rearrange("(k p) c -> p k c", p=128)), "wga", writes=["wga"])
            P.dma("pool", DMA(wgp[:], I["w_in"][:, 6144:7168].rearrange("(k p) c -> p k c", p=128)), "wgp", writes=["wgp"])
            it = 0
            for (c0, nn) in TB:
                for j in range(8):
                    i2 = it % 2
                    it += 1
                    js = slice(j * 128, (j + 1) * 128)
                    pA, pAn = ps()
                    pB, pBn = ps()
                    pGa, pGan = ps()
                    pGp, pGpn = ps()
                    for kc in range(4):
                        P.op("pe", MM(pA[:, 0:nn], wua[:, kc, js], aoT[:, kc, c0:c0 + nn], kc == 0, kc == 3), reads=["wua", "aoT", "aoT_s"], writes=[pAn])
                    for kc in range(4):
                        P.op("pe", MM(pB[:, 0:nn], wup[:, kc, js], poT[:, kc, c0:c0 + nn], kc == 0, kc == 3), reads=["wup", "poT"], writes=[pBn])
                    for kc in range(8):
                        P.op("pe", MM(pGa[:, 0:nn], wga[:, kc, js], hTo[:, kc, c0:c0 + nn], kc == 0, kc == 7), reads=["wga", "hTo"], writes=[pGan])
                    for kc in range(8):
                        P.op("pe", MM(pGp[:, 0:nn], wgp[:, kc, js], hTo[:, kc, c0:c0 + nn], kc == 0, kc == 7), reads=["wgp", "hTo"], writes=[pGpn])
                    P.op("act", ACTV(sa[i2][:, 0:nn], pGa[:, 0:nn], AF.Sigmoid), reads=[pGan], writes=["sa%d" % i2])
                    P.op("act", ACTV(sp_[i2][:, 0:nn], pGp[:, 0:nn], AF.Sigmoid), reads=[pGpn], writes=["sp_%d" % i2])
                    P.op("dve", TT(m1[i2][:, 0:nn], pA[:, 0:nn], sa[i2][:, 0:nn], ALU.mult), reads=[pAn, "sa%d" % i2], writes=["m1%d" % i2])
                    P.op("dve", TT(m2[i2][:, 0:nn], pB[:, 0:nn], sp_[i2][:, 0:nn], ALU.mult), reads=[pBn, "sp_%d" % i2], writes=["m2%d" % i2])
                    P.op("dve", TT(mgT[:, j, c0:c0 + nn], m1[i2][:, 0:nn], m2[i2][:, 0:nn], ALU.add), reads=["m1%d" % i2, "m2%d" % i2], writes=["mgT"])
          P.barrier()

        if STAGE >= 4:
          with ExitStack() as ph:
            wo = sbt(ph, "wo", (128, 8, D), BF16)
            wr = sbt(ph, "wr", (128, 8, 32))
            brb = sbt(ph, "brb", (128, 32))
            iot = sbt(ph, "iot", (128, 32))
            gt1b = sbt(ph, "gt1b", (128, D))
            a2b = sbt(ph, "a2b", (128, D))
            b2b = sbt(ph, "b2b", (128, D))
            gt1s = sbt(ph, "gt1s", (16, D))
            a2s = sbt(ph, "a2s", (16, D))
            b2s = sbt(ph, "b2s", (16, D))
            xts = [sbt(ph, "xd%d" % i, (128, D)) for i in range(2)]
            x1t = [sbt(ph, "x1t%d" % i, (128, D)) for i in range(2)]
            h2t = [sbt(ph, "h2t%d" % i, (128, D)) for i in range(2)]
            h2b = [sbt(ph, "h2b%d" % i, (128, D), BF16) for i in range(2)]
            junk = sbt(ph, "junk2", (128, D), BF16)
            h2T = sbt(ph, "h2T", (128, 8, 128))
            ssq = sbt(ph, "ssq2", (128, 1))
            rs = sbt(ph, "rs2", (128, 1))
            carry = sbt(ph, "carry", (128, 32))
            lg = sbt(ph, "lg", (128, 32))
            t8 = sbt(ph, "t8", (128, 8))
            mk = sbt(ph, "mk", (128, 32))
            mkb = sbt(ph, "mkb", (128, 32), BF16)
            nm = sbt(ph, "nm", (128, 1))
            ex = sbt(ph, "ex", (128, 32))
            den = sbt(ph, "den", (128, 1))
            G = sbt(ph, "G", (128, 32))
            posf = sbt(ph, "posf", (128, 32))
            v2 = sbt(ph, "v2", (128, 32))
            rowsel = sbt(ph, "rowsel", (128, 32))
            ohk = sbt(ph, "ohk", (128, 32))
            ohk2 = sbt(ph, "ohk2", (128, 32))
            posTs = sbt(ph, "posTs", (32, 128))
            P.dma("pool", DMA(wo[:], I["w_out"].rearrange("(k p) c -> p k c", p=128)), "wo", writes=["wo"])
            P.dma("sp", DMA(wr[:], I["w_router"].rearrange("(k p) c -> p k c", p=128)), "wr", writes=["wr"])
            P.dma("sp", DMA(brb[:], I["b_router"].partition_broadcast(128)), "brb", writes=["brb"])
            P.dma("sp", DMA(iot[:], C["iota32"]), "iot", writes=["iot"])
            load_mod("sp", gt1b, "gt1b", MGT1, False, "gt1b")
            load_mod("sp", a2b, "a2b", MA2, False, "a2b")
            load_mod("sp", b2b, "b2b", MSH2, False, "b2b")
            load_mod("sp", gt1s, "gt1s", MGT1, True, "gt1s")
            load_mod("sp", a2s, "a2s", MA2, True, "a2s")
            load_mod("sp", b2s, "b2s", MSH2, True, "b2s")
            P.op("dve", MSET(carry[:], 0.0), writes=["carry"])
            def dc_stage_a(ti):
                smp = ti == 16
                npart = 16 if smp else 128
                c0 = TOK if smp else ti * 128
                i2 = ti % 2
                xt, xn = xts[i2], "xd%d" % i2
                x1, x1n = x1t[i2], "x1t%d" % i2
                h2, h2n = h2t[i2], "h2t%d" % i2
                hb_, hbn_ = h2b[i2], "h2b%d" % i2
                gt, gtn = (gt1s, "gt1s") if smp else (gt1b, "gt1b")
                for half in range(2):
                    hs = slice(half * 512, (half + 1) * 512)
                    pt, pn = ps()
                    for kc in range(8):
                        P.op("pe", MM(pt[0:npart, :], mgT[:, kc, c0:c0 + npart], wo[:, kc, hs], kc == 0, kc == 7), reads=["mgT", "wo"], writes=[pn])
                    P.op("dve", TT(x1[0:npart, hs], pt[0:npart, :], gt[0:npart, hs], ALU.mult), reads=[pn, gtn], writes=[x1n])
                P.op("dve", TT(x1[0:npart, :], x1[0:npart, :], xt[0:npart, :], ALU.add), reads=[x1n, xn], writes=[x1n])
                P.dma("sp", DMA(x1d[c0:c0 + npart, :], x1[0:npart, :]), "x1d_%d" % i2, reads=[x1n], writes=["x1d"])
                norm_tile(x1, x1n, npart, a2s if smp else a2b, "a2s" if smp else "a2b", b2s if smp else b2b, "b2s" if smp else "b2b",
                          h2, h2n, junk, "junk2", ssq, rs, "D")
                P.op("act", ACTV(hb_[0:npart, :], h2[0:npart, :], AF.Copy), reads=[h2n], writes=[hbn_])

            def dc_stage_b(ti):
                smp = ti == 16
                npart = 16 if smp else 128
                c0 = TOK if smp else ti * 128
                i2 = ti % 2
                xt, xn = xts[i2], "xd%d" % i2
                x1, x1n = x1t[i2], "x1t%d" % i2
                h2, h2n = h2t[i2], "h2t%d" % i2
                hb_, hbn_ = h2b[i2], "h2b%d" % i2
                p0, p0n = ps()
                p1, p1n = ps()
                for kc in range(8):
                    pp, ppn = (p0, p0n) if kc < 4 else (p1, p1n)
                    P.op("pe", TR(pp[:, (kc % 4) * 128:(kc % 4) * 128 + npart], h2[0:npart, kc * 128:(kc + 1) * 128], ident[0:npart, 0:npart]),
                         reads=[h2n, "ident"], writes=[ppn])
                for half, (pp, ppn) in enumerate(((p0, p0n), (p1, p1n))):
                    P.op("dve", CP(h2T[:, half * 4:(half + 1) * 4, 0:npart], pp[:].rearrange("p (k t) -> p k t", t=128)[:, :, 0:npart]),
                         reads=[ppn], writes=["h2T"])
                pr_, prn = ps()
                for kc in range(8):
                    P.op("pe", MM(pr_[0:npart, 0:32], h2T[:, kc, 0:npart], wr[:, kc, :], kc == 0, kc == 7), reads=["h2T", "wr"], writes=[prn])
                NP = slice(0, npart)
                P.op("dve", TT(lg[NP, :], pr_[NP, 0:32], brb[NP, :], ALU.add), reads=[prn, "brb"], writes=["lg"])
                P.op("dve", lambda E, NP=NP: E.max(out=t8[NP, :], in_=lg[NP, :]), reads=["lg"], writes=["t8"])
                P.op("dve", TS(mk[NP, :], lg[NP, :], t8[NP, 3:4], None, ALU.is_ge), reads=["lg", "t8"], writes=["mk"])
                P.op("dve", TS(mkb[NP, :], lg[NP, :], t8[NP, 3:4], None, ALU.is_ge), reads=["lg", "t8"], writes=["mkb"])
                P.op("dve", TS(nm[NP, :], t8[NP, 0:1], -1.0, None, ALU.mult), reads=["t8"], writes=["nm"])
                P.op("act", ACTV(ex[NP, :], lg[NP, :], AF.Exp, bias=nm[NP, 0:1]), reads=["lg", "nm"], writes=["ex"])
                P.op("dve", TT(ex[NP, :], ex[NP, :], mk[NP, :], ALU.mult), reads=["ex", "mk"], writes=["ex"])
                P.op("dve", lambda E, NP=NP: E.tensor_reduce(out=den[NP, :], in_=ex[NP, :], axis=AX.X, op=ALU.add), reads=["ex"], writes=["den"])
                P.op("dve", RCP(den[NP, :], den[NP, :]), reads=["den"], writes=["den"])
                P.op("dve", TS(G[NP, :], ex[NP, :], den[NP, 0:1], None, ALU.mult), reads=["ex", "den"], writes=["G"])
                pp_, ppn_ = ps()
                P.op("pe", MM(pp_[0:npart, 0:32], ustr_b[0:npart, 0:npart], mkb[0:npart, :]), reads=["ustr_b", "mkb"], writes=[ppn_])
                P.op("dve", TT(posf[NP, :], pp_[NP, 0:32], carry[NP, :], ALU.add), reads=[ppn_, "carry"], writes=["posf"])
                pc_, pcn_ = ps()
                P.op("pe", MM(pc_[:, 0:32], ones_b[0:npart, :], mkb[0:npart, :]), reads=["ones_b", "mkb"], writes=[pcn_])
                P.op("dve", TT(carry[:], carry[:], pc_[:, 0:32], ALU.add), reads=[pcn_, "carry", "posf"], writes=["carry"])
                P.op("dve", TS(v2[NP, :], posf[NP, :], float(CG), None, ALU.is_lt), reads=["posf"], writes=["v2"])
                P.op("dve", TT(v2[NP, :], v2[NP, :], mk[NP, :], ALU.mult), reads=["v2", "mk"], writes=["v2"])
                P.op("dve", TS(rowsel[NP, :], posf[NP, :], 1.0, None, ALU.add), reads=["posf"], writes=["rowsel"])
                P.op("dve", TT(rowsel[NP, :], rowsel[NP, :], v2[NP, :], ALU.mult), reads=["rowsel", "v2"], writes=["rowsel"])
                P.op("dve", TS(rowsel[NP, :], rowsel[NP, :], -1.0, None, ALU.add), reads=["rowsel"], writes=["rowsel"])
                P.dma("sp", DMA(posd[c0:c0 + npart, :], rowsel[NP, :]), "posd", reads=["rowsel"], writes=["posd"])
                P.dma("sp", DMA(gd[c0:c0 + npart, :], G[NP, :]), "gd", reads=["G"], writes=["gd"])
                P.dma("sp", DMA(h2d[c0:c0 + npart, :], hb_[NP, :]), "h2d_%d" % i2, reads=[hbn_], writes=["h2d"])
                pq, pqn = ps()
                P.op("pe", TR(pq[0:32, 0:npart], rowsel[NP, :], ident[0:npart, 0:npart]), reads=["rowsel", "ident"], writes=[pqn])
                P.op("act", ACTV(posTs[:, 0:npart], pq[0:32, 0:npart], AF.Copy), reads=[pqn], writes=["posTs"])
                P.dma("sp", DMA(posTd[:, c0:c0 + npart], posTs[:, 0:npart]), "posTd", reads=["posTs"], writes=["posTd"])
                if ti == 7:
                    P.op("dve", MSET(carry[:], 0.0), reads=["carry"], writes=["carry"])

            def dc_load(ti):
                smp = ti == 16
                npart = 16 if smp else 128
                P.dma("sp", DMA(xts[ti % 2][0:npart, :], I["xs"] if smp else I["xh"][HALO + ti * 128:HALO + (ti + 1) * 128, :]),
                      "xd%d" % (ti % 2), writes=["xd%d" % (ti % 2)])

            dc_load(0)
            for tt_ in range(18):
                if tt_ < 17:
                    dc_stage_a(tt_)
                if tt_ + 1 < 17:
                    dc_load(tt_ + 1)
                if tt_ >= 1:
                    dc_stage_b(tt_ - 1)
          P.barrier()
        sc_o.close()

        if STAGE >= 5:
          with ExitStack() as ph:
            bguT = sbt(ph, "bguT", (128, 16, 32))
            Ghl = sbt(ph, "Ghl", (128, 17, 32, 2), BF16)
            with ExitStack() as ph2:
                bgu = sbt(ph2, "bgu", (32, 2 * D))
                G_tm = sbt(ph2, "G_tm", (128, 17, 32))
                Gf = sbt(ph2, "Gf", (128, 17, 32))
                P.op("dve", MSET(G_tm[:], 0.0), writes=["G_tm"])
                P.dma("sp", DMA(G_tm[:, 0:16, :], gd[0:TOK, :].rearrange("(t p) e -> p t e", p=128)), "G_tm", reads=["G_tm"], writes=["G_tm"])
                P.dma("sp", DMA(G_tm[0:16, 16, :], gd[TOK:NTOK, :]), "G_tm", reads=["G_tm"], writes=["G_tm"])
                P.op("dve", CP(Ghl[:, :, :, 0], G_tm[:]), reads=["G_tm"], writes=["Ghl"])
                P.op("dve", CP(Gf[:], Ghl[:, :, :, 0]), reads=["Ghl"], writes=["Gf"])
                P.op("dve", TT(Gf[:], G_tm[:], Gf[:], ALU.subtract), reads=["G_tm", "Gf"], writes=["Gf"])
                P.op("dve", CP(Ghl[:, :, :, 1], Gf[:]), reads=["Gf"], writes=["Ghl"])
                P.dma("sp", DMA(bgu[:], I["b_gate_up"]), "bgu", writes=["bgu"])
                pt, pn = ps()
                for fc in range(16):
                    P.op("pe", TR(pt[:, fc * 32:(fc + 1) * 32], bgu[0:32, fc * 128:(fc + 1) * 128], ident[0:32, 0:32]), reads=["bgu", "ident"], writes=[pn])
                P.op("dve", CP(bguT[:].rearrange("p f e -> p (f e)"), pt[:, :]), reads=[pn], writes=["bguT"])
            P.barrier()
            wpc = [sbt(ph, "wpc%d" % i, (128, 8, 512), BF16) for i in range(2)]
            wd = sbt(ph, "wd", (128, 8, D), BF16)
            bdb = sbt(ph, "bdb", (128, D))
            bdbg = sbt(ph, "bdbg", (128, 512))
            h2bf = sbt(ph, "h2bf", (128, 17, D), BF16)
            oacc = sbt(ph, "oacc", (128, 17, D))
            Pms = [sbt(ph, "Pm%d_" % i, (128, 9, CG), BF16) for i in range(2)]
            xsT = [sbt(ph, "xsT%d" % i, (128, 8, CG), BF16) for i in range(2)]
            actT = [sbt(ph, "actT%d" % i, (128, 8, CG), BF16) for i in range(2)]
            posbc = sbt(ph, "posbc", (128, 1040))
            STm = sbt(ph, "STm", (128, 3, 1040), BF16)
            ygt = sbt(ph, "ygt", (128, 3, D), BF16)
            gs = [sbt(ph, "gs%d" % i, (128, 4)) for i in range(2)]
            gs2 = sbt(ph, "gs2", (128, 2))
            hgc = [sbt(ph, "hgc%d" % i, (128, CG)) for i in range(2)]
            sg = [sbt(ph, "sg%d" % i, (128, CG)) for i in range(2)]
            hu = [sbt(ph, "hu%d" % i, (128, CG)) for i in range(2)]
            pos_tm = sbt(ph, "pos_tm", (128, 17, 32))
            iotac = sbt(ph, "iotac", (128, CG))
            iotaps = sbt(ph, "iotaps", (128, 3))
            P.dma("sp", DMA(iotac[:], C["iotac"]), "iotac", writes=["iotac"])
            P.dma("sp", DMA(iotaps[:], C["iotaps"]), "iotaps", writes=["iotaps"])
            P.op("dve", MSET(pos_tm[:], -1.0), writes=["pos_tm"])
            P.op("pool", MSET(h2bf[:, 16, :], 0.0), writes=["h2bf"])
            P.dma("sp", DMA(pos_tm[:, 0:16, :], posd[0:TOK, :].rearrange("(t p) e -> p t e", p=128)), "pos_tm", reads=["pos_tm"], writes=["pos_tm"])
            P.dma("sp", DMA(pos_tm[0:16, 16, :], posd[TOK:NTOK, :]), "pos_tm", reads=["pos_tm"], writes=["pos_tm"])
            P.dma("sp", DMA(h2bf[:, 0:16, :], h2d[0:TOK, :].rearrange("(t p) d -> p t d", p=128)), "h2bf", reads=["h2bf"], writes=["h2bf"])
            P.dma("sp", DMA(h2bf[0:16, 16, :], h2d[TOK:NTOK, :]), "h2bf", reads=["h2bf"], writes=["h2bf"])
            HALVES = ((0, 8), (8, 17))
            npc = 0

            def build_P(e_, only=None):
                for hf_, (a_, b_) in enumerate(HALVES):
                    if only is not None and hf_ != only:
                        continue
                    for tl_, ti_ in enumerate(range(a_, b_)):
                        np_ = 16 if ti_ == 16 else 128
                        nw_ = min(CG, 128 * (tl_ + 1))
                        P.op("dve", TS(Pms[hf_][0:np_, tl_, 0:nw_], iotac[0:np_, 0:nw_], pos_tm[0:np_, ti_, e_:e_ + 1], None, ALU.is_equal),
                             reads=["iotac", "pos_tm"], writes=["Pm%d_%d" % (hf_, tl_)])

            build_P(0)
            for e in range(NEXP):
                P.dma("pool", DMA(wd[:], I["w_down"][e].rearrange("(k p) f -> p k f", p=128)), "wd", writes=["wd"])
                P.dma("sp", DMA(bdb[:], I["b_down"][e:e + 1, :].partition_broadcast(128)), "bdb", writes=["bdb"])
                for hf, (t_lo, t_hi) in enumerate(HALVES):
                    T0 = t_lo * 128
                    ntok = (t_hi - t_lo) * 128 if t_hi < 17 else (16 - t_lo) * 128 + 16
                    Pm = Pms[hf]
                    for kc in range(8):
                        pt, pn = ps()
                        order = list(range(t_hi - t_lo))[::-1]
                        for oi, tl in enumerate(order):
                            ti = t_lo + tl
                            npart = 16 if ti == 16 else 128
                            nw = min(CG, 128 * (tl + 1))
                            P.op("pe", MM(pt[:, 0:nw], h2bf[0:npart, ti, kc * 128:(kc + 1) * 128], Pm[0:npart, tl, 0:nw], oi == 0, oi == len(order) - 1),
                                 reads=["h2bf", "Pm%d_%d" % (hf, tl)], writes=[pn])
                        P.op("act", ACTV(xsT[hf][:, kc, :], pt[:, 0:CG], AF.Copy), reads=[pn], writes=["xsT%d" % hf])
                    for sc, (s0, sn) in enumerate(SCH):
                        pt, pn = ps()
                        tls = [tl for tl in range(t_hi - t_lo) if 128 * (tl + 1) > s0]
                        for oi, tl in enumerate(tls):
                            ti = t_lo + tl
                            npart = 16 if ti == 16 else 128
                            P.op("pe", MM(pt[0:sn, 0:2], Pm[0:npart, tl, s0:s0 + sn], Ghl[0:npart, ti, e, :], oi == 0, oi == len(tls) - 1),
                                 reads=["Ghl", "Pm%d_%d" % (hf, tl)], writes=[pn])
                        P.op("dve", CP(gs2[0:sn, :], pt[0:sn, 0:2]), reads=[pn], writes=["gs2"])
                        P.op("dve", TT(gs[hf][0:sn, sc:sc + 1], gs2[0:sn, 0:1], gs2[0:sn, 1:2], ALU.add), reads=["gs2"], writes=["gs%d" % hf])
                for pc in range(4):
                    w = wpc[npc % 2]
                    wn = "wpc%d" % (npc % 2)
                    npc += 1
                    P.dma("pool", DMA(w[:, :, 0:256], I["w_gate_up"][e][:, pc * 256:(pc + 1) * 256].rearrange("(k p) f -> p k f", p=128)), wn, writes=[wn])
                    P.dma("pool", DMA(w[:, :, 256:512], I["w_gate_up"][e][:, D + pc * 256:D + (pc + 1) * 256].rearrange("(k p) f -> p k f", p=128)), wn, writes=[wn])
                    for hf in range(2):
                        for j in range(2):
                            fc = pc * 2 + j
                            i2 = (fc + hf) % 2
                            pg, pgn = ps()
                            pu, pun = ps()
                            for kc in range(8):
                                P.op("pe", MM(pg[:, 0:CG], w[:, kc, j * 128:(j + 1) * 128], xsT[hf][:, kc, :], kc == 0, kc == 7), reads=[wn, "xsT%d" % hf], writes=[pgn])
                            for kc in range(8):
                                P.op("pe", MM(pu[:, 0:CG], w[:, kc, 256 + j * 128:256 + (j + 1) * 128], xsT[hf][:, kc, :], kc == 0, kc == 7), reads=[wn, "xsT%d" % hf], writes=[pun])
                            P.op("dve", TS(hgc[i2][:], pg[:, 0:CG], bguT[:, fc, e:e + 1], 7.0, ALU.add, ALU.min), reads=[pgn, "bguT"], writes=["hgc%d" % i2])
                            P.op("act", ACTV(sg[i2][:], hgc[i2][:], AF.Sigmoid, scale=1.702), reads=["hgc%d" % i2], writes=["sg%d" % i2])
                            P.op("act", ACTV(hu[i2][:], pu[:, 0:CG], AF.Identity, bias=bguT[:, 8 + fc, e:e + 1]), reads=[pun, "bguT"], writes=["hu%d" % i2])
                            P.op("dve", TS(hu[i2][:], hu[i2][:], 7.0, -7.0, ALU.min, ALU.max), reads=["hu%d" % i2], writes=["hu%d" % i2])
                            P.op("dve", TT(hgc[i2][:], hgc[i2][:], sg[i2][:], ALU.mult), reads=["hgc%d" % i2, "sg%d" % i2], writes=["hgc%d" % i2])
                            P.op("dve", STT(actT[hf][:, fc, :], hu[i2][:], 1.0, hgc[i2][:], ALU.add, ALU.mult), reads=["hgc%d" % i2, "hu%d" % i2], writes=["actT%d" % hf])
                for hf, (t_lo, t_hi) in enumerate(HALVES):
                    T0 = t_lo * 128
                    ntok = (t_hi - t_lo) * 128 if t_hi < 17 else (16 - t_lo) * 128 + 16
                    P.dma("sp", DMA(posbc[:, 0:ntok], posTd[e:e + 1, T0:T0 + ntok].partition_broadcast(128)), "posbc", writes=["posbc"])
                    for sc, (s0, sn) in enumerate(SCH):
                        P.op("dve", TS(STm[0:sn, sc, 0:ntok], posbc[0:sn, 0:ntok], iotaps[0:sn, sc:sc + 1], None, ALU.is_equal),
                             reads=["posbc", "iotaps"], writes=["STm"])
                    for sc, (s0, sn) in enumerate(SCH):
                        for h2_ in range(2):
                            hs = slice(h2_ * 512, (h2_ + 1) * 512)
                            P.op("act", ACTV(bdbg[0:sn, :], bdb[0:sn, hs], AF.Copy, scale=gs[hf][0:sn, sc:sc + 1]), reads=["bdb", "gs%d" % hf], writes=["bdbg"])
                            pt, pn = ps()
                            for fc in range(8):
                                P.op("pe", MM(pt[0:sn, :], actT[hf][:, fc, s0:s0 + sn], wd[:, fc, hs], fc == 0, fc == 7), reads=["actT%d" % hf, "wd"], writes=[pn])
                            P.op("dve", STT(ygt[0:sn, sc, hs], pt[0:sn, :], gs[hf][0:sn, sc:sc + 1], bdbg[0:sn, :], ALU.mult, ALU.add),
                                 reads=[pn, "gs%d" % hf, "bdbg"], writes=["ygt"])
                    if e + 1 < NEXP:
                        build_P(e + 1, only=hf)
                    for tl, ti in enumerate(range(t_lo, t_hi)):
                        npart = 16 if ti == 16 else 128
                        for h2_ in range(2):
                            hs = slice(h2_ * 512, (h2_ + 1) * 512)
                            pt, pn = ps()
                            scs = [(sc, s0, sn) for sc, (s0, sn) in enumerate(SCH) if 128 * (tl + 1) > s0]
                            for oi, (sc, s0, sn) in enumerate(scs):
                                P.op("pe", MM(pt[0:npart, :], STm[0:sn, sc, tl * 128:tl * 128 + npart], ygt[0:sn, sc, hs], oi == 0, oi == len(scs) - 1),
                                     reads=["STm", "ygt"], writes=[pn])
                            on = "oacc%d_%d" % (ti, h2_)
                            if e == 0:
                                P.op("act", ACTV(oacc[0:npart, ti, hs], pt[0:npart, :], AF.Copy), reads=[pn], writes=[on])
                            else:
                                P.op("dve", TT(oacc[0:npart, ti, hs], pt[0:npart, :], oacc[0:npart, ti, hs], ALU.add), reads=[pn, on], writes=[on])
            P.barrier()
            wdf = wd[:].rearrange("p k f -> p (k f)").bitcast(F32)
            gt2b = wdf[:, 0:D]
            gt2s = wdf[:, D:2 * D]
            P.dma("sp", DMA(gt2b, modd[0, MGT2:MGT2 + 1, :].partition_broadcast(128)), "gt2b", writes=["gt2b"])
            P.dma("sp", DMA(gt2s[0:16, :], modd[1:17, MGT2, :]), "gt2s", writes=["gt2s"])
            for ti in range(17):
                smp = ti == 16
                npart = 16 if smp else 128
                NP = slice(0, npart)
                c0 = TOK if smp else ti * 128
                xr = wdf[:, (2 + ti % 2) * D:(3 + ti % 2) * D]
                xrn = "xr%d" % (ti % 2)
                P.dma("sp", DMA(xr[NP, :], x1d[c0:c0 + npart, :]), xrn, writes=[xrn])
                gt = gt2s if smp else gt2b
                on = "oaccf%d" % ti
                P.op("dve", TT(oacc[NP, ti, :], oacc[NP, ti, :], gt[NP, :], ALU.mult), reads=["gt2b", "gt2s"], writes=[on])
                P.op("dve", TT(oacc[NP, ti, :], oacc[NP, ti, :], xr[NP, :], ALU.add), reads=[on, xrn], writes=[on])
                finals.append(P.dma("sp", DMA(O["y"][c0:c0 + npart, :], oacc[NP, ti, :]), "yout%d" % (ti % 4), reads=[on], writes=["o_y"]))

    P.emit(nc, finals)
    return nc


_NC_CACHE = {}


def kernel(**inputs):
    inp = {k: np.ascontiguousarray(np.asarray(v, dtype=np.float32)) for k, v in inputs.items()}
    xp = inp["x_prompt"][0]
    in_maps = []
    for c in range(NCORES):
        m = {}
        xh = np.zeros((HALO + TOK, D), np.float32)
        if c > 0:
            xh[:HALO] = xp[(c - 1) * TOK:c * TOK]
        xh[HALO:] = xp[c * TOK:(c + 1) * TOK]
        m["xh"] = xh
        s0, s1 = c * NS, (c + 1) * NS
        m["xs"] = inp["x_sample"][s0:s1, 0]
        m["cp"] = inp["c_prompt"]
        m["cs"] = inp["c_sample"][s0:s1]
        m["kv1"] = inp["cache_kv_w128"][0, s0:s1].reshape(NS, 128, 1024)
        m["kv2"] = inp["cache_kv_w512"][0, s0:s1, 0::4].reshape(NS, 128, 1024)
        m["kv3"] = inp["cache_kv_w2048"][0, s0:s1, 0::16].reshape(NS, 128, 1024)
        m["spool"] = inp["state_pool"][0, s0:s1]
        m["w_ada"] = inp["w_ada"][0]
        m["b_ada"] = inp["b_ada"]
        m["g1"] = inp["norm_mix_g"]
        m["g2"] = inp["norm_ffn_g"]
        m["w_in"] = inp["w_in"][0]
        m["qg"] = inp["q_norm_g"]
        m["kg"] = inp["k_norm_g"]
        m["rel_bias"] = inp["rel_bias"]
        m["w_pool_mix"] = inp["w_pool_mix"][0]
        m["pool_scale"] = inp["pool_scale"]
        m["w_up_attn"] = inp["w_up_attn"][0]
        m["w_up_pool"] = inp["w_up_pool"][0]
        m["w_out"] = inp["w_out"][0]
        m["w_router"] = inp["w_router"][0]
        m["b_router"] = inp["b_router"]
        m["w_gate_up"] = inp["w_gate_up"][0]
        m["b_gate_up"] = inp["b_gate_up"][0]
        m["w_down"] = inp["w_down"][0]
        m["b_down"] = inp["b_down"][0]
        for k, v in static_consts(c).items():
            m["c_" + k] = v
        m = {k: np.ascontiguousarray(v, dtype=np.float32) for k, v in m.items() if (k in IN_SHAPES or k.startswith("c_"))}
        in_maps.append(m)
    if "nc" not in _NC_CACHE:
        _NC_CACHE["nc"] = build()
    res = run_bass_kernel_spmd(_NC_CACHE["nc"], in_maps, core_ids=list(range(NCORES)))
    R = res.results
    if os.environ.get("MK_DBG"):
        _NC_CACHE["res"] = R
    y_prompt = np.concatenate([R[c]["y"][:TOK] for c in range(NCORES)], 0)[None]
    y_sample = np.concatenate([R[c]["y"][TOK:] for c in range(NCORES)], 0)[:, None, :]
    L = R[NCORES - 1]
    kv128 = L["kvp1"].reshape(1, 1, 128, 2, 8, 64)
    kv512 = L["kvp2"].reshape(1, 1, 512, 2, 8, 64)
    kv2048 = L["kvp3"].reshape(1, 1, 2048, 2, 8, 64)
    pool_p = L["poolp"][1:16].reshape(1, 1, 15, 512)
    kvs = np.concatenate([R[c]["kvs"] for c in range(NCORES)], 0)
    ks = [kvs[:, g].reshape(1, 128, 1, 2, 8, 64) for g in range(3)]
    pool_s = np.concatenate([R[c]["pools"] for c in range(NCORES)], 0)[None]
    return (y_prompt.astype(np.float32), y_sample.astype(np.float32), kv128, kv512, kv2048, pool_p,
            ks[0], ks[1], ks[2], pool_s.astype(np.float32))
```
